# Optimizing a Trainium2 kernel written in Bass

```python
import math
import jax, jax.numpy as jnp
from jax import lax
import numpy as np

D_MODEL = 1024
BATCH = 2
SEQ = 8192
DEPTH = 2

BLOCK = 128
EPS = 1e-6
ML_HEADS = 4
ML_HEAD_DIM = 128
ML_WIDTH = ML_HEADS * ML_HEAD_DIM
ML_CONV = 4
SG_GROUPS = 4
SG_GROUP_DIM = 128
SG_WIDTH = SG_GROUPS * SG_GROUP_DIM
FOX_HEADS = 4
FOX_HEAD_DIM = 128
FOX_WIDTH = FOX_HEADS * FOX_HEAD_DIM
DIFF_HEADS = 4
DIFF_QK_DIM = 64
DIFF_V_DIM = 128
DIFF_WIDTH = DIFF_HEADS * DIFF_V_DIM
ROPE_THETA = 500000.0
ROPE_DIMS = DIFF_QK_DIM // 4
D_FF = 2816
N_EXPERTS = 8
TOP_K = 2
D_FF_EXPERT = 3584

EVEN_SPLITS = (ML_WIDTH, ML_WIDTH, ML_WIDTH, ML_WIDTH, 2 * ML_HEADS, SG_WIDTH, SG_WIDTH)
ODD_SPLITS = (FOX_WIDTH, FOX_WIDTH, FOX_WIDTH, FOX_HEADS,
              DIFF_HEADS * 2 * DIFF_QK_DIM, DIFF_HEADS * 2 * DIFF_QK_DIM, DIFF_WIDTH)
EVEN_PROJ = sum(EVEN_SPLITS)
ODD_PROJ = sum(ODD_SPLITS)

kernel_name = "hybrid_mlstm_gmlp_fox_diff_moe"


def split_cols(z, sizes):
    idx = np.cumsum(sizes)[:-1].tolist()
    return jnp.split(z, idx, axis=-1)


def rms_norm(x, g):
    xf = x.astype(jnp.float32)
    y = xf * lax.rsqrt(jnp.mean(xf * xf, axis=-1, keepdims=True) + EPS)
    return (y * g.astype(jnp.float32)).astype(x.dtype)


def causal_dwconv(x, w, b):
    C = x.shape[-1]
    y = lax.conv_general_dilated(x, w[:, None, :].astype(x.dtype), window_strides=(1,),
                                 padding=((w.shape[0] - 1, 0),),
                                 dimension_numbers=("NWC", "WIO", "NWC"),
                                 feature_group_count=C)
    return y + b


def swiglu(t, w_gate_up, w_down):
    g, u = jnp.split(t @ w_gate_up, 2, axis=-1)
    return (jax.nn.silu(g) * u) @ w_down


def partial_rope(x):
    S = x.shape[-2]
    half = ROPE_DIMS // 2
    inv = ROPE_THETA ** (-jnp.arange(half, dtype=jnp.float32) / half)
    ang = jnp.arange(S, dtype=jnp.float32)[:, None] * inv[None, :]
    cos, sin = jnp.cos(ang), jnp.sin(ang)
    xf = x[..., :ROPE_DIMS].astype(jnp.float32)
    x1, x2 = xf[..., :half], xf[..., half:]
    rot = jnp.concatenate([x1 * cos - x2 * sin, x2 * cos + x1 * sin], axis=-1)
    return jnp.concatenate([rot.astype(x.dtype), x[..., ROPE_DIMS:]], axis=-1)


def mlstm_chunkwise(q, k, v, i_pre, f_pre):
    B, H, S, D = q.shape
    L = BLOCK
    NC = S // L
    out_dtype = v.dtype
    f32 = jnp.float32
    q = q.astype(f32) * (D ** -0.5)
    k = k.astype(f32)
    v = v.astype(f32)
    log_f = jax.nn.log_sigmoid(f_pre.astype(f32))
    log_i = i_pre.astype(f32)

    def to_chunks(a):
        return jnp.moveaxis(a.reshape(B, H, NC, L, *a.shape[3:]), 2, 0)

    causal = jnp.tril(jnp.ones((L, L), dtype=bool))

    def step(carry, inp):
        C, n, m = carry
        qb, kb, vb, lfb, lib = inp
        b = jnp.cumsum(lfb, axis=-1)
        dmat = jnp.where(causal, b[..., :, None] - b[..., None, :] + lib[..., None, :], -jnp.inf)
        inter = b + m[..., None]
        m_row = jnp.maximum(inter, jnp.max(dmat, axis=-1))
        w_intra = jnp.exp(dmat - m_row[..., None])
        w_inter = jnp.exp(inter - m_row)
        s = jnp.einsum('bhtd,bhsd->bhts', qb, kb) * w_intra
        num = jnp.einsum('bhts,bhsd->bhtd', s, vb) + w_inter[..., None] * jnp.einsum('bhed,bhtd->bhte', C, qb)
        den = jnp.sum(s, axis=-1) + w_inter * jnp.einsum('bhd,bhtd->bht', n, qb)
        h = num / jnp.maximum(jnp.abs(den), jnp.exp(-m_row))[..., None]
        b_last = b[..., -1]
        g = b_last[..., None] - b + lib
        m_new = jnp.maximum(b_last + m, jnp.max(g, axis=-1))
        w_s = jnp.exp(g - m_new[..., None])
        decay = jnp.exp(b_last + m - m_new)
        C_new = decay[..., None, None] * C + jnp.einsum('bhs,bhse,bhsd->bhed', w_s, vb, kb)
        n_new = decay[..., None] * n + jnp.einsum('bhs,bhsd->bhd', w_s, kb)
        return (C_new, n_new, m_new), h

    init = (jnp.zeros((B, H, D, D), f32), jnp.zeros((B, H, D), f32), jnp.zeros((B, H), f32))
    _, hs = lax.scan(step, init, (to_chunks(q), to_chunks(k), to_chunks(v),
                                  to_chunks(log_f), to_chunks(log_i)))
    return jnp.moveaxis(hs, 0, 2).reshape(B, H, S, D).astype(out_dtype)


def spatial_gating(u, v, w_s, b_s):
    B, S, G, Dg = v.shape
    L = BLOCK
    NC = S // L
    tril = jnp.tril(jnp.ones((L, L), dtype=bool))
    w = jnp.where(tril, w_s, jnp.zeros_like(w_s))
    mixed = jnp.einsum('gts,bnsgc->bntgc', w, v.reshape(B, NC, L, G, Dg)) + b_s.T[None, None, :, :, None]
    return u * mixed.reshape(B, S, G, Dg)


def blocked_causal_attention(q, k, v, scale, log_fcum=None):
    B, H, G, S, Dk = q.shape
    Dv = v.shape[-1]
    NB = S // BLOCK
    q_blocks = jnp.moveaxis(q.reshape(B, H, G, NB, BLOCK, Dk), 3, 0)
    k_pos = jnp.arange(S)
    xs = (q_blocks, jnp.arange(NB))
    if log_fcum is not None:
        xs = xs + (jnp.moveaxis(log_fcum.reshape(B, H, NB, BLOCK), 2, 0),)

    def one_block(args):
        q_blk, idx = args[0], args[1]
        q_pos = idx * BLOCK + jnp.arange(BLOCK)
        logits = jnp.einsum('bhgqd,bhgkd->bhgqk', q_blk, k).astype(jnp.float32) * scale
        if log_fcum is not None:
            fq = args[2]
            logits = logits + (fq[..., :, None] - log_fcum[..., None, :])[:, :, None]
        mask = k_pos[None, :] <= q_pos[:, None]
        logits = jnp.where(mask, logits, -jnp.inf)
        p = jax.nn.softmax(logits, axis=-1)
        return jnp.einsum('bhgqk,bhkv->bhgqv', p.astype(v.dtype), v)

    out = lax.map(one_block, xs)
    return jnp.moveaxis(out, 0, 3).reshape(B, H, G, S, Dv)


def even_mixer(h, w_in, ml_conv_w, ml_conv_b, ml_gate_b, ml_norm_g, sg_norm_g, sg_w, sg_b, w_out):
    B, S, _ = h.shape
    q, k, v, o, gates, u, vs = split_cols(h @ w_in, EVEN_SPLITS)
    qk = jax.nn.silu(causal_dwconv(jnp.concatenate([q, k], axis=-1), ml_conv_w, ml_conv_b))
    q, k = jnp.split(qk, 2, axis=-1)

    def heads(t):
        return t.reshape(B, S, ML_HEADS, ML_HEAD_DIM).transpose(0, 2, 1, 3)

    i_pre = (gates[..., :ML_HEADS] + ml_gate_b[0]).transpose(0, 2, 1)
    f_pre = (gates[..., ML_HEADS:] + ml_gate_b[1]).transpose(0, 2, 1)
    hm = mlstm_chunkwise(heads(q), heads(k), heads(v), i_pre, f_pre).transpose(0, 2, 1, 3)
    hm = rms_norm(hm, ml_norm_g.reshape(ML_HEADS, ML_HEAD_DIM))
    y_ml = jax.nn.sigmoid(o) * hm.reshape(B, S, ML_WIDTH)
    u = jax.nn.gelu(u)
    vs = rms_norm(jax.nn.gelu(vs), sg_norm_g)
    y_sg = spatial_gating(u.reshape(B, S, SG_GROUPS, SG_GROUP_DIM),
                          vs.reshape(B, S, SG_GROUPS, SG_GROUP_DIM), sg_w, sg_b).reshape(B, S, SG_WIDTH)
    return jnp.concatenate([y_ml, y_sg], axis=-1) @ w_out


def odd_mixer(h, w_in, fox_f_b, diff_lambda, diff_norm_g, w_out, lambda_init):
    B, S, _ = h.shape
    fq, fk, fv, ff, dq, dk, dv = split_cols(h @ w_in, ODD_SPLITS)
    log_f = jax.nn.log_sigmoid(ff.astype(jnp.float32) + fox_f_b.astype(jnp.float32))
    F = jnp.cumsum(log_f, axis=1).transpose(0, 2, 1)

    def fheads(t):
        return t.reshape(B, S, FOX_HEADS, FOX_HEAD_DIM).transpose(0, 2, 1, 3)

    y_fox = blocked_causal_attention(fheads(fq)[:, :, None], fheads(fk)[:, :, None], fheads(fv),
                                     FOX_HEAD_DIM ** -0.5, F)[:, :, 0]
    y_fox = y_fox.transpose(0, 2, 1, 3).reshape(B, S, FOX_WIDTH)
    def dheads(t):
        return t.reshape(B, S, DIFF_HEADS, 2, DIFF_QK_DIM).transpose(0, 2, 3, 1, 4)

    qd = partial_rope(dheads(dq))
    kd = partial_rope(dheads(dk))
    vd = dv.reshape(B, S, DIFF_HEADS, DIFF_V_DIM).transpose(0, 2, 1, 3)
    a = blocked_causal_attention(qd, kd, vd, DIFF_QK_DIM ** -0.5)
    lam_f = diff_lambda.astype(jnp.float32)
    lam = (jnp.exp(jnp.sum(lam_f[0] * lam_f[1])) - jnp.exp(jnp.sum(lam_f[2] * lam_f[3])) + lambda_init)
    y_d = a[:, :, 0] - lam.astype(a.dtype) * a[:, :, 1]
    y_d = rms_norm(y_d, diff_norm_g) * (1.0 - lambda_init)
    y_d = y_d.transpose(0, 2, 1, 3).reshape(B, S, DIFF_WIDTH)
    return jnp.concatenate([y_fox, y_d], axis=-1) @ w_out


def moe_swiglu(h, w_router, w_gate_up, w_down):
    B, S, D = h.shape
    t = h.reshape(B * S, D)
    logits = (t @ w_router).astype(jnp.float32)
    top_v, top_i = lax.top_k(logits, TOP_K)
    top_w = jax.nn.softmax(top_v, axis=-1)
    gates = jnp.sum(jax.nn.one_hot(top_i, N_EXPERTS, dtype=jnp.float32) * top_w[..., None], axis=1)
    out = jnp.zeros_like(t)
    for e in range(N_EXPERTS):
        out = out + gates[:, e:e + 1].astype(t.dtype) * swiglu(t, w_gate_up[e], w_down[e])
    return out.reshape(B, S, D)


def setup_inputs(seed: int = 0) -> dict:
    key = jax.random.key(seed)
    ks = iter(jax.random.split(key, 40))
    NE = (DEPTH + 1) // 2
    NO = DEPTH // 2
    D = D_MODEL

    def nrm(shape, scale):
        return jax.random.normal(next(ks), shape, jnp.float32) * scale

    def gain(shape):
        return 1.0 + nrm(shape, 0.02)

    x = nrm((BATCH, SEQ, D), 1.0)
    ml_gate_b = jnp.concatenate([
        nrm((NE, 1, ML_HEADS), 0.1),
        jnp.linspace(3.0, 6.0, ML_HEADS, dtype=jnp.float32)[None, None, :] + nrm((NE, 1, ML_HEADS), 0.1)], axis=1)
    return {
        "x": x,
        "even_norm_mix": gain((NE, D)),
        "even_w_in": nrm((NE, D, EVEN_PROJ), D ** -0.5),
        "even_ml_conv_w": nrm((NE, ML_CONV, 2 * ML_WIDTH), ML_CONV ** -0.5),
        "even_ml_conv_b": nrm((NE, 2 * ML_WIDTH), 0.02),
        "even_ml_gate_b": ml_gate_b,
        "even_ml_norm_g": gain((NE, ML_WIDTH)),
        "even_sg_norm_g": gain((NE, SG_WIDTH)),
        "even_sg_w": nrm((NE, SG_GROUPS, BLOCK, BLOCK), BLOCK ** -0.5),
        "even_sg_b": 1.0 + nrm((NE, SG_GROUPS, BLOCK), 0.02),
        "even_w_out": nrm((NE, ML_WIDTH + SG_WIDTH, D), (ML_WIDTH + SG_WIDTH) ** -0.5),
        "even_norm_ffn": gain((NE, D)),
        "ffn_w_gate_up": nrm((NE, D, 2 * D_FF), D ** -0.5),
        "ffn_w_down": nrm((NE, D_FF, D), D_FF ** -0.5),
        "odd_norm_mix": gain((NO, D)),
        "odd_w_in": nrm((NO, D, ODD_PROJ), D ** -0.5),
        "odd_fox_f_b": jnp.linspace(2.0, 5.0, FOX_HEADS, dtype=jnp.float32)[None, :] + nrm((NO, FOX_HEADS), 0.1),
        "odd_diff_lambda": nrm((NO, 4, DIFF_QK_DIM), 0.1),
        "odd_diff_norm_g": gain((NO, DIFF_V_DIM)),
        "odd_w_out": nrm((NO, FOX_WIDTH + DIFF_WIDTH, D), (FOX_WIDTH + DIFF_WIDTH) ** -0.5),
        "odd_norm_ffn": gain((NO, D)),
        "moe_w_router": nrm((NO, D, N_EXPERTS), D ** -0.5),
        "moe_w_gate_up": nrm((NO, N_EXPERTS, D, 2 * D_FF_EXPERT), D ** -0.5),
        "moe_w_down": nrm((NO, N_EXPERTS, D_FF_EXPERT, D), D_FF_EXPERT ** -0.5),
        "final_norm": gain((D,)),
    }


def reference(x, even_norm_mix, even_w_in, even_ml_conv_w, even_ml_conv_b, even_ml_gate_b,
              even_ml_norm_g, even_sg_norm_g, even_sg_w, even_sg_b, even_w_out, even_norm_ffn,
              ffn_w_gate_up, ffn_w_down, odd_norm_mix, odd_w_in, odd_fox_f_b, odd_diff_lambda,
              odd_diff_norm_g, odd_w_out, odd_norm_ffn, moe_w_router, moe_w_gate_up, moe_w_down,
              final_norm):
    for layer in range(DEPTH):
        p = layer // 2
        if layer % 2 == 0:
            x = x + even_mixer(rms_norm(x, even_norm_mix[p]), even_w_in[p], even_ml_conv_w[p],
                               even_ml_conv_b[p], even_ml_gate_b[p], even_ml_norm_g[p],
                               even_sg_norm_g[p], even_sg_w[p], even_sg_b[p], even_w_out[p])
            x = x + swiglu(rms_norm(x, even_norm_ffn[p]), ffn_w_gate_up[p], ffn_w_down[p])
        else:
            lambda_init = 0.8 - 0.6 * math.exp(-0.3 * layer)
            x = x + odd_mixer(rms_norm(x, odd_norm_mix[p]), odd_w_in[p], odd_fox_f_b[p],
                              odd_diff_lambda[p], odd_diff_norm_g[p], odd_w_out[p], lambda_init)
            x = x + moe_swiglu(rms_norm(x, odd_norm_ffn[p]), moe_w_router[p], moe_w_gate_up[p], moe_w_down[p])
    return rms_norm(x, final_norm)
```

```python
from contextlib import ExitStack
import math
import numpy as np
import ml_dtypes
import concourse.bass as bass
import concourse.mybir as mybir
from concourse.bass_utils import run_bass_kernel_spmd

F32 = mybir.dt.float32
BF16 = mybir.dt.bfloat16
AF = mybir.ActivationFunctionType
ALU = mybir.AluOpType
AX = mybir.AxisListType

NCORES = 8
D = 1024
SEQ = 8192
EPS = 1e-6
SEM_EPOCH = 30000
DEBUG = False
E_OVERRIDE = None
PIPE_DEPTH = 5
DBG_SKIP = set()


class Op:
    __slots__ = ("eng", "fn", "deps", "is_dma", "sem", "val", "flag", "key", "force", "inc")

    def __init__(self, eng, fn, is_dma=False, key=None):
        self.eng = eng
        self.fn = fn
        self.deps = []
        self.is_dma = is_dma
        self.sem = None
        self.val = None
        self.flag = False
        self.key = key
        self.force = False
        self.inc = 16


class Prog:
    ENGS = ("pe", "act", "dve", "pool", "sp")

    def __init__(self, nc, pfx="", semstack=None):
        self.nc = nc
        self.pfx = pfx
        self.semstack = semstack
        self.ops = {e: [] for e in self.ENGS}
        self.last_w = {}
        self.readers = {}
        self.stack = ExitStack()
        self.dma_sems = {}
        self.dma_cnt = {}
        self.n_sems = 0
        self._group = None
        self.same_engine_sync = True

    def sbuf(self, name, shape, dtype):
        return self.stack.enter_context(self.nc.sbuf_tensor(self.pfx + name, list(shape), dtype))

    def psum(self, name, shape, dtype):
        return self.stack.enter_context(self.nc.psum_tensor(self.pfx + name, list(shape), dtype))

    def _new_sem(self, name):
        self.n_sems += 1
        if self.semstack == "raw":
            return self.nc.alloc_semaphore(name=self.pfx + name)
        return self.stack.enter_context(self.nc.semaphore(self.pfx + name))

    def finish(self, barrier=True):
        if barrier:
            dmas = [op for e in self.ENGS for op in self.ops[e] if op.is_dma]
            ends = []
            for e in self.ENGS:
                last = [op for op in self.ops[e] if not op.is_dma]
                op = self.add(e, lambda eng: eng.nop(), force=True)
                op.deps = last[-1:] if last else []
                ends.append(op)
            for e in self.ENGS:
                op = self.add(e, lambda eng: eng.nop(), force=True)
                op.deps = [x for x in ends if x.eng != e] + dmas
        self.emit()
        self.close()

    def _track(self, op, reads, writes):
        deps = []
        seen = set()

        def add(d):
            if d is None or id(d) in seen or d is op:
                return
            seen.add(id(d))
            deps.append(d)

        for t in reads:
            add(self.last_w.get(t))
        for t in writes:
            add(self.last_w.get(t))
            for r in self.readers.get(t, ()):
                add(r)
        op.deps = deps
        for t in reads:
            self.readers.setdefault(t, []).append(op)
        for t in writes:
            self.last_w[t] = op
            self.readers[t] = []

    def _commit(self, op, reads, writes):
        if self._group is not None:
            self._group.append((op, tuple(reads), tuple(writes)))
        else:
            self._track(op, reads, writes)
            self.ops[op.eng].append(op)
        return op

    def begin_group(self):
        self._group = []

    def end_group(self):
        g, self._group = self._group, None
        return g

    def interleave(self, groups, depth, window=8):
        if not groups:
            return
        last = []
        lastw = []
        for g in groups:
            d, dw = {}, {}
            for i, (op, reads, writes) in enumerate(g):
                for t in reads:
                    d[t] = i
                for t in writes:
                    d[t] = i
                    dw[t] = i
            last.append(d)
            lastw.append(dw)
        L = max(len(g) for g in groups)
        step = max(1, L // depth)
        pos = [0] * len(groups)
        t = 0
        done = 0
        first_active = 0
        while done < len(groups):
            progressed = False
            for gi in range(first_active, len(groups)):
                if gi * step > t:
                    break
                if pos[gi] >= len(groups[gi]):
                    continue
                op, reads, writes = groups[gi][pos[gi]]
                ok = True
                for gj in range(max(first_active, gi - window), gi):
                    lj = last[gj]
                    lwj = lastw[gj]
                    pj = pos[gj]
                    for tk in writes:
                        if tk in lj and pj <= lj[tk]:
                            ok = False
                            break
                    if ok:
                        for tk in reads:
                            if tk in lwj and pj <= lwj[tk]:
                                ok = False
                                break
                    if not ok:
                        break
                if not ok:
                    continue
                pos[gi] += 1
                progressed = True
                if pos[gi] == len(groups[gi]):
                    done += 1
                self._track(op, reads, writes)
                self.ops[op.eng].append(op)
            while first_active < len(groups) and pos[first_active] >= len(groups[first_active]):
                first_active += 1
            t += 1
            assert progressed or first_active >= len(groups) or first_active * step > t - 1, "interleave stuck"

    def add(self, eng, fn, reads=(), writes=(), force=False):
        op = Op(eng, fn)
        op.force = force
        return self._commit(op, reads, writes)

    def dma(self, q, out, in_, reads=(), writes=(), key=None, **kw):
        if key is None:
            key = ("auto", writes[0] if writes else reads[0])
        op = Op(q, None, is_dma=True, key=key)
        op.fn = lambda e: e.dma_start(out=out, in_=in_, **kw)
        return self._commit(op, reads, writes)

    def dma_fn(self, q, fn, reads=(), writes=(), key=None, inc=16):
        op = Op(q, fn, is_dma=True, key=key)
        op.inc = inc
        return self._commit(op, reads, writes)

    def mm(self, out, lhsT, rhs, start, stop, reads, writes):
        return self.add("pe", lambda e: e.matmul(out, lhsT=lhsT, rhs=rhs, start=start, stop=stop),
                        reads, writes)

    def tr(self, out, in_, ident, reads, writes):
        return self.add("pe", lambda e: e.transpose(out=out, in_=in_, identity=ident), reads, writes)

    def act(self, out, in_, func, reads, writes, eng="act", **kw):
        force = any(not isinstance(v, (int, float)) for k, v in kw.items() if k in ("bias", "scale"))
        return self.add(eng, lambda e: e.activation(out=out, in_=in_, func=func, **kw), reads, writes,
                        force=force)

    def copy(self, eng, out, in_, reads, writes):
        if eng == "act":
            return self.add("act", lambda e: e.copy(out=out, in_=in_), reads, writes)
        return self.add(eng, lambda e: e.tensor_copy(out=out, in_=in_), reads, writes)

    def tt(self, eng, out, in0, in1, op, reads, writes):
        return self.add(eng, lambda e: e.tensor_tensor(out=out, in0=in0, in1=in1, op=op), reads, writes)

    def ts(self, eng, out, in0, s1, s2, op0, op1, reads, writes):
        force = not (isinstance(s1, (int, float)) and (s2 is None or isinstance(s2, (int, float))))
        if op1 is None:
            return self.add(eng, lambda e: e.tensor_scalar(out=out, in0=in0, scalar1=s1, scalar2=None,
                                                           op0=op0), reads, writes, force=force)
        return self.add(eng, lambda e: e.tensor_scalar(out=out, in0=in0, scalar1=s1, scalar2=s2,
                                                       op0=op0, op1=op1), reads, writes, force=force)

    def stt(self, eng, out, in0, scalar, in1, op0, op1, reads, writes):
        force = not isinstance(scalar, (int, float))
        return self.add(eng, lambda e: e.scalar_tensor_tensor(out=out, in0=in0, scalar=scalar, in1=in1,
                                                              op0=op0, op1=op1), reads, writes, force=force)

    def memset(self, eng, ap, val, writes):
        return self.add(eng, lambda e: e.memset(ap, val), (), writes)

    def emit(self):
        nc = self.nc
        for e in self.ENGS:
            for op in self.ops[e]:
                for d in op.deps:
                    if d.is_dma:
                        continue
                    if d.eng != op.eng or op.is_dma or op.force or (self.same_engine_sync and op.eng != "pe"):
                        d.flag = True
        for e in self.ENGS:
            cnt = 0
            sem = None
            for op in self.ops[e]:
                if op.is_dma:
                    k = op.key
                    if k not in self.dma_sems:
                        self.dma_sems[k] = self._new_sem("d%d" % len(self.dma_sems))
                        self.dma_cnt[k] = 0
                    self.dma_cnt[k] += op.inc
                    op.sem = self.dma_sems[k]
                    op.val = self.dma_cnt[k]
                elif op.flag:
                    if sem is None or cnt >= SEM_EPOCH:
                        sem = self._new_sem("e_%s_%d" % (e, self.n_sems))
                        cnt = 0
                    cnt += 1
                    op.sem = sem
                    op.val = cnt
        engmap = {"pe": "tensor", "act": "scalar", "dve": "vector", "pool": "gpsimd", "sp": "sync"}
        with nc.Block() as block:
            for e in self.ENGS:
                ops = self.ops[e]
                if not ops:
                    continue

                def body(eng, ops=ops, e=e):
                    waited = {}
                    for op in ops:
                        need = {}
                        for d in op.deps:
                            if d.sem is None:
                                continue
                            if (not d.is_dma) and d.eng == e and not (
                                    op.is_dma or op.force or (self.same_engine_sync and e != "pe")):
                                continue
                            sid = id(d.sem)
                            if waited.get(sid, 0) >= d.val:
                                continue
                            if sid not in need or need[sid][1] < d.val:
                                need[sid] = (d.sem, d.val)
                        for sid, (s, v) in need.items():
                            eng.wait_ge(s, v)
                            waited[sid] = v
                        ins = op.fn(eng)
                        if op.is_dma:
                            ins.then_inc(op.sem, op.inc)
                        elif op.flag:
                            ins.then_inc(op.sem, 1)

                getattr(block, engmap[e])(body)

    def close(self):
        self.stack.close()


def norm_rows(p, x_ap, xtok, g_bc, hn_ap, hntok, st, sttok, junk, junktok, D_=D):
    p.memset("dve", st[:, 0:1], 0.0, [sttok])
    p.act(junk, x_ap, AF.Square, [xtok, sttok], [junktok, sttok], accum_out=st[:, 0:1])
    p.ts("dve", st[:, 1:2], st[:, 0:1], 1.0 / D_, EPS, ALU.mult, ALU.add, [sttok], [sttok])
    p.act(st[:, 2:3], st[:, 1:2], AF.Sqrt, [sttok], [sttok])
    p.add("dve", lambda e: e.reciprocal(out=st[:, 3:4], in_=st[:, 2:3]), [sttok], [sttok])
    p.stt("dve", hn_ap, x_ap, st[:, 3:4], g_bc, ALU.mult, ALU.mult, [xtok, sttok, "const"], [hntok])


def build_tok_kernel(kind, fused=None):
    moe = kind == "moe"
    NTOK = 2048
    PASS = 1024
    NPASS = NTOK // PASS
    TPP = PASS // 128
    if moe:
        E, DFF = E_OVERRIDE or 8, 3584
    else:
        E, DFF = 1, 2816
    NFC = DFF // 128
    UC = 2
    NU = NFC // UC
    PU = 4
    nparts = (NU + PU - 1) // PU
    base, extra = NU // nparts, NU % nparts
    parts, u0 = [], 0
    for i in range(nparts):
        n_ = base + (1 if i < extra else 0)
        parts.append(list(range(u0, u0 + n_)))
        u0 += n_

    if fused is None:
        nc = bass.Bass("TRN2", target_bir_lowering=False)
        pfx = ""
        _dt = lambda name, shape, dt, kind: nc.dram_tensor(name, shape, dt, kind=kind)
    else:
        nc, pfx = fused["nc"], "tk%d_" % (1 if moe else 0)
        _dt = lambda name, shape, dt, kind: fused["tensor"](pfx + name, shape, dt, kind)
    class _W:
        def dram_tensor(self, name, shape, dt, kind="Internal"):
            return _dt(name, shape, dt, kind)
    ncd = _W()
    xin = ncd.dram_tensor("xin", [NTOK, D], F32, kind="ExternalInput").ap()
    if fused is None:
        yT = ncd.dram_tensor("yT", [D, NTOK], BF16, kind="ExternalInput").ap()
    else:
        yg = ncd.dram_tensor("yT", [4 * SEQ, 256], BF16, kind="ExternalInput").ap()
    w_out = ncd.dram_tensor("w_out", [D, D], F32, kind="ExternalInput").ap()
    g_norm = ncd.dram_tensor("g_norm", [D], F32, kind="ExternalInput").ap()
    w_gu = ncd.dram_tensor("w_gu", [E, D, 2 * DFF], F32, kind="ExternalInput").ap()
    w_d = ncd.dram_tensor("w_d", [E, DFF, D], F32, kind="ExternalInput").ap()
    ident_d = ncd.dram_tensor("ident", [128, 128], F32, kind="ExternalInput").ap()
    if moe:
        w_r = ncd.dram_tensor("w_r", [128, 64], F32, kind="ExternalInput").ap()
        g_fin = ncd.dram_tensor("g_fin", [D], F32, kind="ExternalInput").ap()
    xout = ncd.dram_tensor("xout", [NTOK, D], F32, kind="ExternalOutput").ap()
    hoist = (fused is not None) and not moe
    if hoist:
        g_next = ncd.dram_tensor("g_next", [D], F32, kind="ExternalInput").ap()
        hnout = ncd.dram_tensor("hnout", [NTOK, D], BF16, kind="ExternalOutput").ap()

    p = Prog(nc, pfx, fused['semstack'] if fused else None)
    NSL = 3
    ident = p.sbuf("ident_sb", [128, 128], F32)
    gbc = p.sbuf("gbc", [128, D], F32)
    mhalf = p.sbuf("mhalf", [128, 1], F32)
    wo = p.sbuf("wo", [128, 8, D], BF16)
    acc = [p.sbuf("acc%d" % i, [128, D], F32) for i in range(TPP)]
    hT = p.sbuf("hT", [128, 8, PASS], BF16)
    yTs = p.sbuf("yTs", [128, 8, PASS], BF16)
    if fused is not None:
        identb = p.sbuf("identb", [128, 128], BF16)
        ysb = p.sbuf("ysb", [128, TPP, 4, 256], BF16)
    NCH_PART = max(len(pp) for pp in parts) * UC
    actT = p.sbuf("actT", [128, NCH_PART, PASS], BF16)
    NWS = 3
    wg = [p.sbuf("wg%d" % i, [128, 8, UC * 128], BF16) for i in range(NWS)]
    wu = [p.sbuf("wu%d" % i, [128, 8, UC * 128], BF16) for i in range(NWS)]
    wd = [[p.sbuf("wd%d_%d" % (i, j), [128, UC, D], BF16) for j in range(PU)] for i in range(2)]
    hn = [p.sbuf("hn%d" % i, [128, D], F32) for i in range(NSL)]
    junk = p.sbuf("junk", [128, D], BF16)
    if hoist:
        gnx = p.sbuf("gnx", [128, D], F32)
        hnb = [p.sbuf("hnb%d" % i, [128, D], BF16) for i in range(2)]
    st = [p.sbuf("st%d" % i, [128, 8], F32) for i in range(NSL)]
    sg = [p.sbuf("sg%d" % i, [128, 512], BF16) for i in range(2)]
    if moe:
        gfin = p.sbuf("gfin", [128, D], F32)
        wr = p.sbuf("wr", [128, 8, 8], F32)
        hT32 = [p.sbuf("hT32_%d" % i, [128, 8, 128], F32) for i in range(NSL)]
        gates = [p.sbuf("gates%d" % i, [128, 8], F32) for i in range(TPP)]
        rt = [p.sbuf("rt%d" % i, [128, 40], F32) for i in range(NSL)]
    tp = p.psum("tp", [128, 8, 128], F32)
    dn = [p.psum("dn%d" % i, [128, 512], F32) for i in range(2)]
    gA = [p.psum("gA%d" % i, [128, 512], F32) for i in range(2)]
    uB = [p.psum("uB%d" % i, [128, 512], F32) for i in range(2)]

    p.dma("sp", ident[:], ident_d, writes=["const_id"])
    p.memset("dve", mhalf[:], -0.5, ["c_mhalf"])
    if fused is not None:
        p.copy("dve", identb[:], ident[:], ["const_id"], ["const_idb"])
    p.dma("sp", gbc[:], g_norm.partition_broadcast(128), writes=["const_g"])
    if hoist:
        p.dma("sp", gnx[:], g_next.partition_broadcast(128), writes=["const_gnx"])
    p.dma("pool", wo[:], w_out.rearrange("(kc p) c -> p kc c", p=128), writes=["const_wo"])
    if moe:
        p.dma("sp", gfin[:], g_fin.partition_broadcast(128), writes=["const_gf"])
        p.dma("sp", wr[:], w_r.rearrange("p (kc e) -> p kc e", e=8), writes=["const_wr"])

    ucount = 0
    pcount = 0
    rank_cache = {}
    dncount = 0
    gucount = 0
    for ps_i in range(NPASS):
        t0p = ps_i * PASS
        pgroups = []
        for ti in range(TPP):
            p.begin_group()
            tok0 = t0p + ti * 128
            ytok = "yTs"
            if fused is None:
                if ti == 0:
                    p.dma("sp", yTs[:], yT.rearrange("(kc p) t -> p kc t", p=128)[:, :, t0p:t0p + PASS],
                          writes=[ytok])
            else:
                ytok = ("yTs", ti)
                if ti == 0:
                    for h_ in range(4):
                        def _ld(e, tl=t0p, h_=h_):
                            if "rank" not in rank_cache:
                                rank_cache["rank"] = e.snap(e.partition_id() % 4, min_val=0, max_val=3)
                            rank = rank_cache["rank"]
                            src = yg.rearrange("(k h t) c -> h k t c", k=4, h=4)[h_, bass.ds(rank, 1), tl:tl + PASS]
                            return e.dma_start(out=ysb[:, :, h_, :], in_=src.rearrange("k (i p) c -> (k p) i c", p=128))

                        p.dma_fn("sp", _ld, reads=[], writes=[("ysb", h_)], key=("ysb", h_))
                ys_ = ysb[:, ti]
                tpy = uB[1][:].bitcast(BF16).rearrange("p (k t) -> p k t", k=8)
                for h_ in range(4):
                    for part in range(2):
                        p.tr(tpy[:, part * 4 + h_, :], ys_[:, h_, part * 128:(part + 1) * 128], identb[:],
                             [("ysb", h_), "const_idb"], [("uB", 1)])
                p.copy("act", yTs[:, :, ti * 128:(ti + 1) * 128], tpy, [], [("uB", 1), ytok])
            a = acc[ti]
            atok = ("acc", ti)
            p.dma("sp", a[:], xin[tok0:tok0 + 128, :], writes=[atok], key=("acc", ti))
            for hf in range(2):
                d_ = dn[dncount % 2]
                dtok = ("dn", dncount % 2)
                dncount += 1
                for kc in range(8):
                    p.mm(d_[:], yTs[:, kc, ti * 128:(ti + 1) * 128], wo[:, kc, hf * 512:(hf + 1) * 512],
                         kc == 0, kc == 7, [ytok, "const_wo"], [dtok])
                p.tt("dve", a[:, hf * 512:(hf + 1) * 512], d_[:], a[:, hf * 512:(hf + 1) * 512], ALU.add,
                     [dtok, atok], [atok])
            s_ = st[ti % NSL]
            stok = ("st", ti % NSL)
            h_ = hn[ti % NSL]
            htok = ("hn", ti % NSL)
            p.memset("dve", s_[:, 0:1], 0.0, [stok])
            p.act(junk[:], a[:], AF.Square, [atok, stok], [stok], accum_out=s_[:, 0:1])
            p.ts("dve", s_[:, 1:2], s_[:, 0:1], 1.0 / D, EPS, ALU.mult, ALU.add, [stok], [stok])
            p.tt("pool", s_[:, 3:4], s_[:, 1:2], mhalf[:], ALU.pow, [stok, "c_mhalf"], [stok])
            p.stt("dve", h_[:], a[:], s_[:, 3:4], gbc[:], ALU.mult, ALU.mult, [atok, stok, "const_g"], [htok])
            for j in range(8):
                p.tr(tp[:, j, :], h_[:, j * 128:(j + 1) * 128], ident[:], [htok, "const_id"], ["tp"])
            if not moe:
                p.copy("act", hT[:, :, ti * 128:(ti + 1) * 128], tp[:], ["tp"], [("hT", ti)])
            else:
                h32 = hT32[ti % NSL]
                h32tok = ("hT32", ti % NSL)
                p.copy("act", h32[:], tp[:], ["tp"], [h32tok])
                p.copy("dve", hT[:, :, ti * 128:(ti + 1) * 128], h32[:], [h32tok], [("hT", ti)])
            if moe and "router" in DBG_SKIP:
                p.memset("dve", gates[ti][:], 0.125, [("gates", ti)])
            elif moe:
                r_ = rt[ti % NSL]
                rtok = ("rt", ti % NSL)
                gtok = ("gates", ti)
                lg = gA[0][:, 0:8]
                for kc in range(8):
                    p.mm(lg, h32[:, kc, :], wr[:, kc, :], kc == 0, kc == 7, [h32tok, "const_wr"], [("gA", 0)])
                p.copy("dve", r_[:, 0:8], lg, [("gA", 0)], [rtok])
                p.add("dve", lambda e, r_=r_: e.max(out=r_[:, 8:16], in_=r_[:, 0:8]), [rtok], [rtok])
                p.ts("dve", r_[:, 16:17], r_[:, 8:9], -1.0, None, ALU.mult, None, [rtok], [rtok])
                p.act(r_[:, 24:32], r_[:, 0:8], AF.Exp, [rtok], [rtok], bias=r_[:, 16:17])
                p.ts("dve", r_[:, 32:40], r_[:, 0:8], r_[:, 9:10], None, ALU.is_ge, None, [rtok], [rtok])
                p.tt("dve", r_[:, 24:32], r_[:, 24:32], r_[:, 32:40], ALU.mult, [rtok], [rtok])
                p.add("dve", lambda e, r_=r_: e.reduce_sum(out=r_[:, 17:18], in_=r_[:, 24:32], axis=AX.X),
                      [rtok], [rtok])
                p.add("dve", lambda e, r_=r_: e.reciprocal(out=r_[:, 18:19], in_=r_[:, 17:18]), [rtok], [rtok])
                p.ts("dve", gates[ti][:], r_[:, 24:32], r_[:, 18:19], None, ALU.mult, None, [rtok], [gtok])
            pgroups.append(p.end_group())
        p.interleave(pgroups, 3)
        hT_all = [("hT", ti) for ti in range(TPP)]
        if DEBUG and ps_i == 0:
            dbg = nc.dram_tensor("dbg_hT", [128, 8, PASS], BF16, kind="ExternalOutput").ap()
            p.dma("sp", dbg, hT[:], reads=hT_all, writes=["dbg_hT"])
            dbg3 = nc.dram_tensor("dbg_hn", [256, D], F32, kind="ExternalOutput").ap()
            for i in range(2):
                p.dma("sp", dbg3[i * 128:(i + 1) * 128, :], hn[i][:], reads=[("hn", i)], writes=[("dbg_hn", i)])
            dbg2 = nc.dram_tensor("dbg_x1", [PASS, D], F32, kind="ExternalOutput").ap()
            for ti in range(TPP):
                p.dma("sp", dbg2[ti * 128:(ti + 1) * 128, :], acc[ti][:], reads=[("acc", ti)], writes=[("dbg_x1", ti)])
        for e_i in range(E):
            for part in parts:
                nch = len(part) * UC
                par = pcount % 2
                pcount += 1
                for ui, u in enumerate(part):
                    ws = ucount % NWS
                    ucount += 1
                    c0 = u * UC * 128
                    wsrc = w_gu[e_i].rearrange("(kc p) f -> p kc f", p=128)
                    p.dma("pool", wg[ws][:], wsrc[:, :, c0:c0 + UC * 128], writes=[("wg", ws)])
                    p.dma("pool", wu[ws][:], wsrc[:, :, DFF + c0:DFF + c0 + UC * 128], writes=[("wu", ws)])
                    p.dma("pool", wd[par][ui][:],
                          w_d[e_i].rearrange("(fc p) c -> p fc c", p=128)[:, u * UC:(u + 1) * UC, :],
                          writes=[("wd", par, ui)])
                    for fl in range(UC):
                        lc = ui * UC + fl
                        for tt_ in range(PASS // 512):
                            gi = gucount % 2
                            gucount += 1
                            ga, ub = gA[gi], uB[gi]
                            gtk, utk = ("gA", gi), ("uB", gi)
                            rhs_tok = hT_all[tt_ * 4:(tt_ + 1) * 4]
                            for kc in range(8):
                                p.mm(ga[:], wg[ws][:, kc, fl * 128:(fl + 1) * 128], hT[:, kc, tt_ * 512:(tt_ + 1) * 512],
                                     kc == 0, kc == 7, [("wg", ws)] + rhs_tok, [gtk])
                            for kc in range(8):
                                p.mm(ub[:], wu[ws][:, kc, fl * 128:(fl + 1) * 128], hT[:, kc, tt_ * 512:(tt_ + 1) * 512],
                                     kc == 0, kc == 7, [("wu", ws)] + rhs_tok, [utk])
                            s_ = sg[gi]
                            p.act(s_[:], ga[:], AF.Silu, [gtk], [("sg", gi)])
                            p.tt("dve", actT[:, lc, tt_ * 512:(tt_ + 1) * 512], ub[:], s_[:], ALU.mult,
                                 [utk, ("sg", gi)], [("actT", lc, tt_)])
                for ti in range(TPP):
                    a = acc[ti]
                    atok = ("acc", ti)
                    for hf in range(2):
                        d_ = dn[dncount % 2]
                        dtok = ("dn", dncount % 2)
                        dncount += 1
                        for lc in range(nch):
                            p.mm(d_[:], actT[:, lc, ti * 128:(ti + 1) * 128],
                                 wd[par][lc // UC][:, lc % UC, hf * 512:(hf + 1) * 512],
                                 lc == 0, lc == nch - 1, [("actT", lc, ti // 4), ("wd", par, lc // UC)], [dtok])
                        sc = gates[ti][:, e_i:e_i + 1] if moe else 1.0
                        rd = [dtok, atok] + ([("gates", ti)] if moe else [])
                        p.stt("dve", a[:, hf * 512:(hf + 1) * 512], d_[:], sc, a[:, hf * 512:(hf + 1) * 512],
                              ALU.mult, ALU.add, rd, [atok])
        for ti in range(TPP):
            tok0 = t0p + ti * 128
            a = acc[ti]
            atok = ("acc", ti)
            if moe and "final" not in DBG_SKIP:
                s_ = st[ti % NSL]
                stok = ("st", ti % NSL)
                o_ = hn[ti % NSL]
                otok = ("hn", ti % NSL)
                p.memset("dve", s_[:, 0:1], 0.0, [stok])
                p.act(junk[:], a[:], AF.Square, [atok, stok], [stok], accum_out=s_[:, 0:1])
                p.ts("dve", s_[:, 1:2], s_[:, 0:1], 1.0 / D, EPS, ALU.mult, ALU.add, [stok], [stok])
                p.tt("pool", s_[:, 3:4], s_[:, 1:2], mhalf[:], ALU.pow, [stok, "c_mhalf"], [stok])
                p.stt("dve", o_[:], a[:], s_[:, 3:4], gfin[:], ALU.mult, ALU.mult, [atok, stok, "const_gf"], [otok])
                p.dma("sp", xout[tok0:tok0 + 128, :], o_[:], reads=[otok], writes=[("xout", ps_i, ti)],
                      key=("hn", ti % NSL))
            else:
                p.dma("sp", xout[tok0:tok0 + 128, :], a[:], reads=[atok], writes=[("xout", ps_i, ti)],
                      key=("acc", ti))
                if hoist:
                    s_ = st[ti % NSL]
                    stok = ("st", ti % NSL)
                    hb = hnb[ti % 2]
                    hbtok = ("hnb", ti % 2)
                    p.memset("dve", s_[:, 0:1], 0.0, [stok])
                    p.act(junk[:], a[:], AF.Square, [atok, stok], [stok], accum_out=s_[:, 0:1])
                    p.ts("dve", s_[:, 1:2], s_[:, 0:1], 1.0 / D, EPS, ALU.mult, ALU.add, [stok], [stok])
                    p.tt("pool", s_[:, 3:4], s_[:, 1:2], mhalf[:], ALU.pow, [stok, "c_mhalf"], [stok])
                    p.stt("dve", hb[:], a[:], s_[:, 3:4], gnx[:], ALU.mult, ALU.mult, [atok, stok, "const_gnx"], [hbtok])
                    p.dma("sp", hnout[tok0:tok0 + 128, :], hb[:], reads=[hbtok], writes=[("hnout", ps_i, ti)],
                          key=("hnb", ti % 2))
                    if ti % 4 == 3:
                        fused["gather_piece"](p, "x2", ps_i * 2 + ti // 4,
                                              [("hnout", ps_i, t_) for t_ in range(ti - 3, ti + 1)])
    p.add("sp", lambda e: e.nop(), reads=[("xout", a_, b_) for a_ in range(NPASS) for b_ in range(TPP)])
    if fused is not None:
        p.finish(barrier=True)
        return None
    p.emit()
    p.close()
    return nc


def build_even_kernel(nchunks=SEQ // 128, fused=None):
    S = nchunks * 128
    NT1 = 386
    if fused is None:
        nc = bass.Bass("TRN2", target_bir_lowering=False)
        pfx = ""
        _dt = lambda name, shape, dt, kind: nc.dram_tensor(name, shape, dt, kind=kind)
    else:
        nc, pfx = fused["nc"], "ev_"
        _dt = lambda name, shape, dt, kind: fused["tensor"](pfx + name, shape, dt, kind)
    class _W:
        def dram_tensor(self, name, shape, dt, kind="Internal"):
            return _dt(name, shape, dt, kind)
    ncd = _W()
    x = ncd.dram_tensor("x", [S, D], F32, kind="ExternalInput").ap()
    g_norm = ncd.dram_tensor("g_norm", [D], F32, kind="ExternalInput").ap()
    wt_d = ncd.dram_tensor("wt", [D, 898], F32, kind="ExternalInput").ap()
    wf_d = ncd.dram_tensor("wf", [D, 256], F32, kind="ExternalInput").ap()
    cw_d = ncd.dram_tensor("cw", [128, 8], F32, kind="ExternalInput").ap()
    cb_d = ncd.dram_tensor("cb", [128, 2], F32, kind="ExternalInput").ap()
    gb_d = ncd.dram_tensor("gb", [2], F32, kind="ExternalInput").ap()
    mlg_d = ncd.dram_tensor("mlg", [128], F32, kind="ExternalInput").ap()
    sgg_d = ncd.dram_tensor("sgg", [128], F32, kind="ExternalInput").ap()
    sgwT_d = ncd.dram_tensor("sgwT", [128, 128], F32, kind="ExternalInput").ap()
    sgb_d = ncd.dram_tensor("sgb", [128, 1], F32, kind="ExternalInput").ap()
    ident_d = ncd.dram_tensor("ident", [128, 128], F32, kind="ExternalInput").ap()
    triu_d = ncd.dram_tensor("triu", [128, 128], F32, kind="ExternalInput").ap()
    negm_d = ncd.dram_tensor("negmask", [128, 128], F32, kind="ExternalInput").ap()
    y = ncd.dram_tensor("y", [S, 256], BF16, kind="ExternalOutput").ap()

    p = Prog(nc, pfx, fused['semstack'] if fused else None)
    ident = p.sbuf("ident_sb", [128, 128], F32)
    triu = p.sbuf("triu_sb", [128, 128], F32)
    negm = p.sbuf("negm_sb", [128, 128], F32)
    ones = p.sbuf("ones_sb", [128, 128], F32)
    mhalf = p.sbuf("mhalf", [128, 1], F32)
    gbc = p.sbuf("gbc", [128, D], F32)
    wt = p.sbuf("wt_sb", [128, 8, 898], BF16)
    wf = p.sbuf("wf_sb", [128, 8, 256], BF16)
    cw = p.sbuf("cw_sb", [128, 8], F32)
    cb = p.sbuf("cb_sb", [128, 2], F32)
    gb = p.sbuf("gb_sb", [128, 4], F32)
    mlg = p.sbuf("mlg_sb", [128, 128], F32)
    sgg = p.sbuf("sgg_sb", [128, 128], F32)
    sgw32 = p.sbuf("sgw32", [128, 128], F32)
    wTm = p.sbuf("wTm", [128, 128], BF16)
    sgb = p.sbuf("sgb_sb", [128, 1], F32)
    Cst = p.sbuf("Cst", [128, 129], F32)
    Cbf = p.sbuf("Cbf", [128, 129], BF16)
    mst = p.sbuf("mst", [128, 1], F32)
    cbuf = p.sbuf("cbuf", [128, 2, 131], F32)
    junk = p.sbuf("junk", [128, D], BF16)
    NS = 6
    NXS = 6
    xs = [p.sbuf("xs%d" % i, [128, D], F32) for i in range(NXS)]
    hn = [p.sbuf("hn%d" % i, [128, D], BF16) for i in range(NS)]
    identb = p.sbuf("identb", [128, 128], BF16)
    hT = [p.sbuf("hT%d" % i, [128, 8, 128], BF16) for i in range(NS)]
    st = [p.sbuf("st%d" % i, [128, 16], F32) for i in range(NS)]
    g = [p.sbuf("g%d" % i, [128, 32], F32) for i in range(NS)]
    vext = [p.sbuf("vext%d" % i, [128, 129], BF16) for i in range(NS)]
    so = [p.sbuf("so%d" % i, [128, 128], F32) for i in range(NS)]
    gu = [p.sbuf("gu%d" % i, [128, 128], F32) for i in range(NS)]
    gv = [p.sbuf("gv%d" % i, [128, 512], F32) for i in range(NS)]
    xh = [p.sbuf("xh%d" % i, [128, 640], F32) for i in range(NS)]
    xp = [p.sbuf("xp%d" % i, [128, 640], F32) for i in range(NS)]
    vsn = [p.sbuf("vsn%d" % i, [128, 128], BF16) for i in range(NS)]
    cc = [p.sbuf("cc%d" % i, [128, 2, 128], F32) for i in range(NS)]
    qk32 = [p.sbuf("qk32_%d" % i, [128, 2, 128], F32) for i in range(NS)]
    qT = [p.sbuf("qT%d" % i, [128, 128], BF16) for i in range(NS)]
    kT = [p.sbuf("kT%d" % i, [128, 128], BF16) for i in range(NS)]
    dg = [p.sbuf("dg%d" % i, [128, 128], F32) for i in range(NS)]
    dg2 = [p.sbuf("dg2_%d" % i, [128, 128], F32) for i in range(NS)]
    tA = [p.sbuf("tA%d" % i, [128, 128], F32) for i in range(NS)]
    tE = [p.sbuf("tE%d" % i, [128, 128], F32) for i in range(NS)]
    Em = [p.sbuf("Em%d" % i, [128, 128], F32) for i in range(NS)]
    sTw = [p.sbuf("sTw%d" % i, [128, 128], BF16) for i in range(NS)]
    intra = [p.sbuf("intra%d" % i, [128, 129], F32) for i in range(NS)]
    numx = [p.sbuf("numx%d" % i, [128, 129], F32) for i in range(NS)]
    gs = [p.sbuf("gs%d" % i, [128, 128], F32) for i in range(NS)]
    kw = [p.sbuf("kw%d" % i, [128, 128], BF16) for i in range(NS)]
    ktok = [p.sbuf("ktok%d" % i, [128, 128], F32) for i in range(NS)]
    ych = [p.sbuf("ych%d" % i, [128, 256], BF16) for i in range(NS)]
    tp = p.psum("tp", [128, 8, 128], F32)
    bA = p.psum("bA", [128, 512], F32)
    bB = p.psum("bB", [128, 512], F32)
    bQ = p.psum("bQ", [128, 512], F32)
    bS = p.psum("bS", [128, 512], F32)
    bT = p.psum("bT", [128, 512], F32)
    bO = p.psum("bO", [128, 512], F32)

    p.dma("sp", ident[:], ident_d, writes=["c_ident"])
    p.dma("sp", triu[:], triu_d, writes=["c_triu"])
    p.dma("sp", negm[:], negm_d, writes=["c_negm"])
    p.dma("sp", gbc[:], g_norm.partition_broadcast(128), writes=["c_gbc"])
    p.dma("pool", wt[:], wt_d.rearrange("(kc p) c -> p kc c", p=128), writes=["c_wt"])
    p.dma("pool", wf[:], wf_d.rearrange("(kc p) c -> p kc c", p=128), writes=["c_wf"])
    p.dma("sp", cw[:], cw_d, writes=["c_cw"])
    p.dma("sp", cb[:], cb_d, writes=["c_cb"])
    p.dma("sp", gb[:, 0:2], gb_d.partition_broadcast(128), writes=["c_gb"])
    p.dma("sp", mlg[:], mlg_d.partition_broadcast(128), writes=["c_mlg"])
    p.dma("sp", sgg[:], sgg_d.partition_broadcast(128), writes=["c_sgg"])
    p.dma("sp", sgw32[:], sgwT_d, writes=["c_sgw32"])
    p.dma("sp", sgb[:], sgb_d, writes=["c_sgb"])
    p.memset("dve", ones[:], 1.0, ["c_ones"])
    p.copy("dve", identb[:], ident[:], ["c_ident"], ["c_identb"])
    p.memset("dve", mhalf[:], -0.5, ["c_mhalf"])
    p.ts("dve", mlg[:], mlg[:], 0.5, None, ALU.mult, None, ["c_mlg"], ["c_mlg"])
    p.ts("dve", cw[:], cw[:], 0.5, None, ALU.mult, None, ["c_cw"], ["c_cw"])
    p.ts("dve", cb[:], cb[:], 0.5, None, ALU.mult, None, ["c_cb"], ["c_cb"])
    p.ts("dve", gb[:, 2:3], gb[:, 1:2], -1.0, None, ALU.mult, None, ["c_gb"], ["c_nbf"])
    p.tt("dve", wTm[:], sgw32[:], triu[:], ALU.mult, ["c_sgw32", "c_triu"], ["c_wTm"])
    p.memset("dve", Cst[:], 0.0, ["Cst"])
    p.memset("dve", Cbf[:], 0.0, ["Cbf"])
    p.memset("dve", mst[:], 0.0, ["mst"])
    p.memset("dve", cbuf[:], 0.0, ["cbuf"])
    for i in range(NS):
        p.memset("dve", vext[i][:, 128:129], 1.0, [("vext1", i)])

    groups = []
    for n in range(nchunks):
        s = n % NS
        x3 = n % NXS
        p.begin_group()
        K_ = lambda name, s=s: (name, s)
        p.dma("sp", xs[x3][:], x[n * 128:(n + 1) * 128, :], writes=[("xs", x3)])
        st_, g_ = st[s], g[s]
        p.memset("dve", st_[:, 0:1], 0.0, [K_("st")])
        p.act(junk[:], xs[x3][:], AF.Square, [("xs", x3), K_("st")], [K_("st")], accum_out=st_[:, 0:1])
        p.ts("dve", st_[:, 1:2], st_[:, 0:1], 1.0 / D, EPS, ALU.mult, ALU.add, [K_("st")], [K_("st")])
        p.tt("pool", st_[:, 3:4], st_[:, 1:2], mhalf[:], ALU.pow, ["c_mhalf"], [K_("st")])
        p.stt("dve", hn[s][:], xs[x3][:], st_[:, 3:4], gbc[:], ALU.mult, ALU.mult,
              [("xs", x3), K_("st"), "c_gbc"], [K_("hn")])
        tpb = tp[:].rearrange("p k t -> p (k t)").bitcast(BF16)[:, 0:1024].rearrange("p (k t) -> p k t", k=8)
        for j in range(8):
            p.tr(tpb[:, j, :], hn[s][:, j * 128:(j + 1) * 128], identb[:], [K_("hn"), "c_identb"], ["tp"])
        p.copy("act", hT[s][:], tpb, [], ["tp", K_("hT")])
        for kc in range(8):
            p.mm(bA[:, 0:NT1], hT[s][:, kc, :], wt[:, kc, 0:NT1], kc == 0, kc == 7, [K_("hT"), "c_wt"], ["bA"])
        for kc in range(8):
            p.mm(bB[:, :], hT[s][:, kc, :], wt[:, kc, NT1:898], kc == 0, kc == 7, [K_("hT"), "c_wt"], ["bB"])
        for jq in range(2):
            for kc in range(8):
                p.mm(bQ[:, jq * 128:(jq + 1) * 128], wf[:, kc, jq * 128:(jq + 1) * 128], hT[s][:, kc, :],
                     kc == 0, kc == 7, [K_("hT"), "c_wf"], ["bQ"])
        p.copy("act", vext[s][:, 0:128], bA[:, 0:128], [("vext1", s)], ["bA", K_("vext")])
        p.act(so[s][:], bA[:, 128:256], AF.Tanh, [], ["bA", K_("so")], scale=0.5)
        p.act(xh[s][:, 0:128], bA[:, 256:384], AF.Copy, [], ["bA", K_("xh")], scale=0.5)
        p.ts("dve", g_[:, 0:1], bA[:, 384:385], gb[:, 0:1], None, ALU.add, None, ["c_gb"], ["bA", K_("g")])
        p.act(g_[:, 1:2], bA[:, 385:386], AF.Exp, ["c_nbf"], ["bA", K_("g")], bias=gb[:, 2:3], scale=-1.0)
        p.act(g_[:, 2:3], g_[:, 1:2], AF.Ln, [], [K_("g")], bias=1.0)
        p.act(xh[s][:, 128:640], bB[:, :], AF.Copy, [], ["bB", K_("xh")], scale=0.5)
        p.tt("pool", xp[s][:], xh[s][:], xh[s][:], ALU.mult, [K_("xh")], [K_("xp")])
        p.ts("pool", xp[s][:], xp[s][:], 4 * 0.044715, 1.0, ALU.mult, ALU.add, [], [K_("xp")])
        p.tt("pool", xp[s][:], xp[s][:], xh[s][:], ALU.mult, [K_("xh")], [K_("xp")])
        p.act(xp[s][:], xp[s][:], AF.Tanh, [], [K_("xp")], scale=2 * 0.7978845608028654)
        p.stt("dve", gu[s][:], xp[s][:, 0:128], 1.0, xh[s][:, 0:128], ALU.add, ALU.mult, [K_("xp"), K_("xh")], [K_("gu")])
        p.stt("dve", gv[s][:], xp[s][:, 128:640], 1.0, xh[s][:, 128:640], ALU.add, ALU.mult, [K_("xp"), K_("xh")], [K_("gv")])
        p.memset("dve", st_[:, 4:5], 0.0, [K_("st")])
        p.act(junk[:, 0:512], gv[s][:], AF.Square, [K_("gv")], [K_("st")], accum_out=st_[:, 4:5])
        p.ts("dve", st_[:, 5:6], st_[:, 4:5], 1.0 / 512, EPS, ALU.mult, ALU.add, [], [K_("st")])
        p.tt("pool", st_[:, 7:8], st_[:, 5:6], mhalf[:], ALU.pow, ["c_mhalf"], [K_("st")])
        p.stt("dve", vsn[s][:], gv[s][:, 0:128], st_[:, 7:8], sgg[:],
              ALU.mult, ALU.mult, [K_("gv"), K_("st"), "c_sgg"], [K_("vsn")])
        p.mm(bT[:, 128:256], wTm[:], vsn[s][:], True, True, ["c_wTm", K_("vsn")], ["bT"])
        p.stt("dve", ych[s][:, 128:256], bT[:, 128:256], sgb[:, 0:1], gu[s][:], ALU.add, ALU.mult,
              ["c_sgb", K_("gu")], ["bT", K_("ych")])
        p.copy("act", cbuf[:, :, 3:131], bQ[:, 0:256].rearrange("p (j t) -> p j t", j=2), [], ["bQ", "cbuf"])
        for j in range(2):
            p.ts("dve", cc[s][:, j, :], cbuf[:, j, 3:131], cw[:, j * 4 + 3:j * 4 + 4], cb[:, j:j + 1], ALU.mult, ALU.add,
                 ["cbuf", "c_cw", "c_cb"], [K_("cc")])
            for tap in (2, 1, 0):
                p.stt("dve", cc[s][:, j, :], cbuf[:, j, tap:tap + 128], cw[:, j * 4 + tap:j * 4 + tap + 1],
                      cc[s][:, j, :], ALU.mult, ALU.add, ["cbuf", "c_cw"], [K_("cc")])
        p.copy("dve", cbuf[:, :, 0:3], cbuf[:, :, 128:131], [], ["cbuf"])
        p.act(qk32[s][:], cc[s][:], AF.Tanh, [K_("cc")], [K_("qk32")])
        p.stt("dve", qk32[s][:], qk32[s][:], 1.0, cc[s][:], ALU.add, ALU.mult, [K_("cc")], [K_("qk32")])
        p.ts("dve", qT[s][:], qk32[s][:, 0, :], 128.0 ** -0.5, None, ALU.mult, None, [K_("qk32")], [K_("qT")])
        p.copy("dve", kT[s][:], qk32[s][:, 1, :], [K_("qk32")], [K_("kT")])
        p.tr(bT[:, 256:384], qk32[s][:, 1, :], ident[:], [K_("qk32"), "c_ident"], ["bT"])
        p.copy("act", ktok[s][:], bT[:, 256:384], [], ["bT", K_("ktok")])
        p.mm(bS[:, 0:1], triu[:], g_[:, 2:3], True, True, ["c_triu", K_("g")], ["bS"])
        p.mm(bS[:, 1:2], ones[:], g_[:, 2:3], True, True, ["c_ones", K_("g")], ["bS"])
        p.copy("dve", g_[:, 3:5], bS[:, 0:2], [], ["bS", K_("g")])
        p.tt("dve", g_[:, 5:6], g_[:, 0:1], g_[:, 3:4], ALU.add, [], [K_("g")])
        p.ts("dve", dg[s][:], ident[:], g_[:, 5:6], None, ALU.mult, None, ["c_ident", K_("g")], [K_("dg")])
        p.mm(bS[:, 128:256], ones[:], dg[s][:], True, True, ["c_ones", K_("dg")], ["bS"])
        p.tt("dve", tA[s][:], bS[:, 128:256], negm[:], ALU.add, ["c_negm"], ["bS", K_("tA")])
        p.add("dve", lambda e, g_=g_, s=s: e.reduce_max(out=g_[:, 6:7], in_=tA[s][:], axis=AX.X), [K_("tA")], [K_("g")])
        p.add("dve", lambda e, g_=g_: e.reduce_max(out=g_[:, 7:8], in_=bS[:, 128:256], axis=AX.X), [], ["bS", K_("g")])
        p.tt("dve", g_[:, 8:9], g_[:, 6:7], mst[:], ALU.max, ["mst"], [K_("g")])
        p.tt("dve", g_[:, 9:10], g_[:, 7:8], mst[:], ALU.max, ["mst"], [K_("g")])
        p.tt("dve", g_[:, 10:11], mst[:], g_[:, 8:9], ALU.subtract, ["mst"], [K_("g")])
        p.tt("dve", g_[:, 11:12], mst[:], g_[:, 9:10], ALU.subtract, ["mst"], [K_("g")])
        p.tt("dve", g_[:, 12:13], g_[:, 5:6], g_[:, 9:10], ALU.subtract, [], [K_("g")])
        p.tt("dve", g_[:, 13:14], g_[:, 3:4], g_[:, 8:9], ALU.subtract, [], [K_("g")])
        p.act(g_[:, 14:18], g_[:, 10:14], AF.Exp, [], [K_("g")])
        p.ts("dve", g_[:, 18:19], g_[:, 8:9], -1.0, None, ALU.mult, None, [], [K_("g")])
        p.ts("dve", dg2[s][:], ident[:], g_[:, 18:19], None, ALU.mult, None, ["c_ident", K_("g")], [K_("dg2")])
        p.mm(bS[:, 256:384], ones[:], dg2[s][:], True, True, ["c_ones", K_("dg2")], ["bS"])
        p.ts("dve", tE[s][:], bS[:, 256:384], g_[:, 5:6], 0.0, ALU.add, ALU.min, [K_("g")], ["bS", K_("tE")])
        p.act(Em[s][:], tE[s][:], AF.Exp, [K_("tE")], [K_("Em")])
        p.tt("pool", Em[s][:], Em[s][:], triu[:], ALU.mult, ["c_triu"], [K_("Em")])
        p.mm(bT[:, 0:128], kT[s][:], qT[s][:], True, True, [K_("kT"), K_("qT")], ["bT"])
        p.tt("dve", sTw[s][:], bT[:, 0:128], Em[s][:], ALU.mult, [K_("Em")], ["bT", K_("sTw")])
        p.mm(bO[:, 0:129], sTw[s][:], vext[s][:], True, True, [K_("sTw"), K_("vext")], ["bO"])
        p.mm(bO[:, 129:258], qT[s][:], Cbf[:], True, True, [K_("qT"), "Cbf"], ["bO"])
        p.copy("act", intra[s][:], bO[:, 0:129], [], ["bO", K_("intra")])
        p.stt("dve", numx[s][:], bO[:, 129:258], g_[:, 14:15], intra[s][:], ALU.mult, ALU.add,
              [K_("g"), K_("intra")], ["bO", K_("numx")])
        p.ts("dve", g_[:, 27:28], numx[s][:, 128:129], -1.0, None, ALU.mult, None, [K_("numx")], [K_("g")])
        p.tt("dve", g_[:, 19:20], g_[:, 27:28], numx[s][:, 128:129], ALU.max, [K_("numx")], [K_("g")])
        p.tt("dve", g_[:, 19:20], g_[:, 19:20], g_[:, 17:18], ALU.max, [], [K_("g")])
        p.add("dve", lambda e, g_=g_: e.reciprocal(out=g_[:, 20:21], in_=g_[:, 19:20]), [], [K_("g")])
        p.memset("dve", st_[:, 8:9], 0.0, [K_("st")])
        p.act(junk[:, 0:128], numx[s][:, 0:128], AF.Square, [K_("numx")], [K_("st")], accum_out=st_[:, 8:9])
        p.tt("dve", g_[:, 21:22], g_[:, 20:21], g_[:, 20:21], ALU.mult, [], [K_("g")])
        p.tt("dve", g_[:, 22:23], g_[:, 21:22], st_[:, 8:9], ALU.mult, [K_("st")], [K_("g")])
        p.ts("dve", g_[:, 23:24], g_[:, 22:23], 1.0 / 128, EPS, ALU.mult, ALU.add, [], [K_("g")])
        p.tt("pool", g_[:, 25:26], g_[:, 23:24], mhalf[:], ALU.pow, ["c_mhalf"], [K_("g")])
        p.tt("dve", g_[:, 26:27], g_[:, 25:26], g_[:, 20:21], ALU.mult, [], [K_("g")])
        p.stt("dve", gs[s][:], so[s][:], 1.0, mlg[:], ALU.add, ALU.mult, [K_("so"), "c_mlg"], [K_("gs")])
        p.stt("dve", ych[s][:, 0:128], numx[s][:, 0:128], g_[:, 26:27], gs[s][:], ALU.mult, ALU.mult,
              [K_("numx"), K_("g"), K_("gs")], [K_("ych")])
        p.ts("dve", kw[s][:], ktok[s][:], g_[:, 16:17], None, ALU.mult, None, [K_("g"), K_("ktok")], [K_("kw")])
        p.mm(bO[:, 258:387], kw[s][:], vext[s][:], True, True, [K_("kw"), K_("vext")], ["bO"])
        p.stt("dve", Cst[:], Cst[:], g_[:, 15:16], bO[:, 258:387], ALU.mult, ALU.add, [K_("g")], ["bO", "Cst"])
        p.copy("act", Cbf[:], Cst[:], ["Cst"], ["Cbf"])
        p.tt("dve", mst[:], g_[:, 9:10], g_[:, 4:5], ALU.subtract, [K_("g")], ["mst"])
        p.dma("sp", y[n * 128:(n + 1) * 128, :], ych[s][:], reads=[K_("ych")], writes=[("y", n)], key=("ych", s))
        if fused is not None and (n + 1) % 16 == 0:
            fused["gather_piece"](p, "y1", n // 16, [("y", m) for m in range(n - 15, n + 1)])
        groups.append(p.end_group())
    p.interleave(groups, PIPE_DEPTH)
    p.add("sp", lambda e: e.nop(), reads=[("y", n) for n in range(nchunks)])
    if fused is not None:
        p.finish(barrier=True)
        return None
    p.emit()
    p.close()
    return nc


def _consts():
    i = np.arange(128)
    triu = (i[:, None] <= i[None, :]).astype(np.float32)
    negm = np.where(i[None, :] <= i[:, None], 0.0, -1e30).astype(np.float32)
    return np.eye(128, dtype=np.float32), triu, negm


def even_core_inputs(inp, b, h, S=SEQ):
    ident, triu, negm = _consts()
    w = inp["even_w_in"][0]
    c = lambda a0: np.arange(a0 + h * 128, a0 + (h + 1) * 128)
    vs_order = np.concatenate([np.arange(2568 + g * 128, 2568 + (g + 1) * 128) for g in [h] + [g for g in range(4) if g != h]])
    tcols = np.concatenate([c(1024), c(1536), c(2056), np.array([2048 + h, 2052 + h]), vs_order])
    fcols = np.concatenate([c(0), c(512)])
    cwf = inp["even_ml_conv_w"][0]
    cw = np.stack([cwf[:, h * 128:(h + 1) * 128].T, cwf[:, 512 + h * 128:512 + (h + 1) * 128].T], axis=1)
    cbf = inp["even_ml_conv_b"][0]
    cb = np.stack([cbf[h * 128:(h + 1) * 128], cbf[512 + h * 128:512 + (h + 1) * 128]], axis=1)
    return {
        "x": np.ascontiguousarray(inp["x"][b, :S]),
        "g_norm": inp["even_norm_mix"][0],
        "wt": np.ascontiguousarray(w[:, tcols]),
        "wf": np.ascontiguousarray(w[:, fcols]),
        "cw": np.ascontiguousarray(cw.reshape(128, 8)),
        "cb": np.ascontiguousarray(cb),
        "gb": np.ascontiguousarray(inp["even_ml_gate_b"][0][:, h]),
        "mlg": np.ascontiguousarray(inp["even_ml_norm_g"][0][h * 128:(h + 1) * 128]),
        "sgg": np.ascontiguousarray(inp["even_sg_norm_g"][0][h * 128:(h + 1) * 128]),
        "sgwT": np.ascontiguousarray(inp["even_sg_w"][0][h].T),
        "sgb": np.ascontiguousarray(inp["even_sg_b"][0][h].reshape(128, 1)),
        "ident": ident, "triu": triu, "negmask": negm,
    }


LAMBDA_INIT = 0.8 - 0.6 * math.exp(-0.3 * 1)
FOX_SCALE = 128.0 ** -0.5
DIFF_SCALE = 64.0 ** -0.5


def build_odd_kernel(nchunks=SEQ // 128, fused=None):
    S = nchunks * 128
    NQB = S // 512
    if fused is None:
        nc = bass.Bass("TRN2", target_bir_lowering=False)
        pfx = ""
        _dt = lambda name, shape, dt, kind: nc.dram_tensor(name, shape, dt, kind=kind)
    else:
        nc, pfx = fused["nc"], "od_"
        _dt = lambda name, shape, dt, kind: fused["tensor"](pfx + name, shape, dt, kind)
    class _W:
        def dram_tensor(self, name, shape, dt, kind="Internal"):
            return _dt(name, shape, dt, kind)
    ncd = _W()
    x = ncd.dram_tensor("x", [S, D], F32 if fused is None else BF16, kind="ExternalInput").ap()
    g_norm = ncd.dram_tensor("g_norm", [D], F32, kind="ExternalInput").ap()
    wf_d = ncd.dram_tensor("wf", [D, 577], F32, kind="ExternalInput").ap()
    wt_d = ncd.dram_tensor("wt", [D, 256], F32, kind="ExternalInput").ap()
    fb_d = ncd.dram_tensor("fb", [1, 1], F32, kind="ExternalInput").ap()
    lam_d = ncd.dram_tensor("lam", [256], F32, kind="ExternalInput").ap()
    dng_d = ncd.dram_tensor("dng", [128], F32, kind="ExternalInput").ap()
    rope_d = ncd.dram_tensor("rope", [nchunks, 128, 2, 256], F32, kind="ExternalInput").ap()
    pt_d = ncd.dram_tensor("ptm", [128, 128], F32, kind="ExternalInput").ap()
    sel_d = ncd.dram_tensor("sel", [128, 3, 65], F32, kind="ExternalInput").ap()
    csc_d = ncd.dram_tensor("cscale", [1, 3], F32, kind="ExternalInput").ap()
    ident_d = ncd.dram_tensor("ident", [128, 128], F32, kind="ExternalInput").ap()
    triu_d = ncd.dram_tensor("triu", [128, 128], F32, kind="ExternalInput").ap()
    y = ncd.dram_tensor("y", [S, 256], BF16, kind="ExternalOutput").ap()

    p = Prog(nc, pfx, fused['semstack'] if fused else None)
    ident = p.sbuf("ident_sb", [128, 128], F32)
    triu32 = p.sbuf("triu32", [128, 128], F32)
    triub = p.sbuf("triub", [128, 128], BF16)
    ptm = p.sbuf("ptm_sb", [128, 128], F32)
    sel32 = p.sbuf("sel32", [128, 3, 65], F32)
    selb = p.sbuf("selb", [128, 3, 65], BF16)
    gbc = p.sbuf("gbc", [128, D], F32)
    wf = p.sbuf("wf_sb", [128, 8, 577], BF16)
    wtk = p.sbuf("wtk_sb", [128, 8, 256], BF16)
    lamt = p.sbuf("lamt", [128, 256], F32)
    lamw = p.sbuf("lamw", [128, 256], F32)
    lams = p.sbuf("lams", [128, 8], F32)
    dng = p.sbuf("dng_sb", [128, 128], F32)
    rowc = p.sbuf("rowc", [65, 16], F32)
    rmax = p.sbuf("rmax", [65, 8], F32)
    rtmp = p.sbuf("rtmp", [65, 8], F32)
    ones32 = p.sbuf("ones32", [65, 128], F32)
    mhalf = p.sbuf("mhalf", [128, 1], F32)
    onesb = p.sbuf("onesb", [65, 128], BF16)
    fqT = p.sbuf("fqT", [128, S], BF16)
    fkT = p.sbuf("fkT", [128, S], BF16)
    dqT = p.sbuf("dqT", [128, S], BF16)
    dkT = p.sbuf("dkT", [128, S], BF16)
    fvx = p.sbuf("fvx", [128, nchunks, 129], BF16)
    dvx = p.sbuf("dvx", [128, nchunks, 129], BF16)
    rF = p.sbuf("rF", [65, S], BF16)
    fcol = p.sbuf("fcol", [128, nchunks], F32)
    fcolc = p.sbuf("fcolc", [128, nchunks], F32)
    negc = p.sbuf("negc", [128, 4], F32)
    junk = p.sbuf("junk", [128, D], BF16)
    NS = 3
    NXS = 3
    if fused is None:
        xs = [p.sbuf("xs%d" % i, [128, D], F32) for i in range(NXS)]
        hn = [p.sbuf("hn%d" % i, [128, D], F32) for i in range(NS)]
    else:
        hnb = [p.sbuf("hnb%d" % i, [128, D], BF16) for i in range(NS)]
        identb = p.sbuf("identb", [128, 128], BF16)
    hT = [p.sbuf("hT%d" % i, [128, 8, 128], BF16) for i in range(NS)]
    st = [p.sbuf("st%d" % i, [128, 8], F32) for i in range(NS)]
    x32f = [p.sbuf("x32f%d" % i, [128, 256], F32) for i in range(NS)]
    x32d = [p.sbuf("x32d%d" % i, [128, 256], F32) for i in range(NS)]
    sqf = [p.sbuf("sqf%d" % i, [128, 256], BF16) for i in range(NS)]
    sqd = [p.sbuf("sqd%d" % i, [128, 256], BF16) for i in range(NS)]
    ropet = [p.sbuf("ropet%d" % i, [128, 2, 256], F32) for i in range(NS)]
    t1 = [p.sbuf("t1_%d" % i, [128, 256], F32) for i in range(NS)]
    t2 = [p.sbuf("t2_%d" % i, [128, 256], F32) for i in range(NS)]
    grow = [p.sbuf("grow%d" % i, [65, 3, 128], F32) for i in range(NS)]
    NPT = 5
    PT = [p.sbuf("PT%d" % i, [128, 512], BF16) for i in range(NPT)]
    yblk = [p.sbuf("yblk%d" % i, [128, 4, 256], BF16) for i in range(2)]
    a0 = p.sbuf("a0", [128, 4, 128], F32)
    rd = [p.sbuf("rd%d" % i, [128, 4], F32) for i in range(2)]
    tmpa = [p.sbuf("tmpa%d" % i, [128, 128], F32) for i in range(2)]
    yd = [p.sbuf("yd%d" % i, [128, 128], F32) for i in range(2)]
    st2 = [p.sbuf("stb%d" % i, [128, 8], F32) for i in range(2)]
    tp = p.psum("tp", [128, 8, 128], F32)
    pb = [p.psum("pb%d" % i, [128, 512], F32) for i in range(6)]
    bFK, bD, bG, bG2, bR, bV = pb
    BK = lambda i: "bank%d" % i

    p.dma("sp", ident[:], ident_d, writes=["c_ident"])
    p.dma("sp", triu32[:], triu_d, writes=["c_triu32"])
    p.dma("sp", ptm[:], pt_d, writes=["c_ptm"])
    p.dma("sp", sel32[:], sel_d, writes=["c_sel32"])
    p.dma("sp", gbc[:], g_norm.partition_broadcast(128), writes=["c_gbc"])
    p.dma("pool", wf[:], wf_d.rearrange("(kc p) c -> p kc c", p=128), writes=["c_wf"])
    p.dma("pool", wtk[:], wt_d.rearrange("(kc p) c -> p kc c", p=128), writes=["c_wtk"])
    p.dma("sp", lamt[:], lam_d.partition_broadcast(128), writes=["c_lamt"])
    p.dma("sp", dng[:], dng_d.partition_broadcast(128), writes=["c_dng"])
    p.memset("dve", rowc[:], 0.0, ["rowc"])
    p.dma("sp", rowc[64:65, 0:1], fb_d, reads=[], writes=["rowc"])
    p.dma("sp", rowc[64:65, 3:6], csc_d, reads=[], writes=["rowc"])
    p.copy("dve", triub[:], triu32[:], ["c_triu32"], ["c_triub"])
    if fused is not None:
        p.copy("dve", identb[:], ident[:], ["c_ident"], ["c_identb"])
    p.copy("dve", selb[:], sel32[:], ["c_sel32"], ["c_selb"])
    p.memset("dve", ones32[:], 1.0, ["c_ones32"])
    p.memset("dve", mhalf[:], -0.5, ["c_mhalf"])
    p.memset("dve", onesb[:], 1.0, ["c_onesb"])
    p.memset("dve", rmax[:], 0.0, ["rmax"])
    p.ts("dve", rowc[64:65, 1:2], rowc[64:65, 0:1], -1.0, None, ALU.mult, None, [], ["rowc"])
    p.ts("dve", dng[:], dng[:], 1.0 - LAMBDA_INIT, None, ALU.mult, None, ["c_dng"], ["c_dng"])
    p.memset("dve", fvx[:, :, 128:129], 1.0, ["fvx1"])
    p.memset("dve", dvx[:, :, 128:129], 1.0, ["dvx1"])
    p.tt("dve", lamw[:, 0:64], lamt[:, 0:64], lamt[:, 64:128], ALU.mult, ["c_lamt"], ["lamw"])
    p.tt("dve", lamw[:, 64:128], lamt[:, 128:192], lamt[:, 192:256], ALU.mult, ["c_lamt"], ["lamw"])
    p.add("dve", lambda e: e.reduce_sum(out=lams[:, 0:1], in_=lamw[:, 0:64], axis=AX.X), ["lamw"], ["lams"])
    p.add("dve", lambda e: e.reduce_sum(out=lams[:, 1:2], in_=lamw[:, 64:128], axis=AX.X), ["lamw"], ["lams"])
    p.act(lams[:, 2:4], lams[:, 0:2], AF.Exp, [], ["lams"])
    p.tt("dve", lams[:, 4:5], lams[:, 3:4], lams[:, 2:3], ALU.subtract, [], ["lams"])
    p.ts("dve", lams[:, 5:6], lams[:, 4:5], -LAMBDA_INIT, None, ALU.add, None, [], ["lams"])

    groups = []
    for n in range(nchunks):
        s = n % NS
        x3 = n % NXS
        p.begin_group()
        K_ = lambda name, s=s: (name, s)
        c0, c1 = n * 128, (n + 1) * 128
        p.dma("sp", ropet[s][:], rope_d[n], writes=[K_("ropet")])
        if fused is not None:
            xr0 = (((c0 % 2048) // 512) * 4 + c0 // 2048) * 512 + c0 % 512
            p.dma("sp", hnb[s][:], x[xr0:xr0 + 128, :], writes=[K_("hn")])
            tpb = tp[:].rearrange("p k t -> p (k t)").bitcast(BF16)[:, 0:1024].rearrange("p (k t) -> p k t", k=8)
            for j in range(8):
                p.tr(tpb[:, j, :], hnb[s][:, j * 128:(j + 1) * 128], identb[:], [K_("hn"), "c_identb"], ["tp"])
            p.copy("act", hT[s][:], tpb, [], ["tp", K_("hT")])
        else:
            p.dma("sp", xs[x3][:], x[c0:c1, :], writes=[("xs", x3)])
        st_ = st[s]
        if fused is None:
            p.memset("dve", st_[:, 0:1], 0.0, [K_("st")])
            p.act(junk[:], xs[x3][:], AF.Square, [("xs", x3)], [K_("st")], accum_out=st_[:, 0:1])
            p.ts("dve", st_[:, 1:2], st_[:, 0:1], 1.0 / D, EPS, ALU.mult, ALU.add, [], [K_("st")])
            p.tt("pool", st_[:, 3:4], st_[:, 1:2], mhalf[:], ALU.pow, ["c_mhalf"], [K_("st")])
            p.stt("dve", hn[s][:], xs[x3][:], st_[:, 3:4], gbc[:], ALU.mult, ALU.mult,
                  [("xs", x3), K_("st"), "c_gbc"], [K_("hn")])
            for j in range(8):
                p.tr(tp[:, j, :], hn[s][:, j * 128:(j + 1) * 128], ident[:], [K_("hn"), "c_ident"], ["tp"])
            p.copy("act", hT[s][:], tp[:], [], ["tp", K_("hT")])
        for j in range(2):
            for kc in range(8):
                p.mm(bFK[:, j * 128:(j + 1) * 128], wf[:, kc, j * 128:(j + 1) * 128], hT[s][:, kc, :],
                     kc == 0, kc == 7, [K_("hT"), "c_wf"], [BK(0)])
        for j in range(2):
            for kc in range(8):
                p.mm(bD[:, j * 128:(j + 1) * 128], wf[:, kc, 256 + j * 128:256 + (j + 1) * 128], hT[s][:, kc, :],
                     kc == 0, kc == 7, [K_("hT"), "c_wf"], [BK(1)])
        for kc in range(8):
            p.mm(bG[0:65, 0:128], wf[:, kc, 512:577], hT[s][:, kc, :], kc == 0, kc == 7, [K_("hT"), "c_wf"], [BK(2)])
        for kc in range(8):
            p.mm(bV[:, 0:256], hT[s][:, kc, :], wtk[:, kc, :], kc == 0, kc == 7, [K_("hT"), "c_wtk"], [BK(5)])
        p.copy("act", x32f[s][:], bFK[:, 0:256], [], [BK(0), K_("x32f")])
        p.copy("dve", fqT[:, c0:c1], x32f[s][:, 0:128], [K_("x32f")], ["fqT"])
        p.copy("dve", fkT[:, c0:c1], x32f[s][:, 128:256], [K_("x32f")], ["fkT"])
        p.tt("pool", sqf[s][:], x32f[s][:], x32f[s][:], ALU.mult, [K_("x32f")], [K_("sqf")])
        p.mm(bG[0:65, 128:384], selb[:, 0, :], sqf[s][:], True, True, ["c_selb", K_("sqf")], [BK(2)])
        p.copy("act", x32d[s][:], bD[:, 0:256], [], [BK(1), K_("x32d")])
        p.tt("pool", sqd[s][:], x32d[s][:], x32d[s][:], ALU.mult, [K_("x32d")], [K_("sqd")])
        p.mm(bG2[0:65, 0:256], selb[:, 1, :], sqd[s][:], True, True, ["c_selb", K_("sqd")], [BK(3)])
        p.mm(bG2[0:65, 256:512], selb[:, 2, :], sqd[s][:], True, True, ["c_selb", K_("sqd")], [BK(3)])
        p.mm(bR[:, 0:256], ptm[:], x32d[s][:], True, True, ["c_ptm", K_("x32d")], [BK(4)])
        p.tt("dve", t1[s][:], x32d[s][:], ropet[s][:, 0, :], ALU.mult, [K_("x32d"), K_("ropet")], [K_("t1")])
        p.tt("dve", t2[s][:], bR[:, 0:256], ropet[s][:, 1, :], ALU.mult, [K_("ropet")], [BK(4), K_("t2")])
        p.tt("pool", dqT[:, c0:c1], t1[s][:, 0:128], t2[s][:, 0:128], ALU.add, [K_("t1"), K_("t2")], ["dqT"])
        p.tt("pool", dkT[:, c0:c1], t1[s][:, 128:256], t2[s][:, 128:256], ALU.add, [K_("t1"), K_("t2")], ["dkT"])
        p.add("dve", lambda e: e.reduce_max(out=rtmp[64:65, 0:2],
                                            in_=bG[64:65, 128:384].rearrange("p (a t) -> p a t", a=2), axis=AX.X),
              [], [BK(2), "rtmp"])
        p.add("dve", lambda e: e.reduce_max(out=rtmp[64:65, 2:6],
                                            in_=bG2[64:65, 0:512].rearrange("p (a t) -> p a t", a=4), axis=AX.X),
              [], [BK(3), "rtmp"])
        p.tt("dve", rmax[64:65, 0:6], rmax[64:65, 0:6], rtmp[64:65, 0:6], ALU.max, ["rtmp"], ["rmax"])
        gr = grow[s]
        p.act(gr[64:65, 0, :], bG[64:65, 0:128], AF.Exp, ["rowc"], [BK(2), K_("grow")], bias=rowc[64:65, 1:2], scale=-1.0)
        p.act(gr[64:65, 1, :], gr[64:65, 0, :], AF.Ln, [], [K_("grow")], bias=1.0)
        p.add("dve", lambda e, gr=gr: e.tensor_tensor_scan(out=gr[64:65, 2, :], data0=gr[64:65, 1, :], data1=gr[64:65, 1, :],
                                                          initial=rowc[64:65, 2:3], op0=ALU.add, op1=ALU.bypass),
              ["rowc"], [K_("grow")], force=True)
        p.copy("dve", rowc[64:65, 2:3], gr[64:65, 2, 127:128], [K_("grow")], ["rowc"])
        p.ts("dve", rF[64:65, c0:c1], gr[64:65, 2, :], -1.0 / FOX_SCALE, None, ALU.mult, None, [K_("grow")], ["rF"])
        p.mm(bV[:, 256:257], gr[64:65, 2, :], ones32[64:65, 0:1], True, True, [K_("grow"), "c_ones32"], [BK(5)])
        p.copy("dve", fcol[:, n:n + 1], bV[:, 256:257], [], [BK(5), "fcol"])
        p.copy("act", fvx[:, n, 0:128], bV[:, 0:128], ["fvx1"], [BK(5), "fvx"])
        p.copy("act", dvx[:, n, 0:128], bV[:, 128:256], ["dvx1"], [BK(5), "dvx"])
        groups.append(p.end_group())
    p.interleave(groups, 3)

    p.tt("dve", rowc[64:65, 6:9], rmax[64:65, 0:6:2], rmax[64:65, 1:6:2], ALU.mult, ["rmax"], ["rowc"])
    p.act(rowc[64:65, 6:9], rowc[64:65, 6:9], AF.Sqrt, [], ["rowc"])
    p.tt("dve", rowc[64:65, 9:12], rowc[64:65, 6:9], rowc[64:65, 3:6], ALU.mult, [], ["rowc"])
    p.mm(bR[:, 0:3], ones32[64:65, :], rowc[64:65, 9:12], True, True, ["c_ones32", "rowc"], [BK(4)])
    p.copy("dve", negc[:, 0:3], bR[:, 0:3], [], [BK(4), "negc"])
    p.ts("dve", fcolc[:], fcol[:], negc[:, 0:1], None, ALU.add, None, ["fcol", "negc"], ["fcolc"])

    accs = [[pb[0], pb[1]], [pb[2], pb[3]]]
    acc_tok = [[BK(0), BK(1)], [BK(2), BK(3)]]
    tpv = tp[:].rearrange("p k t -> p (k t)")
    stb = [pb[4][:, :], pb[5][:, :], tpv[:, 0:512], tpv[:, 512:1024]]
    st_tok = [BK(4), BK(5), "tp_a", "tp_b"]
    NSTB = 4
    LOOKA = 2
    jobc = 0
    kbc = 0
    for qb in range(NQB):
        q0 = qb * 512
        yb = yblk[qb % 2]
        ytok = ("yblk", qb % 2)
        for job in range(3):
            a = jobc % 2
            jobc += 1
            accv = [accs[a][0][:, 0:258].rearrange("p (i c) -> p i c", i=2),
                    accs[a][1][:, 0:258].rearrange("p (i c) -> p i c", i=2)]
            p.memset("dve", accs[a][0][:, 0:258], 0.0, [acc_tok[a][0]])
            p.memset("dve", accs[a][1][:, 0:258], 0.0, [acc_tok[a][1]])
            if job == 0:
                kT_, qT_, vx_, scale_, prow = fkT, fqT, fvx, FOX_SCALE, slice(0, 128)
                ktok, qtok, vtok = "fkT", "fqT", "fvx"
            else:
                g_ = job - 1
                kT_, qT_, vx_, scale_, prow = dkT, dqT, dvx, DIFF_SCALE, slice(g_ * 64, (g_ + 1) * 64)
                ktok, qtok, vtok = "dkT", "dqT", "dvx"
            nkb = 4 * qb + 4
            pend = {}

            def part1(kb):
                nonlocal kbc
                j = kb - 4 * qb
                cs = max(j, 0) * 128
                sb_ = kbc % NSTB
                pt_ = PT[kbc % NPT]
                pttok = ("PT", kbc % NPT)
                kbc += 1
                ST = stb[sb_]
                if job == 0:
                    p.mm(ST[:, cs:512], kT_[prow, kb * 128:(kb + 1) * 128], qT_[prow, q0 + cs:q0 + 512], True, False,
                         [ktok, qtok], [st_tok[sb_]])
                    p.mm(ST[:, cs:512], onesb[64:65, :], rF[64:65, q0 + cs:q0 + 512], False, True,
                         ["c_onesb", "rF"], [st_tok[sb_]])
                    bias_ = fcolc[:, kb:kb + 1]
                    btok = "fcolc"
                else:
                    p.mm(ST[:, cs:512], kT_[prow, kb * 128:(kb + 1) * 128], qT_[prow, q0 + cs:q0 + 512], True, True,
                         [ktok, qtok], [st_tok[sb_]])
                    bias_ = negc[:, job:job + 1]
                    btok = "negc"
                p.act(pt_[:, cs:512], ST[:, cs:512], AF.Exp, [btok], [st_tok[sb_], pttok], bias=bias_, scale=scale_)
                if j >= 0:
                    p.tt("pool", pt_[:, cs:cs + 128], pt_[:, cs:cs + 128], triub[:], ALU.mult, ["c_triub"], [pttok])
                pend[kb] = (pt_, pttok, j)

            def part2(kb):
                pt_, pttok, j = pend.pop(kb)
                for i in range(max(j, 0), 4):
                    p.mm(accv[i // 2][:, i % 2, :], pt_[:, i * 128:(i + 1) * 128], vx_[:, kb, :], False, kb == 4 * qb + i,
                         [pttok, vtok], [acc_tok[a][i // 2]])

            for kb in range(min(LOOKA, nkb)):
                part1(kb)
            for kb in range(nkb):
                if kb + LOOKA < nkb:
                    part1(kb + LOOKA)
                part2(kb)
            r_ = rd[a]
            rtok = ("rd", a)
            for bi in range(2):
                p.add("dve", lambda e, r_=r_, bi=bi, accv=accv: e.reciprocal(out=r_[:, bi * 2:bi * 2 + 2],
                                                                          in_=accv[bi][:, :, 128]),
                      [], [acc_tok[a][bi], rtok])
            for i in range(4):
                src = accv[i // 2][:, i % 2, 0:128]
                atk = acc_tok[a][i // 2]
                if job == 0:
                    p.ts("dve", yb[:, i, 0:128], src, r_[:, i:i + 1], None, ALU.mult, None, [rtok], [atk, ytok])
                elif job == 1:
                    p.ts("dve", a0[:, i, :], src, r_[:, i:i + 1], None, ALU.mult, None, [rtok], [atk, "a0"])
                else:
                    w_ = i % 2
                    p.ts("dve", tmpa[w_][:], src, r_[:, i:i + 1], None, ALU.mult, None, [rtok], [atk, ("tmpa", w_)])
                    p.stt("dve", yd[w_][:], tmpa[w_][:], lams[:, 5:6], a0[:, i, :], ALU.mult, ALU.add,
                          [("tmpa", w_), "lams", "a0"], [("yd", w_)])
                    s2 = st2[w_]
                    s2t = ("st2", w_)
                    p.memset("dve", s2[:, 0:1], 0.0, [s2t])
                    p.act(junk[:, 0:128], yd[w_][:], AF.Square, [("yd", w_)], [s2t], accum_out=s2[:, 0:1])
                    p.ts("dve", s2[:, 1:2], s2[:, 0:1], 1.0 / 128, EPS, ALU.mult, ALU.add, [], [s2t])
                    p.tt("pool", s2[:, 3:4], s2[:, 1:2], mhalf[:], ALU.pow, ["c_mhalf"], [s2t])
                    p.stt("dve", yb[:, i, 128:256], yd[w_][:], s2[:, 3:4], dng[:], ALU.mult, ALU.mult,
                          [("yd", w_), s2t, "c_dng"], [ytok])
        p.dma("sp", y[q0:q0 + 512, :].rearrange("(i p) c -> p i c", p=128), yb[:], reads=[ytok], writes=[("y", qb)],
              key=("yblk", qb % 2))
        if fused is not None and (qb + 1) % 4 == 0:
            fused["gather_piece"](p, "y2", qb // 4, [("y", m) for m in range(qb - 3, qb + 1)])
    p.add("sp", lambda e: e.nop(), reads=[("y", qb) for qb in range(NQB)])
    if fused is not None:
        p.finish(barrier=True)
        return None
    p.emit()
    p.close()
    return nc


def odd_core_inputs(inp, x2b, h, S=SEQ):
    ident, triu, _ = _consts()
    w = inp["odd_w_in"][0]
    c = lambda a0: np.arange(a0 + h * 128, a0 + (h + 1) * 128)
    ffpad = np.zeros((D, 65), np.float32)
    ffpad[:, 64] = w[:, 1536 + h]
    wf = np.concatenate([w[:, c(0)], w[:, c(512)], w[:, c(1540)], w[:, c(2052)], ffpad], axis=1)
    wt = np.concatenate([w[:, c(1024)], w[:, c(2564)]], axis=1)
    nch = S // 128
    half = 8
    inv = (np.float32(500000.0) ** (-np.arange(half, dtype=np.float32) / half)).astype(np.float32)
    ang = np.arange(S, dtype=np.float32)[:, None] * inv[None, :]
    cos = np.cos(ang).astype(np.float32)
    sin = np.sin(ang).astype(np.float32)
    tab = np.zeros((nch, 128, 2, 256), np.float32)
    tab[:, :, 0, :] = 1.0
    ct = cos.reshape(nch, 128, 8).transpose(0, 2, 1)
    stt = sin.reshape(nch, 128, 8).transpose(0, 2, 1)
    for base in (0, 64):
        for hf in (0, 8):
            for qk in (0, 128):
                tab[:, base + hf:base + hf + 8, 0, qk:qk + 128] = ct
                tab[:, base + hf:base + hf + 8, 1, qk:qk + 128] = stt
    ptm = np.zeros((128, 128), np.float32)
    for base in (0, 64):
        for i in range(8):
            ptm[base + i + 8, base + i] = -1.0
            ptm[base + i, base + i + 8] = 1.0
    sel = np.zeros((128, 3, 65), np.float32)
    sel[:, 0, 64] = 1.0
    sel[0:64, 1, 64] = 1.0
    sel[64:128, 2, 64] = 1.0
    csc = (-1.02 * np.array([[FOX_SCALE, DIFF_SCALE, DIFF_SCALE]])).astype(np.float32)
    return {
        "x": None if x2b is None else np.ascontiguousarray(x2b[:S]),
        "g_norm": inp["odd_norm_mix"][0],
        "wf": np.ascontiguousarray(wf), "wt": np.ascontiguousarray(wt),
        "fb": inp["odd_fox_f_b"][0][h].reshape(1, 1).astype(np.float32),
        "lam": np.ascontiguousarray(inp["odd_diff_lambda"][0].reshape(256)),
        "dng": inp["odd_diff_norm_g"][0],
        "rope": tab, "ptm": ptm, "sel": sel, "cscale": csc, "ident": ident, "triu": triu,
    }


_PROGS = {}


def _prog(name, builder):
    if name not in _PROGS:
        _PROGS[name] = builder()
    return _PROGS[name]


def _run(nc, in_maps):
    res = run_bass_kernel_spmd(nc, in_maps, core_ids=list(range(NCORES)))
    return res.results


def _assemble_y(results):
    yfull = np.empty((2, SEQ, D), dtype=ml_dtypes.bfloat16)
    for c in range(NCORES):
        b, h = c // 4, c % 4
        yc = results[c]["y"]
        yfull[b, :, h * 128:(h + 1) * 128] = yc[:, 0:128]
        yfull[b, :, 512 + h * 128:512 + (h + 1) * 128] = yc[:, 128:256]
    yflat = yfull.reshape(2 * SEQ, D)
    return [np.ascontiguousarray(yflat[c * 2048:(c + 1) * 2048].T) for c in range(NCORES)]


def kernel_unfused(**inp):
    inp = {k: np.asarray(v) for k, v in inp.items()}
    ident = np.eye(128, dtype=np.float32)
    x = np.ascontiguousarray(inp["x"], dtype=np.float32)
    nc1 = _prog("even", build_even_kernel)
    r1 = _run(nc1, [even_core_inputs(inp, c // 4, c % 4) for c in range(NCORES)])
    yT = _assemble_y(r1)
    xflat = x.reshape(2 * SEQ, D)
    nc2 = _prog("ffn", lambda: build_tok_kernel("ffn"))
    r2 = _run(nc2, [{"xin": np.ascontiguousarray(xflat[c * 2048:(c + 1) * 2048]), "yT": yT[c],
                     "w_out": inp["even_w_out"][0], "g_norm": inp["even_norm_ffn"][0],
                     "w_gu": inp["ffn_w_gate_up"], "w_d": inp["ffn_w_down"], "ident": ident}
                    for c in range(NCORES)])
    x2 = np.concatenate([r2[c]["xout"] for c in range(NCORES)], axis=0).reshape(2, SEQ, D)
    nc3 = _prog("odd", build_odd_kernel)
    r3 = _run(nc3, [odd_core_inputs(inp, x2[c // 4], c % 4) for c in range(NCORES)])
    yT2 = _assemble_y(r3)
    x2flat = x2.reshape(2 * SEQ, D)
    w_r = np.ascontiguousarray(inp["moe_w_router"][0].reshape(8, 128, 8).transpose(1, 0, 2)).reshape(128, 64)
    nc4 = _prog("moe", lambda: build_tok_kernel("moe"))
    r4 = _run(nc4, [{"xin": np.ascontiguousarray(x2flat[c * 2048:(c + 1) * 2048]), "yT": yT2[c],
                     "w_out": inp["odd_w_out"][0], "g_norm": inp["odd_norm_ffn"][0],
                     "w_gu": inp["moe_w_gate_up"][0], "w_d": inp["moe_w_down"][0], "ident": ident,
                     "w_r": w_r, "g_fin": inp["final_norm"]}
                    for c in range(NCORES)])
    out = np.concatenate([r4[c]["xout"] for c in range(NCORES)], axis=0).reshape(2, SEQ, D)
    return out.astype(np.float32)


RG = [[0, 1, 2, 3], [4, 5, 6, 7]]


def build_fused():
    nc = bass.Bass("TRN2", target_bir_lowering=False)
    ov = {}
    names = {"in": [], "out": []}

    def tensor(name, shape, dt, kind):
        if name in ov:
            return ov[name]
        names["in" if kind == "ExternalInput" else "out"].append(name)
        return nc.dram_tensor(name, shape, dt, kind=kind)

    fused = {"nc": nc, "tensor": tensor, "semstack": "raw"}
    y1 = nc.dram_tensor("i_y1", [SEQ, 256], BF16)
    y1g = nc.dram_tensor("i_y1g", [4 * SEQ, 256], BF16)
    x2s = nc.dram_tensor("i_x2s", [2048, D], F32)
    x2h = nc.dram_tensor("i_x2h", [2048, D], BF16)
    x2g = nc.dram_tensor("i_x2g", [SEQ, D], BF16)
    y2 = nc.dram_tensor("i_y2", [SEQ, 256], BF16)
    y2g = nc.dram_tensor("i_y2g", [4 * SEQ, 256], BF16)

    gspec = {"y1": (y1, y1g, 2048), "x2": (x2h, x2g, 512), "y2": (y2, y2g, 2048)}

    def gather_piece(p, which, j, read_tokens):
        src, dst, rows = gspec[which]
        p.dma_fn("pool", lambda e: e.collective_compute(
            "AllGather", ALU.bypass, replica_groups=RG,
            ins=[src.ap()[j * rows:(j + 1) * rows, :].opt()],
            outs=[dst.ap()[j * 4 * rows:(j + 1) * 4 * rows, :].opt()]),
            reads=read_tokens, writes=[("gath", which, j)], key=("cc", which, j), inc=1)

    fused["gather_piece"] = gather_piece
    ov["ev_y"] = y1
    with nc.cleanup_on_exit():
        build_even_kernel(fused=fused)
    ov["tk0_yT"] = y1g
    ov["tk0_xout"] = x2s
    ov["tk0_hnout"] = x2h
    with nc.cleanup_on_exit():
        build_tok_kernel("ffn", fused=fused)
    ov["od_x"] = x2g
    ov["od_y"] = y2
    with nc.cleanup_on_exit():
        build_odd_kernel(fused=fused)
    ov["tk1_xin"] = x2s
    ov["tk1_yT"] = y2g
    with nc.cleanup_on_exit():
        build_tok_kernel("moe", fused=fused)
    return nc, names


def kernel(**inp):
    inp = {k: np.asarray(v) for k, v in inp.items()}
    ident = np.eye(128, dtype=np.float32)
    x = np.ascontiguousarray(inp["x"], dtype=np.float32)
    xflat = x.reshape(2 * SEQ, D)
    if "fused" not in _PROGS:
        _PROGS["fused"] = build_fused()
    nc, names = _PROGS["fused"]
    w_r = np.ascontiguousarray(inp["moe_w_router"][0].reshape(8, 128, 8).transpose(1, 0, 2)).reshape(128, 64)
    in_maps = []
    for c in range(NCORES):
        b, h = c // 4, c % 4
        m = {}
        for k, v in even_core_inputs(inp, b, h).items():
            m["ev_" + k] = v
        od = odd_core_inputs(inp, None, h)
        for k, v in od.items():
            if k != "x":
                m["od_" + k] = v
        m.update({"tk0_xin": np.ascontiguousarray(xflat[c * 2048:(c + 1) * 2048]),
                  "tk0_w_out": inp["even_w_out"][0], "tk0_g_norm": inp["even_norm_ffn"][0],
                  "tk0_w_gu": inp["ffn_w_gate_up"], "tk0_w_d": inp["ffn_w_down"], "tk0_ident": ident,
                  "tk0_g_next": inp["odd_norm_mix"][0]})
        m.update({"tk1_w_out": inp["odd_w_out"][0], "tk1_g_norm": inp["odd_norm_ffn"][0],
                  "tk1_w_gu": inp["moe_w_gate_up"][0], "tk1_w_d": inp["moe_w_down"][0], "tk1_ident": ident,
                  "tk1_w_r": w_r, "tk1_g_fin": inp["final_norm"]})
        assert set(m.keys()) == set(names["in"]), (set(m.keys()) ^ set(names["in"]))
        in_maps.append(m)
    res = run_bass_kernel_spmd(nc, in_maps, core_ids=list(range(NCORES)))
    out = np.concatenate([res.results[c]["tk1_xout"] for c in range(NCORES)], axis=0).reshape(2, SEQ, D)
    return out.astype(np.float32)
```

```python
from contextlib import ExitStack
import math
import numpy as np
import ml_dtypes
import concourse.bass as bass
import concourse.mybir as mybir
from concourse.bass_utils import run_bass_kernel_spmd

F32 = mybir.dt.float32
BF16 = mybir.dt.bfloat16
AF = mybir.ActivationFunctionType
ALU = mybir.AluOpType
AX = mybir.AxisListType

NCORES = 8
D = 1024
SEQ = 8192
EPS = 1e-6
SEM_EPOCH = 30000
DEBUG = False
E_OVERRIDE = None
PIPE_DEPTH = 5
DBG_SKIP = set()


class Op:
    __slots__ = ("eng", "fn", "deps", "is_dma", "sem", "val", "flag", "key", "force", "inc")

    def __init__(self, eng, fn, is_dma=False, key=None):
        self.eng = eng
        self.fn = fn
        self.deps = []
        self.is_dma = is_dma
        self.sem = None
        self.val = None
        self.flag = False
        self.key = key
        self.force = False
        self.inc = 16


class Prog:
    ENGS = ("pe", "act", "dve", "pool", "sp")

    def __init__(self, nc, pfx="", semstack=None):
        self.nc = nc
        self.pfx = pfx
        self.semstack = semstack
        self.ops = {e: [] for e in self.ENGS}
        self.last_w = {}
        self.readers = {}
        self.stack = ExitStack()
        self.dma_sems = {}
        self.dma_cnt = {}
        self.n_sems = 0
        self._group = None
        self.same_engine_sync = True

    def sbuf(self, name, shape, dtype):
        return self.stack.enter_context(self.nc.sbuf_tensor(self.pfx + name, list(shape), dtype))

    def psum(self, name, shape, dtype):
        return self.stack.enter_context(self.nc.psum_tensor(self.pfx + name, list(shape), dtype))

    def _new_sem(self, name):
        self.n_sems += 1
        if self.semstack == "raw":
            return self.nc.alloc_semaphore(name=self.pfx + name)
        return self.stack.enter_context(self.nc.semaphore(self.pfx + name))

    def finish(self, barrier=True):
        if barrier:
            dmas = [op for e in self.ENGS for op in self.ops[e] if op.is_dma]
            ends = []
            for e in self.ENGS:
                last = [op for op in self.ops[e] if not op.is_dma]
                op = self.add(e, lambda eng: eng.nop(), force=True)
                op.deps = last[-1:] if last else []
                ends.append(op)
            for e in self.ENGS:
                op = self.add(e, lambda eng: eng.nop(), force=True)
                op.deps = [x for x in ends if x.eng != e] + dmas
        self.emit()
        self.close()

    def _track(self, op, reads, writes):
        deps = []
        seen = set()

        def add(d):
            if d is None or id(d) in seen or d is op:
                return
            seen.add(id(d))
            deps.append(d)

        for t in reads:
            add(self.last_w.get(t))
        for t in writes:
            add(self.last_w.get(t))
            for r in self.readers.get(t, ()):
                add(r)
        op.deps = deps
        for t in reads:
            self.readers.setdefault(t, []).append(op)
        for t in writes:
            self.last_w[t] = op
            self.readers[t] = []

    def _commit(self, op, reads, writes):
        if self._group is not None:
            self._group.append((op, tuple(reads), tuple(writes)))
        else:
            self._track(op, reads, writes)
            self.ops[op.eng].append(op)
        return op

    def begin_group(self):
        self._group = []

    def end_group(self):
        g, self._group = self._group, None
        return g

    def interleave(self, groups, depth, window=8):
        if not groups:
            return
        last = []
        lastw = []
        for g in groups:
            d, dw = {}, {}
            for i, (op, reads, writes) in enumerate(g):
                for t in reads:
                    d[t] = i
                for t in writes:
                    d[t] = i
                    dw[t] = i
            last.append(d)
            lastw.append(dw)
        L = max(len(g) for g in groups)
        step = max(1, L // depth)
        pos = [0] * len(groups)
        t = 0
        done = 0
        first_active = 0
        while done < len(groups):
            progressed = False
            for gi in range(first_active, len(groups)):
                if gi * step > t:
                    break
                if pos[gi] >= len(groups[gi]):
                    continue
                op, reads, writes = groups[gi][pos[gi]]
                ok = True
                for gj in range(max(first_active, gi - window), gi):
                    lj = last[gj]
                    lwj = lastw[gj]
                    pj = pos[gj]
                    for tk in writes:
                        if tk in lj and pj <= lj[tk]:
                            ok = False
                            break
                    if ok:
                        for tk in reads:
                            if tk in lwj and pj <= lwj[tk]:
                                ok = False
                                break
                    if not ok:
                        break
                if not ok:
                    continue
                pos[gi] += 1
                progressed = True
                if pos[gi] == len(groups[gi]):
                    done += 1
                self._track(op, reads, writes)
                self.ops[op.eng].append(op)
            while first_active < len(groups) and pos[first_active] >= len(groups[first_active]):
                first_active += 1
            t += 1
            assert progressed or first_active >= len(groups) or first_active * step > t - 1, "interleave stuck"

    def add(self, eng, fn, reads=(), writes=(), force=False):
        op = Op(eng, fn)
        op.force = force
        return self._commit(op, reads, writes)

    def dma(self, q, out, in_, reads=(), writes=(), key=None, **kw):
        if key is None:
            key = ("auto", writes[0] if writes else reads[0])
        op = Op(q, None, is_dma=True, key=key)
        op.fn = lambda e: e.dma_start(out=out, in_=in_, **kw)
        return self._commit(op, reads, writes)

    def dma_fn(self, q, fn, reads=(), writes=(), key=None, inc=16):
        op = Op(q, fn, is_dma=True, key=key)
        op.inc = inc
        return self._commit(op, reads, writes)

    def mm(self, out, lhsT, rhs, start, stop, reads, writes):
        return self.add("pe", lambda e: e.matmul(out, lhsT=lhsT, rhs=rhs, start=start, stop=stop),
                        reads, writes)

    def tr(self, out, in_, ident, reads, writes):
        return self.add("pe", lambda e: e.transpose(out=out, in_=in_, identity=ident), reads, writes)

    def act(self, out, in_, func, reads, writes, eng="act", **kw):
        force = any(not isinstance(v, (int, float)) for k, v in kw.items() if k in ("bias", "scale"))
        return self.add(eng, lambda e: e.activation(out=out, in_=in_, func=func, **kw), reads, writes,
                        force=force)

    def copy(self, eng, out, in_, reads, writes):
        if eng == "act":
            return self.add("act", lambda e: e.copy(out=out, in_=in_), reads, writes)
        return self.add(eng, lambda e: e.tensor_copy(out=out, in_=in_), reads, writes)

    def tt(self, eng, out, in0, in1, op, reads, writes):
        return self.add(eng, lambda e: e.tensor_tensor(out=out, in0=in0, in1=in1, op=op), reads, writes)

    def ts(self, eng, out, in0, s1, s2, op0, op1, reads, writes):
        force = not (isinstance(s1, (int, float)) and (s2 is None or isinstance(s2, (int, float))))
        if op1 is None:
            return self.add(eng, lambda e: e.tensor_scalar(out=out, in0=in0, scalar1=s1, scalar2=None,
                                                           op0=op0), reads, writes, force=force)
        return self.add(eng, lambda e: e.tensor_scalar(out=out, in0=in0, scalar1=s1, scalar2=s2,
                                                       op0=op0, op1=op1), reads, writes, force=force)

    def stt(self, eng, out, in0, scalar, in1, op0, op1, reads, writes):
        force = not isinstance(scalar, (int, float))
        return self.add(eng, lambda e: e.scalar_tensor_tensor(out=out, in0=in0, scalar=scalar, in1=in1,
                                                              op0=op0, op1=op1), reads, writes, force=force)

    def memset(self, eng, ap, val, writes):
        return self.add(eng, lambda e: e.memset(ap, val), (), writes)

    def emit(self):
        nc = self.nc
        for e in self.ENGS:
            for op in self.ops[e]:
                for d in op.deps:
                    if d.is_dma:
                        continue
                    if d.eng != op.eng or op.is_dma or op.force or (self.same_engine_sync and op.eng != "pe"):
                        d.flag = True
        for e in self.ENGS:
            cnt = 0
            sem = None
            for op in self.ops[e]:
                if op.is_dma:
                    k = op.key
                    if k not in self.dma_sems:
                        self.dma_sems[k] = self._new_sem("d%d" % len(self.dma_sems))
                        self.dma_cnt[k] = 0
                    self.dma_cnt[k] += op.inc
                    op.sem = self.dma_sems[k]
                    op.val = self.dma_cnt[k]
                elif op.flag:
                    if sem is None or cnt >= SEM_EPOCH:
                        sem = self._new_sem("e_%s_%d" % (e, self.n_sems))
                        cnt = 0
                    cnt += 1
                    op.sem = sem
                    op.val = cnt
        engmap = {"pe": "tensor", "act": "scalar", "dve": "vector", "pool": "gpsimd", "sp": "sync"}
        with nc.Block() as block:
            for e in self.ENGS:
                ops = self.ops[e]
                if not ops:
                    continue

                def body(eng, ops=ops, e=e):
                    waited = {}
                    for op in ops:
                        need = {}
                        for d in op.deps:
                            if d.sem is None:
                                continue
                            if (not d.is_dma) and d.eng == e and not (
                                    op.is_dma or op.force or (self.same_engine_sync and e != "pe")):
                                continue
                            sid = id(d.sem)
                            if waited.get(sid, 0) >= d.val:
                                continue
                            if sid not in need or need[sid][1] < d.val:
                                need[sid] = (d.sem, d.val)
                        for sid, (s, v) in need.items():
                            eng.wait_ge(s, v)
                            waited[sid] = v
                        ins = op.fn(eng)
                        if op.is_dma:
                            ins.then_inc(op.sem, op.inc)
                        elif op.flag:
                            ins.then_inc(op.sem, 1)

                getattr(block, engmap[e])(body)

    def close(self):
        self.stack.close()


def norm_rows(p, x_ap, xtok, g_bc, hn_ap, hntok, st, sttok, junk, junktok, D_=D):
    p.memset("dve", st[:, 0:1], 0.0, [sttok])
    p.act(junk, x_ap, AF.Square, [xtok, sttok], [junktok, sttok], accum_out=st[:, 0:1])
    p.ts("dve", st[:, 1:2], st[:, 0:1], 1.0 / D_, EPS, ALU.mult, ALU.add, [sttok], [sttok])
    p.act(st[:, 2:3], st[:, 1:2], AF.Sqrt, [sttok], [sttok])
    p.add("dve", lambda e: e.reciprocal(out=st[:, 3:4], in_=st[:, 2:3]), [sttok], [sttok])
    p.stt("dve", hn_ap, x_ap, st[:, 3:4], g_bc, ALU.mult, ALU.mult, [xtok, sttok, "const"], [hntok])


def build_tok_kernel(kind, fused=None):
    moe = kind == "moe"
    NTOK = 2048
    PASS = 1024
    NPASS = NTOK // PASS
    TPP = PASS // 128
    if moe:
        E, DFF = E_OVERRIDE or 8, 3584
    else:
        E, DFF = 1, 2816
    NFC = DFF // 128
    UC = 2
    NU = NFC // UC
    PU = 4
    nparts = (NU + PU - 1) // PU
    base, extra = NU // nparts, NU % nparts
    parts, u0 = [], 0
    for i in range(nparts):
        n_ = base + (1 if i < extra else 0)
        parts.append(list(range(u0, u0 + n_)))
        u0 += n_

    if fused is None:
        nc = bass.Bass("TRN2", target_bir_lowering=False)
        pfx = ""
        _dt = lambda name, shape, dt, kind: nc.dram_tensor(name, shape, dt, kind=kind)
    else:
        nc, pfx = fused["nc"], "tk%d_" % (1 if moe else 0)
        _dt = lambda name, shape, dt, kind: fused["tensor"](pfx + name, shape, dt, kind)
    class _W:
        def dram_tensor(self, name, shape, dt, kind="Internal"):
            return _dt(name, shape, dt, kind)
    ncd = _W()
    xin = ncd.dram_tensor("xin", [NTOK, D], F32, kind="ExternalInput").ap()
    if fused is None:
        yT = ncd.dram_tensor("yT", [D, NTOK], BF16, kind="ExternalInput").ap()
    else:
        yg = ncd.dram_tensor("yT", [4 * SEQ, 256], BF16, kind="ExternalInput").ap()
    w_out = ncd.dram_tensor("w_out", [D, D], F32, kind="ExternalInput").ap()
    g_norm = ncd.dram_tensor("g_norm", [D], F32, kind="ExternalInput").ap()
    w_gu = ncd.dram_tensor("w_gu", [E, D, 2 * DFF], F32, kind="ExternalInput").ap()
    w_d = ncd.dram_tensor("w_d", [E, DFF, D], F32, kind="ExternalInput").ap()
    ident_d = ncd.dram_tensor("ident", [128, 128], F32, kind="ExternalInput").ap()
    if moe:
        w_r = ncd.dram_tensor("w_r", [128, 64], F32, kind="ExternalInput").ap()
        g_fin = ncd.dram_tensor("g_fin", [D], F32, kind="ExternalInput").ap()
    xout = ncd.dram_tensor("xout", [NTOK, D], F32, kind="ExternalOutput").ap()
    hoist = (fused is not None) and not moe
    if hoist:
        g_next = ncd.dram_tensor("g_next", [D], F32, kind="ExternalInput").ap()
        hnout = ncd.dram_tensor("hnout", [NTOK, D], BF16, kind="ExternalOutput").ap()

    p = Prog(nc, pfx, fused['semstack'] if fused else None)
    NSL = 3
    ident = p.sbuf("ident_sb", [128, 128], F32)
    gbc = p.sbuf("gbc", [128, D], F32)
    mhalf = p.sbuf("mhalf", [128, 1], F32)
    wo = p.sbuf("wo", [128, 8, D], BF16)
    acc = [p.sbuf("acc%d" % i, [128, D], F32) for i in range(TPP)]
    hT = p.sbuf("hT", [128, 8, PASS], BF16)
    yTs = p.sbuf("yTs", [128, 8, PASS], BF16)
    if fused is not None:
        identb = p.sbuf("identb", [128, 128], BF16)
        ysb = p.sbuf("ysb", [128, TPP, 4, 256], BF16)
    NCH_PART = max(len(pp) for pp in parts) * UC
    actT = p.sbuf("actT", [128, NCH_PART, PASS], BF16)
    NWS = 3
    wg = [p.sbuf("wg%d" % i, [128, 8, UC * 128], BF16) for i in range(NWS)]
    wu = [p.sbuf("wu%d" % i, [128, 8, UC * 128], BF16) for i in range(NWS)]
    wd = [[p.sbuf("wd%d_%d" % (i, j), [128, UC, D], BF16) for j in range(PU)] for i in range(2)]
    hn = [p.sbuf("hn%d" % i, [128, D], F32) for i in range(NSL)]
    junk = p.sbuf("junk", [128, D], BF16)
    if hoist:
        gnx = p.sbuf("gnx", [128, D], F32)
        hnb = [p.sbuf("hnb%d" % i, [128, D], BF16) for i in range(2)]
    st = [p.sbuf("st%d" % i, [128, 8], F32) for i in range(NSL)]
    sg = [p.sbuf("sg%d" % i, [128, 512], BF16) for i in range(2)]
    if moe:
        gfin = p.sbuf("gfin", [128, D], F32)
        wr = p.sbuf("wr", [128, 8, 8], F32)
        hT32 = [p.sbuf("hT32_%d" % i, [128, 8, 128], F32) for i in range(NSL)]
        gates = [p.sbuf("gates%d" % i, [128, 8], F32) for i in range(TPP)]
        rt = [p.sbuf("rt%d" % i, [128, 40], F32) for i in range(NSL)]
    tp = p.psum("tp", [128, 8, 128], F32)
    dn = [p.psum("dn%d" % i, [128, 512], F32) for i in range(2)]
    gA = [p.psum("gA%d" % i, [128, 512], F32) for i in range(2)]
    uB = [p.psum("uB%d" % i, [128, 512], F32) for i in range(2)]

    p.dma("sp", ident[:], ident_d, writes=["const_id"])
    p.memset("dve", mhalf[:], -0.5, ["c_mhalf"])
    if fused is not None:
        p.copy("dve", identb[:], ident[:], ["const_id"], ["const_idb"])
    p.dma("sp", gbc[:], g_norm.partition_broadcast(128), writes=["const_g"])
    if hoist:
        p.dma("sp", gnx[:], g_next.partition_broadcast(128), writes=["const_gnx"])
    p.dma("pool", wo[:], w_out.rearrange("(kc p) c -> p kc c", p=128), writes=["const_wo"])
    if moe:
        p.dma("sp", gfin[:], g_fin.partition_broadcast(128), writes=["const_gf"])
        p.dma("sp", wr[:], w_r.rearrange("p (kc e) -> p kc e", e=8), writes=["const_wr"])

    ucount = 0
    pcount = 0
    rank_cache = {}
    dncount = 0
    gucount = 0
    for ps_i in range(NPASS):
        t0p = ps_i * PASS
        pgroups = []
        for ti in range(TPP):
            p.begin_group()
            tok0 = t0p + ti * 128
            ytok = "yTs"
            if fused is None:
                if ti == 0:
                    p.dma("sp", yTs[:], yT.rearrange("(kc p) t -> p kc t", p=128)[:, :, t0p:t0p + PASS],
                          writes=[ytok])
            else:
                ytok = ("yTs", ti)
                if ti == 0:
                    for h_ in range(4):
                        def _ld(e, tl=t0p, h_=h_):
                            if "rank" not in rank_cache:
                                rank_cache["rank"] = e.snap(e.partition_id() % 4, min_val=0, max_val=3)
                            rank = rank_cache["rank"]
                            src = yg.rearrange("(k h t) c -> h k t c", k=4, h=4)[h_, bass.ds(rank, 1), tl:tl + PASS]
                            return e.dma_start(out=ysb[:, :, h_, :], in_=src.rearrange("k (i p) c -> (k p) i c", p=128))

                        p.dma_fn("sp", _ld, reads=[], writes=[("ysb", h_)], key=("ysb", h_))
                ys_ = ysb[:, ti]
                tpy = uB[1][:].bitcast(BF16).rearrange("p (k t) -> p k t", k=8)
                for h_ in range(4):
                    for part in range(2):
                        p.tr(tpy[:, part * 4 + h_, :], ys_[:, h_, part * 128:(part + 1) * 128], identb[:],
                             [("ysb", h_), "const_idb"], [("uB", 1)])
                p.copy("act", yTs[:, :, ti * 128:(ti + 1) * 128], tpy, [], [("uB", 1), ytok])
            a = acc[ti]
            atok = ("acc", ti)
            p.dma("sp", a[:], xin[tok0:tok0 + 128, :], writes=[atok], key=("acc", ti))
            for hf in range(2):
                d_ = dn[dncount % 2]
                dtok = ("dn", dncount % 2)
                dncount += 1
                for kc in range(8):
                    p.mm(d_[:], yTs[:, kc, ti * 128:(ti + 1) * 128], wo[:, kc, hf * 512:(hf + 1) * 512],
                         kc == 0, kc == 7, [ytok, "const_wo"], [dtok])
                p.tt("dve", a[:, hf * 512:(hf + 1) * 512], d_[:], a[:, hf * 512:(hf + 1) * 512], ALU.add,
                     [dtok, atok], [atok])
            s_ = st[ti % NSL]
            stok = ("st", ti % NSL)
            h_ = hn[ti % NSL]
            htok = ("hn", ti % NSL)
            p.memset("dve", s_[:, 0:1], 0.0, [stok])
            p.act(junk[:], a[:], AF.Square, [atok, stok], [stok], accum_out=s_[:, 0:1])
            p.ts("dve", s_[:, 1:2], s_[:, 0:1], 1.0 / D, EPS, ALU.mult, ALU.add, [stok], [stok])
            p.tt("pool", s_[:, 3:4], s_[:, 1:2], mhalf[:], ALU.pow, [stok, "c_mhalf"], [stok])
            p.stt("dve", h_[:], a[:], s_[:, 3:4], gbc[:], ALU.mult, ALU.mult, [atok, stok, "const_g"], [htok])
            for j in range(8):
                p.tr(tp[:, j, :], h_[:, j * 128:(j + 1) * 128], ident[:], [htok, "const_id"], ["tp"])
            if not moe:
                p.copy("act", hT[:, :, ti * 128:(ti + 1) * 128], tp[:], ["tp"], [("hT", ti)])
            else:
                h32 = hT32[ti % NSL]
                h32tok = ("hT32", ti % NSL)
                p.copy("act", h32[:], tp[:], ["tp"], [h32tok])
                p.copy("dve", hT[:, :, ti * 128:(ti + 1) * 128], h32[:], [h32tok], [("hT", ti)])
            if moe and "router" in DBG_SKIP:
                p.memset("dve", gates[ti][:], 0.125, [("gates", ti)])
            elif moe:
                r_ = rt[ti % NSL]
                rtok = ("rt", ti % NSL)
                gtok = ("gates", ti)
                lg = gA[0][:, 0:8]
                for kc in range(8):
                    p.mm(lg, h32[:, kc, :], wr[:, kc, :], kc == 0, kc == 7, [h32tok, "const_wr"], [("gA", 0)])
                p.copy("dve", r_[:, 0:8], lg, [("gA", 0)], [rtok])
                p.add("dve", lambda e, r_=r_: e.max(out=r_[:, 8:16], in_=r_[:, 0:8]), [rtok], [rtok])
                p.ts("dve", r_[:, 16:17], r_[:, 8:9], -1.0, None, ALU.mult, None, [rtok], [rtok])
                p.act(r_[:, 24:32], r_[:, 0:8], AF.Exp, [rtok], [rtok], bias=r_[:, 16:17])
                p.ts("dve", r_[:, 32:40], r_[:, 0:8], r_[:, 9:10], None, ALU.is_ge, None, [rtok], [rtok])
                p.tt("dve", r_[:, 24:32], r_[:, 24:32], r_[:, 32:40], ALU.mult, [rtok], [rtok])
                p.add("dve", lambda e, r_=r_: e.reduce_sum(out=r_[:, 17:18], in_=r_[:, 24:32], axis=AX.X),
                      [rtok], [rtok])
                p.add("dve", lambda e, r_=r_: e.reciprocal(out=r_[:, 18:19], in_=r_[:, 17:18]), [rtok], [rtok])
                p.ts("dve", gates[ti][:], r_[:, 24:32], r_[:, 18:19], None, ALU.mult, None, [rtok], [gtok])
            pgroups.append(p.end_group())
        p.interleave(pgroups, 3)
        hT_all = [("hT", ti) for ti in range(TPP)]
        if DEBUG and ps_i == 0:
            dbg = nc.dram_tensor("dbg_hT", [128, 8, PASS], BF16, kind="ExternalOutput").ap()
            p.dma("sp", dbg, hT[:], reads=hT_all, writes=["dbg_hT"])
            dbg3 = nc.dram_tensor("dbg_hn", [256, D], F32, kind="ExternalOutput").ap()
            for i in range(2):
                p.dma("sp", dbg3[i * 128:(i + 1) * 128, :], hn[i][:], reads=[("hn", i)], writes=[("dbg_hn", i)])
            dbg2 = nc.dram_tensor("dbg_x1", [PASS, D], F32, kind="ExternalOutput").ap()
            for ti in range(TPP):
                p.dma("sp", dbg2[ti * 128:(ti + 1) * 128, :], acc[ti][:], reads=[("acc", ti)], writes=[("dbg_x1", ti)])
        for e_i in range(E):
            for part in parts:
                nch = len(part) * UC
                par = pcount % 2
                pcount += 1
                for ui, u in enumerate(part):
                    ws = ucount % NWS
                    ucount += 1
                    c0 = u * UC * 128
                    wsrc = w_gu[e_i].rearrange("(kc p) f -> p kc f", p=128)
                    p.dma("pool", wg[ws][:], wsrc[:, :, c0:c0 + UC * 128], writes=[("wg", ws)])
                    p.dma("pool", wu[ws][:], wsrc[:, :, DFF + c0:DFF + c0 + UC * 128], writes=[("wu", ws)])
                    p.dma("pool", wd[par][ui][:],
                          w_d[e_i].rearrange("(fc p) c -> p fc c", p=128)[:, u * UC:(u + 1) * UC, :],
                          writes=[("wd", par, ui)])
                    for fl in range(UC):
                        lc = ui * UC + fl
                        for tt_ in range(PASS // 512):
                            gi = gucount % 2
                            gucount += 1
                            ga, ub = gA[gi], uB[gi]
                            gtk, utk = ("gA", gi), ("uB", gi)
                            rhs_tok = hT_all[tt_ * 4:(tt_ + 1) * 4]
                            for kc in range(8):
                                p.mm(ga[:], wg[ws][:, kc, fl * 128:(fl + 1) * 128], hT[:, kc, tt_ * 512:(tt_ + 1) * 512],
                                     kc == 0, kc == 7, [("wg", ws)] + rhs_tok, [gtk])
                            for kc in range(8):
                                p.mm(ub[:], wu[ws][:, kc, fl * 128:(fl + 1) * 128], hT[:, kc, tt_ * 512:(tt_ + 1) * 512],
                                     kc == 0, kc == 7, [("wu", ws)] + rhs_tok, [utk])
                            s_ = sg[gi]
                            p.act(s_[:], ga[:], AF.Silu, [gtk], [("sg", gi)])
                            p.tt("dve", actT[:, lc, tt_ * 512:(tt_ + 1) * 512], ub[:], s_[:], ALU.mult,
                                 [utk, ("sg", gi)], [("actT", lc, tt_)])
                for ti in range(TPP):
                    a = acc[ti]
                    atok = ("acc", ti)
                    for hf in range(2):
                        d_ = dn[dncount % 2]
                        dtok = ("dn", dncount % 2)
                        dncount += 1
                        for lc in range(nch):
                            p.mm(d_[:], actT[:, lc, ti * 128:(ti + 1) * 128],
                                 wd[par][lc // UC][:, lc % UC, hf * 512:(hf + 1) * 512],
                                 lc == 0, lc == nch - 1, [("actT", lc, ti // 4), ("wd", par, lc // UC)], [dtok])
                        sc = gates[ti][:, e_i:e_i + 1] if moe else 1.0
                        rd = [dtok, atok] + ([("gates", ti)] if moe else [])
                        p.stt("dve", a[:, hf * 512:(hf + 1) * 512], d_[:], sc, a[:, hf * 512:(hf + 1) * 512],
                              ALU.mult, ALU.add, rd, [atok])
        for ti in range(TPP):
            tok0 = t0p + ti * 128
            a = acc[ti]
            atok = ("acc", ti)
            if moe and "final" not in DBG_SKIP:
                s_ = st[ti % NSL]
                stok = ("st", ti % NSL)
                o_ = hn[ti % NSL]
                otok = ("hn", ti % NSL)
                p.memset("dve", s_[:, 0:1], 0.0, [stok])
                p.act(junk[:], a[:], AF.Square, [atok, stok], [stok], accum_out=s_[:, 0:1])
                p.ts("dve", s_[:, 1:2], s_[:, 0:1], 1.0 / D, EPS, ALU.mult, ALU.add, [stok], [stok])
                p.tt("pool", s_[:, 3:4], s_[:, 1:2], mhalf[:], ALU.pow, [stok, "c_mhalf"], [stok])
                p.stt("dve", o_[:], a[:], s_[:, 3:4], gfin[:], ALU.mult, ALU.mult, [atok, stok, "const_gf"], [otok])
                p.dma("sp", xout[tok0:tok0 + 128, :], o_[:], reads=[otok], writes=[("xout", ps_i, ti)],
                      key=("hn", ti % NSL))
            else:
                p.dma("sp", xout[tok0:tok0 + 128, :], a[:], reads=[atok], writes=[("xout", ps_i, ti)],
                      key=("acc", ti))
                if hoist:
                    s_ = st[ti % NSL]
                    stok = ("st", ti % NSL)
                    hb = hnb[ti % 2]
                    hbtok = ("hnb", ti % 2)
                    p.memset("dve", s_[:, 0:1], 0.0, [stok])
                    p.act(junk[:], a[:], AF.Square, [atok, stok], [stok], accum_out=s_[:, 0:1])
                    p.ts("dve", s_[:, 1:2], s_[:, 0:1], 1.0 / D, EPS, ALU.mult, ALU.add, [stok], [stok])
                    p.tt("pool", s_[:, 3:4], s_[:, 1:2], mhalf[:], ALU.pow, [stok, "c_mhalf"], [stok])
                    p.stt("dve", hb[:], a[:], s_[:, 3:4], gnx[:], ALU.mult, ALU.mult, [atok, stok, "const_gnx"], [hbtok])
                    p.dma("sp", hnout[tok0:tok0 + 128, :], hb[:], reads=[hbtok], writes=[("hnout", ps_i, ti)],
                          key=("hnb", ti % 2))
                    if ti % 4 == 3:
                        fused["gather_piece"](p, "x2", ps_i * 2 + ti // 4,
                                              [("hnout", ps_i, t_) for t_ in range(ti - 3, ti + 1)])
    p.add("sp", lambda e: e.nop(), reads=[("xout", a_, b_) for a_ in range(NPASS) for b_ in range(TPP)])
    if fused is not None:
        p.finish(barrier=True)
        return None
    p.emit()
    p.close()
    return nc


def build_even_kernel(nchunks=SEQ // 128, fused=None):
    S = nchunks * 128
    NT1 = 386
    if fused is None:
        nc = bass.Bass("TRN2", target_bir_lowering=False)
        pfx = ""
        _dt = lambda name, shape, dt, kind: nc.dram_tensor(name, shape, dt, kind=kind)
    else:
        nc, pfx = fused["nc"], "ev_"
        _dt = lambda name, shape, dt, kind: fused["tensor"](pfx + name, shape, dt, kind)
    class _W:
        def dram_tensor(self, name, shape, dt, kind="Internal"):
            return _dt(name, shape, dt, kind)
    ncd = _W()
    x = ncd.dram_tensor("x", [S, D], F32, kind="ExternalInput").ap()
    g_norm = ncd.dram_tensor("g_norm", [D], F32, kind="ExternalInput").ap()
    wt_d = ncd.dram_tensor("wt", [D, 898], F32, kind="ExternalInput").ap()
    wf_d = ncd.dram_tensor("wf", [D, 256], F32, kind="ExternalInput").ap()
    cw_d = ncd.dram_tensor("cw", [128, 8], F32, kind="ExternalInput").ap()
    cb_d = ncd.dram_tensor("cb", [128, 2], F32, kind="ExternalInput").ap()
    gb_d = ncd.dram_tensor("gb", [2], F32, kind="ExternalInput").ap()
    mlg_d = ncd.dram_tensor("mlg", [128], F32, kind="ExternalInput").ap()
    sgg_d = ncd.dram_tensor("sgg", [128], F32, kind="ExternalInput").ap()
    sgwT_d = ncd.dram_tensor("sgwT", [128, 128], F32, kind="ExternalInput").ap()
    sgb_d = ncd.dram_tensor("sgb", [128, 1], F32, kind="ExternalInput").ap()
    ident_d = ncd.dram_tensor("ident", [128, 128], F32, kind="ExternalInput").ap()
    triu_d = ncd.dram_tensor("triu", [128, 128], F32, kind="ExternalInput").ap()
    negm_d = ncd.dram_tensor("negmask", [128, 128], F32, kind="ExternalInput").ap()
    y = ncd.dram_tensor("y", [S, 256], BF16, kind="ExternalOutput").ap()

    p = Prog(nc, pfx, fused['semstack'] if fused else None)
    ident = p.sbuf("ident_sb", [128, 128], F32)
    triu = p.sbuf("triu_sb", [128, 128], F32)
    negm = p.sbuf("negm_sb", [128, 128], F32)
    ones = p.sbuf("ones_sb", [128, 128], F32)
    mhalf = p.sbuf("mhalf", [128, 1], F32)
    gbc = p.sbuf("gbc", [128, D], F32)
    wt = p.sbuf("wt_sb", [128, 8, 898], BF16)
    wf = p.sbuf("wf_sb", [128, 8, 256], BF16)
    cw = p.sbuf("cw_sb", [128, 8], F32)
    cb = p.sbuf("cb_sb", [128, 2], F32)
    gb = p.sbuf("gb_sb", [128, 4], F32)
    mlg = p.sbuf("mlg_sb", [128, 128], F32)
    sgg = p.sbuf("sgg_sb", [128, 128], F32)
    sgw32 = p.sbuf("sgw32", [128, 128], F32)
    wTm = p.sbuf("wTm", [128, 128], BF16)
    sgb = p.sbuf("sgb_sb", [128, 1], F32)
    Cst = p.sbuf("Cst", [128, 129], F32)
    Cbf = p.sbuf("Cbf", [128, 129], BF16)
    mst = p.sbuf("mst", [128, 1], F32)
    cbuf = p.sbuf("cbuf", [128, 2, 131], F32)
    junk = p.sbuf("junk", [128, D], BF16)
    NS = 6
    NXS = 6
    xs = [p.sbuf("xs%d" % i, [128, D], F32) for i in range(NXS)]
    hn = [p.sbuf("hn%d" % i, [128, D], BF16) for i in range(NS)]
    identb = p.sbuf("identb", [128, 128], BF16)
    hT = [p.sbuf("hT%d" % i, [128, 8, 128], BF16) for i in range(NS)]
    st = [p.sbuf("st%d" % i, [128, 16], F32) for i in range(NS)]
    g = [p.sbuf("g%d" % i, [128, 32], F32) for i in range(NS)]
    vext = [p.sbuf("vext%d" % i, [128, 129], BF16) for i in range(NS)]
    so = [p.sbuf("so%d" % i, [128, 128], F32) for i in range(NS)]
    gu = [p.sbuf("gu%d" % i, [128, 128], F32) for i in range(NS)]
    gv = [p.sbuf("gv%d" % i, [128, 512], F32) for i in range(NS)]
    xh = [p.sbuf("xh%d" % i, [128, 640], F32) for i in range(NS)]
    xp = [p.sbuf("xp%d" % i, [128, 640], F32) for i in range(NS)]
    vsn = [p.sbuf("vsn%d" % i, [128, 128], BF16) for i in range(NS)]
    cc = [p.sbuf("cc%d" % i, [128, 2, 128], F32) for i in range(NS)]
    qk32 = [p.sbuf("qk32_%d" % i, [128, 2, 128], F32) for i in range(NS)]
    qT = [p.sbuf("qT%d" % i, [128, 128], BF16) for i in range(NS)]
    kT = [p.sbuf("kT%d" % i, [128, 128], BF16) for i in range(NS)]
    dg = [p.sbuf("dg%d" % i, [128, 128], F32) for i in range(NS)]
    dg2 = [p.sbuf("dg2_%d" % i, [128, 128], F32) for i in range(NS)]
    tA = [p.sbuf("tA%d" % i, [128, 128], F32) for i in range(NS)]
    tE = [p.sbuf("tE%d" % i, [128, 128], F32) for i in range(NS)]
    Em = [p.sbuf("Em%d" % i, [128, 128], F32) for i in range(NS)]
    sTw = [p.sbuf("sTw%d" % i, [128, 128], BF16) for i in range(NS)]
    intra = [p.sbuf("intra%d" % i, [128, 129], F32) for i in range(NS)]
    numx = [p.sbuf("numx%d" % i, [128, 129], F32) for i in range(NS)]
    gs = [p.sbuf("gs%d" % i, [128, 128], F32) for i in range(NS)]
    kw = [p.sbuf("kw%d" % i, [128, 128], BF16) for i in range(NS)]
    ktok = [p.sbuf("ktok%d" % i, [128, 128], F32) for i in range(NS)]
    ych = [p.sbuf("ych%d" % i, [128, 256], BF16) for i in range(NS)]
    tp = p.psum("tp", [128, 8, 128], F32)
    bA = p.psum("bA", [128, 512], F32)
    bB = p.psum("bB", [128, 512], F32)
    bQ = p.psum("bQ", [128, 512], F32)
    bS = p.psum("bS", [128, 512], F32)
    bT = p.psum("bT", [128, 512], F32)
    bO = p.psum("bO", [128, 512], F32)

    p.dma("sp", ident[:], ident_d, writes=["c_ident"])
    p.dma("sp", triu[:], triu_d, writes=["c_triu"])
    p.dma("sp", negm[:], negm_d, writes=["c_negm"])
    p.dma("sp", gbc[:], g_norm.partition_broadcast(128), writes=["c_gbc"])
    p.dma("pool", wt[:], wt_d.rearrange("(kc p) c -> p kc c", p=128), writes=["c_wt"])
    p.dma("pool", wf[:], wf_d.rearrange("(kc p) c -> p kc c", p=128), writes=["c_wf"])
    p.dma("sp", cw[:], cw_d, writes=["c_cw"])
    p.dma("sp", cb[:], cb_d, writes=["c_cb"])
    p.dma("sp", gb[:, 0:2], gb_d.partition_broadcast(128), writes=["c_gb"])
    p.dma("sp", mlg[:], mlg_d.partition_broadcast(128), writes=["c_mlg"])
    p.dma("sp", sgg[:], sgg_d.partition_broadcast(128), writes=["c_sgg"])
    p.dma("sp", sgw32[:], sgwT_d, writes=["c_sgw32"])
    p.dma("sp", sgb[:], sgb_d, writes=["c_sgb"])
    p.memset("dve", ones[:], 1.0, ["c_ones"])
    p.copy("dve", identb[:], ident[:], ["c_ident"], ["c_identb"])
    p.memset("dve", mhalf[:], -0.5, ["c_mhalf"])
    p.ts("dve", mlg[:], mlg[:], 0.5, None, ALU.mult, None, ["c_mlg"], ["c_mlg"])
    p.ts("dve", cw[:], cw[:], 0.5, None, ALU.mult, None, ["c_cw"], ["c_cw"])
    p.ts("dve", cb[:], cb[:], 0.5, None, ALU.mult, None, ["c_cb"], ["c_cb"])
    p.ts("dve", gb[:, 2:3], gb[:, 1:2], -1.0, None, ALU.mult, None, ["c_gb"], ["c_nbf"])
    p.tt("dve", wTm[:], sgw32[:], triu[:], ALU.mult, ["c_sgw32", "c_triu"], ["c_wTm"])
    p.memset("dve", Cst[:], 0.0, ["Cst"])
    p.memset("dve", Cbf[:], 0.0, ["Cbf"])
    p.memset("dve", mst[:], 0.0, ["mst"])
    p.memset("dve", cbuf[:], 0.0, ["cbuf"])
    for i in range(NS):
        p.memset("dve", vext[i][:, 128:129], 1.0, [("vext1", i)])

    groups = []
    for n in range(nchunks):
        s = n % NS
        x3 = n % NXS
        p.begin_group()
        K_ = lambda name, s=s: (name, s)
        p.dma("sp", xs[x3][:], x[n * 128:(n + 1) * 128, :], writes=[("xs", x3)])
        st_, g_ = st[s], g[s]
        p.memset("dve", st_[:, 0:1], 0.0, [K_("st")])
        p.act(junk[:], xs[x3][:], AF.Square, [("xs", x3), K_("st")], [K_("st")], accum_out=st_[:, 0:1])
        p.ts("dve", st_[:, 1:2], st_[:, 0:1], 1.0 / D, EPS, ALU.mult, ALU.add, [K_("st")], [K_("st")])
        p.tt("pool", st_[:, 3:4], st_[:, 1:2], mhalf[:], ALU.pow, ["c_mhalf"], [K_("st")])
        p.stt("dve", hn[s][:], xs[x3][:], st_[:, 3:4], gbc[:], ALU.mult, ALU.mult,
              [("xs", x3), K_("st"), "c_gbc"], [K_("hn")])
        tpb = tp[:].rearrange("p k t -> p (k t)").bitcast(BF16)[:, 0:1024].rearrange("p (k t) -> p k t", k=8)
        for j in range(8):
            p.tr(tpb[:, j, :], hn[s][:, j * 128:(j + 1) * 128], identb[:], [K_("hn"), "c_identb"], ["tp"])
        p.copy("act", hT[s][:], tpb, [], ["tp", K_("hT")])
        for kc in range(8):
            p.mm(bA[:, 0:NT1], hT[s][:, kc, :], wt[:, kc, 0:NT1], kc == 0, kc == 7, [K_("hT"), "c_wt"], ["bA"])
        for kc in range(8):
            p.mm(bB[:, :], hT[s][:, kc, :], wt[:, kc, NT1:898], kc == 0, kc == 7, [K_("hT"), "c_wt"], ["bB"])
        for jq in range(2):
            for kc in range(8):
                p.mm(bQ[:, jq * 128:(jq + 1) * 128], wf[:, kc, jq * 128:(jq + 1) * 128], hT[s][:, kc, :],
                     kc == 0, kc == 7, [K_("hT"), "c_wf"], ["bQ"])
        p.copy("act", vext[s][:, 0:128], bA[:, 0:128], [("vext1", s)], ["bA", K_("vext")])
        p.act(so[s][:], bA[:, 128:256], AF.Tanh, [], ["bA", K_("so")], scale=0.5)
        p.act(xh[s][:, 0:128], bA[:, 256:384], AF.Copy, [], ["bA", K_("xh")], scale=0.5)
        p.ts("dve", g_[:, 0:1], bA[:, 384:385], gb[:, 0:1], None, ALU.add, None, ["c_gb"], ["bA", K_("g")])
        p.act(g_[:, 1:2], bA[:, 385:386], AF.Exp, ["c_nbf"], ["bA", K_("g")], bias=gb[:, 2:3], scale=-1.0)
        p.act(g_[:, 2:3], g_[:, 1:2], AF.Ln, [], [K_("g")], bias=1.0)
        p.act(xh[s][:, 128:640], bB[:, :], AF.Copy, [], ["bB", K_("xh")], scale=0.5)
        p.tt("pool", xp[s][:], xh[s][:], xh[s][:], ALU.mult, [K_("xh")], [K_("xp")])
        p.ts("pool", xp[s][:], xp[s][:], 4 * 0.044715, 1.0, ALU.mult, ALU.add, [], [K_("xp")])
        p.tt("pool", xp[s][:], xp[s][:], xh[s][:], ALU.mult, [K_("xh")], [K_("xp")])
        p.act(xp[s][:], xp[s][:], AF.Tanh, [], [K_("xp")], scale=2 * 0.7978845608028654)
        p.stt("dve", gu[s][:], xp[s][:, 0:128], 1.0, xh[s][:, 0:128], ALU.add, ALU.mult, [K_("xp"), K_("xh")], [K_("gu")])
        p.stt("dve", gv[s][:], xp[s][:, 128:640], 1.0, xh[s][:, 128:640], ALU.add, ALU.mult, [K_("xp"), K_("xh")], [K_("gv")])
        p.memset("dve", st_[:, 4:5], 0.0, [K_("st")])
        p.act(junk[:, 0:512], gv[s][:], AF.Square, [K_("gv")], [K_("st")], accum_out=st_[:, 4:5])
        p.ts("dve", st_[:, 5:6], st_[:, 4:5], 1.0 / 512, EPS, ALU.mult, ALU.add, [], [K_("st")])
        p.tt("pool", st_[:, 7:8], st_[:, 5:6], mhalf[:], ALU.pow, ["c_mhalf"], [K_("st")])
        p.stt("dve", vsn[s][:], gv[s][:, 0:128], st_[:, 7:8], sgg[:],
              ALU.mult, ALU.mult, [K_("gv"), K_("st"), "c_sgg"], [K_("vsn")])
        p.mm(bT[:, 128:256], wTm[:], vsn[s][:], True, True, ["c_wTm", K_("vsn")], ["bT"])
        p.stt("dve", ych[s][:, 128:256], bT[:, 128:256], sgb[:, 0:1], gu[s][:], ALU.add, ALU.mult,
              ["c_sgb", K_("gu")], ["bT", K_("ych")])
        p.copy("act", cbuf[:, :, 3:131], bQ[:, 0:256].rearrange("p (j t) -> p j t", j=2), [], ["bQ", "cbuf"])
        for j in range(2):
            p.ts("dve", cc[s][:, j, :], cbuf[:, j, 3:131], cw[:, j * 4 + 3:j * 4 + 4], cb[:, j:j + 1], ALU.mult, ALU.add,
                 ["cbuf", "c_cw", "c_cb"], [K_("cc")])
            for tap in (2, 1, 0):
                p.stt("dve", cc[s][:, j, :], cbuf[:, j, tap:tap + 128], cw[:, j * 4 + tap:j * 4 + tap + 1],
                      cc[s][:, j, :], ALU.mult, ALU.add, ["cbuf", "c_cw"], [K_("cc")])
        p.copy("dve", cbuf[:, :, 0:3], cbuf[:, :, 128:131], [], ["cbuf"])
        p.act(qk32[s][:], cc[s][:], AF.Tanh, [K_("cc")], [K_("qk32")])
        p.stt("dve", qk32[s][:], qk32[s][:], 1.0, cc[s][:], ALU.add, ALU.mult, [K_("cc")], [K_("qk32")])
        p.ts("dve", qT[s][:], qk32[s][:, 0, :], 128.0 ** -0.5, None, ALU.mult, None, [K_("qk32")], [K_("qT")])
        p.copy("dve", kT[s][:], qk32[s][:, 1, :], [K_("qk32")], [K_("kT")])
        p.tr(bT[:, 256:384], qk32[s][:, 1, :], ident[:], [K_("qk32"), "c_ident"], ["bT"])
        p.copy("act", ktok[s][:], bT[:, 256:384], [], ["bT", K_("ktok")])
        p.mm(bS[:, 0:1], triu[:], g_[:, 2:3], True, True, ["c_triu", K_("g")], ["bS"])
        p.mm(bS[:, 1:2], ones[:], g_[:, 2:3], True, True, ["c_ones", K_("g")], ["bS"])
        p.copy("dve", g_[:, 3:5], bS[:, 0:2], [], ["bS", K_("g")])
        p.tt("dve", g_[:, 5:6], g_[:, 0:1], g_[:, 3:4], ALU.add, [], [K_("g")])
        p.ts("dve", dg[s][:], ident[:], g_[:, 5:6], None, ALU.mult, None, ["c_ident", K_("g")], [K_("dg")])
        p.mm(bS[:, 128:256], ones[:], dg[s][:], True, True, ["c_ones", K_("dg")], ["bS"])
        p.tt("dve", tA[s][:], bS[:, 128:256], negm[:], ALU.add, ["c_negm"], ["bS", K_("tA")])
        p.add("dve", lambda e, g_=g_, s=s: e.reduce_max(out=g_[:, 6:7], in_=tA[s][:], axis=AX.X), [K_("tA")], [K_("g")])
        p.add("dve", lambda e, g_=g_: e.reduce_max(out=g_[:, 7:8], in_=bS[:, 128:256], axis=AX.X), [], ["bS", K_("g")])
        p.tt("dve", g_[:, 8:9], g_[:, 6:7], mst[:], ALU.max, ["mst"], [K_("g")])
        p.tt("dve", g_[:, 9:10], g_[:, 7:8], mst[:], ALU.max, ["mst"], [K_("g")])
        p.tt("dve", g_[:, 10:11], mst[:], g_[:, 8:9], ALU.subtract, ["mst"], [K_("g")])
        p.tt("dve", g_[:, 11:12], mst[:], g_[:, 9:10], ALU.subtract, ["mst"], [K_("g")])
        p.tt("dve", g_[:, 12:13], g_[:, 5:6], g_[:, 9:10], ALU.subtract, [], [K_("g")])
        p.tt("dve", g_[:, 13:14], g_[:, 3:4], g_[:, 8:9], ALU.subtract, [], [K_("g")])
        p.act(g_[:, 14:18], g_[:, 10:14], AF.Exp, [], [K_("g")])
        p.ts("dve", g_[:, 18:19], g_[:, 8:9], -1.0, None, ALU.mult, None, [], [K_("g")])
        p.ts("dve", dg2[s][:], ident[:], g_[:, 18:19], None, ALU.mult, None, ["c_ident", K_("g")], [K_("dg2")])
        p.mm(bS[:, 256:384], ones[:], dg2[s][:], True, True, ["c_ones", K_("dg2")], ["bS"])
        p.ts("dve", tE[s][:], bS[:, 256:384], g_[:, 5:6], 0.0, ALU.add, ALU.min, [K_("g")], ["bS", K_("tE")])
        p.act(Em[s][:], tE[s][:], AF.Exp, [K_("tE")], [K_("Em")])
        p.tt("pool", Em[s][:], Em[s][:], triu[:], ALU.mult, ["c_triu"], [K_("Em")])
        p.mm(bT[:, 0:128], kT[s][:], qT[s][:], True, True, [K_("kT"), K_("qT")], ["bT"])
        p.tt("dve", sTw[s][:], bT[:, 0:128], Em[s][:], ALU.mult, [K_("Em")], ["bT", K_("sTw")])
        p.mm(bO[:, 0:129], sTw[s][:], vext[s][:], True, True, [K_("sTw"), K_("vext")], ["bO"])
        p.mm(bO[:, 129:258], qT[s][:], Cbf[:], True, True, [K_("qT"), "Cbf"], ["bO"])
        p.copy("act", intra[s][:], bO[:, 0:129], [], ["bO", K_("intra")])
        p.stt("dve", numx[s][:], bO[:, 129:258], g_[:, 14:15], intra[s][:], ALU.mult, ALU.add,
              [K_("g"), K_("intra")], ["bO", K_("numx")])
        p.ts("dve", g_[:, 27:28], numx[s][:, 128:129], -1.0, None, ALU.mult, None, [K_("numx")], [K_("g")])
        p.tt("dve", g_[:, 19:20], g_[:, 27:28], numx[s][:, 128:129], ALU.max, [K_("numx")], [K_("g")])
        p.tt("dve", g_[:, 19:20], g_[:, 19:20], g_[:, 17:18], ALU.max, [], [K_("g")])
        p.add("dve", lambda e, g_=g_: e.reciprocal(out=g_[:, 20:21], in_=g_[:, 19:20]), [], [K_("g")])
        p.memset("dve", st_[:, 8:9], 0.0, [K_("st")])
        p.act(junk[:, 0:128], numx[s][:, 0:128], AF.Square, [K_("numx")], [K_("st")], accum_out=st_[:, 8:9])
        p.tt("dve", g_[:, 21:22], g_[:, 20:21], g_[:, 20:21], ALU.mult, [], [K_("g")])
        p.tt("dve", g_[:, 22:23], g_[:, 21:22], st_[:, 8:9], ALU.mult, [K_("st")], [K_("g")])
        p.ts("dve", g_[:, 23:24], g_[:, 22:23], 1.0 / 128, EPS, ALU.mult, ALU.add, [], [K_("g")])
        p.tt("pool", g_[:, 25:26], g_[:, 23:24], mhalf[:], ALU.pow, ["c_mhalf"], [K_("g")])
        p.tt("dve", g_[:, 26:27], g_[:, 25:26], g_[:, 20:21], ALU.mult, [], [K_("g")])
        p.stt("dve", gs[s][:], so[s][:], 1.0, mlg[:], ALU.add, ALU.mult, [K_("so"), "c_mlg"], [K_("gs")])
        p.stt("dve", ych[s][:, 0:128], numx[s][:, 0:128], g_[:, 26:27], gs[s][:], ALU.mult, ALU.mult,
              [K_("numx"), K_("g"), K_("gs")], [K_("ych")])
        p.ts("dve", kw[s][:], ktok[s][:], g_[:, 16:17], None, ALU.mult, None, [K_("g"), K_("ktok")], [K_("kw")])
        p.mm(bO[:, 258:387], kw[s][:], vext[s][:], True, True, [K_("kw"), K_("vext")], ["bO"])
        p.stt("dve", Cst[:], Cst[:], g_[:, 15:16], bO[:, 258:387], ALU.mult, ALU.add, [K_("g")], ["bO", "Cst"])
        p.copy("act", Cbf[:], Cst[:], ["Cst"], ["Cbf"])
        p.tt("dve", mst[:], g_[:, 9:10], g_[:, 4:5], ALU.subtract, [K_("g")], ["mst"])
        p.dma("sp", y[n * 128:(n + 1) * 128, :], ych[s][:], reads=[K_("ych")], writes=[("y", n)], key=("ych", s))
        if fused is not None and (n + 1) % 16 == 0:
            fused["gather_piece"](p, "y1", n // 16, [("y", m) for m in range(n - 15, n + 1)])
        groups.append(p.end_group())
    p.interleave(groups, PIPE_DEPTH)
    p.add("sp", lambda e: e.nop(), reads=[("y", n) for n in range(nchunks)])
    if fused is not None:
        p.finish(barrier=True)
        return None
    p.emit()
    p.close()
    return nc


def _consts():
    i = np.arange(128)
    triu = (i[:, None] <= i[None, :]).astype(np.float32)
    negm = np.where(i[None, :] <= i[:, None], 0.0, -1e30).astype(np.float32)
    return np.eye(128, dtype=np.float32), triu, negm


def even_core_inputs(inp, b, h, S=SEQ):
    ident, triu, negm = _consts()
    w = inp["even_w_in"][0]
    c = lambda a0: np.arange(a0 + h * 128, a0 + (h + 1) * 128)
    vs_order = np.concatenate([np.arange(2568 + g * 128, 2568 + (g + 1) * 128) for g in [h] + [g for g in range(4) if g != h]])
    tcols = np.concatenate([c(1024), c(1536), c(2056), np.array([2048 + h, 2052 + h]), vs_order])
    fcols = np.concatenate([c(0), c(512)])
    cwf = inp["even_ml_conv_w"][0]
    cw = np.stack([cwf[:, h * 128:(h + 1) * 128].T, cwf[:, 512 + h * 128:512 + (h + 1) * 128].T], axis=1)
    cbf = inp["even_ml_conv_b"][0]
    cb = np.stack([cbf[h * 128:(h + 1) * 128], cbf[512 + h * 128:512 + (h + 1) * 128]], axis=1)
    return {
        "x": np.ascontiguousarray(inp["x"][b, :S]),
        "g_norm": inp["even_norm_mix"][0],
        "wt": np.ascontiguousarray(w[:, tcols]),
        "wf": np.ascontiguousarray(w[:, fcols]),
        "cw": np.ascontiguousarray(cw.reshape(128, 8)),
        "cb": np.ascontiguousarray(cb),
        "gb": np.ascontiguousarray(inp["even_ml_gate_b"][0][:, h]),
        "mlg": np.ascontiguousarray(inp["even_ml_norm_g"][0][h * 128:(h + 1) * 128]),
        "sgg": np.ascontiguousarray(inp["even_sg_norm_g"][0][h * 128:(h + 1) * 128]),
        "sgwT": np.ascontiguousarray(inp["even_sg_w"][0][h].T),
        "sgb": np.ascontiguousarray(inp["even_sg_b"][0][h].reshape(128, 1)),
        "ident": ident, "triu": triu, "negmask": negm,
    }


LAMBDA_INIT = 0.8 - 0.6 * math.exp(-0.3 * 1)
FOX_SCALE = 128.0 ** -0.5
DIFF_SCALE = 64.0 ** -0.5


def build_odd_kernel(nchunks=SEQ // 128, fused=None):
    S = nchunks * 128
    NQB = S // 512
    if fused is None:
        nc = bass.Bass("TRN2", target_bir_lowering=False)
        pfx = ""
        _dt = lambda name, shape, dt, kind: nc.dram_tensor(name, shape, dt, kind=kind)
    else:
        nc, pfx = fused["nc"], "od_"
        _dt = lambda name, shape, dt, kind: fused["tensor"](pfx + name, shape, dt, kind)
    class _W:
        def dram_tensor(self, name, shape, dt, kind="Internal"):
            return _dt(name, shape, dt, kind)
    ncd = _W()
    x = ncd.dram_tensor("x", [S, D], F32 if fused is None else BF16, kind="ExternalInput").ap()
    g_norm = ncd.dram_tensor("g_norm", [D], F32, kind="ExternalInput").ap()
    wf_d = ncd.dram_tensor("wf", [D, 577], F32, kind="ExternalInput").ap()
    wt_d = ncd.dram_tensor("wt", [D, 256], F32, kind="ExternalInput").ap()
    fb_d = ncd.dram_tensor("fb", [1, 1], F32, kind="ExternalInput").ap()
    lam_d = ncd.dram_tensor("lam", [256], F32, kind="ExternalInput").ap()
    dng_d = ncd.dram_tensor("dng", [128], F32, kind="ExternalInput").ap()
    rope_d = ncd.dram_tensor("rope", [nchunks, 128, 2, 256], F32, kind="ExternalInput").ap()
    pt_d = ncd.dram_tensor("ptm", [128, 128], F32, kind="ExternalInput").ap()
    sel_d = ncd.dram_tensor("sel", [128, 3, 65], F32, kind="ExternalInput").ap()
    csc_d = ncd.dram_tensor("cscale", [1, 3], F32, kind="ExternalInput").ap()
    ident_d = ncd.dram_tensor("ident", [128, 128], F32, kind="ExternalInput").ap()
    triu_d = ncd.dram_tensor("triu", [128, 128], F32, kind="ExternalInput").ap()
    y = ncd.dram_tensor("y", [S, 256], BF16, kind="ExternalOutput").ap()

    p = Prog(nc, pfx, fused['semstack'] if fused else None)
    ident = p.sbuf("ident_sb", [128, 128], F32)
    triu32 = p.sbuf("triu32", [128, 128], F32)
    triub = p.sbuf("triub", [128, 128], BF16)
    ptm = p.sbuf("ptm_sb", [128, 128], F32)
    sel32 = p.sbuf("sel32", [128, 3, 65], F32)
    selb = p.sbuf("selb", [128, 3, 65], BF16)
    gbc = p.sbuf("gbc", [128, D], F32)
    wf = p.sbuf("wf_sb", [128, 8, 577], BF16)
    wtk = p.sbuf("wtk_sb", [128, 8, 256], BF16)
    lamt = p.sbuf("lamt", [128, 256], F32)
    lamw = p.sbuf("lamw", [128, 256], F32)
    lams = p.sbuf("lams", [128, 8], F32)
    dng = p.sbuf("dng_sb", [128, 128], F32)
    rowc = p.sbuf("rowc", [65, 16], F32)
    rmax = p.sbuf("rmax", [65, 8], F32)
    rtmp = p.sbuf("rtmp", [65, 8], F32)
    ones32 = p.sbuf("ones32", [65, 128], F32)
    mhalf = p.sbuf("mhalf", [128, 1], F32)
    onesb = p.sbuf("onesb", [65, 128], BF16)
    fqT = p.sbuf("fqT", [128, S], BF16)
    fkT = p.sbuf("fkT", [128, S], BF16)
    dqT = p.sbuf("dqT", [128, S], BF16)
    dkT = p.sbuf("dkT", [128, S], BF16)
    fvx = p.sbuf("fvx", [128, nchunks, 129], BF16)
    dvx = p.sbuf("dvx", [128, nchunks, 129], BF16)
    rF = p.sbuf("rF", [65, S], BF16)
    fcol = p.sbuf("fcol", [128, nchunks], F32)
    fcolc = p.sbuf("fcolc", [128, nchunks], F32)
    negc = p.sbuf("negc", [128, 4], F32)
    junk = p.sbuf("junk", [128, D], BF16)
    NS = 3
    NXS = 3
    if fused is None:
        xs = [p.sbuf("xs%d" % i, [128, D], F32) for i in range(NXS)]
        hn = [p.sbuf("hn%d" % i, [128, D], F32) for i in range(NS)]
    else:
        hnb = [p.sbuf("hnb%d" % i, [128, D], BF16) for i in range(NS)]
        identb = p.sbuf("identb", [128, 128], BF16)
    hT = [p.sbuf("hT%d" % i, [128, 8, 128], BF16) for i in range(NS)]
    st = [p.sbuf("st%d" % i, [128, 8], F32) for i in range(NS)]
    x32f = [p.sbuf("x32f%d" % i, [128, 256], F32) for i in range(NS)]
    x32d = [p.sbuf("x32d%d" % i, [128, 256], F32) for i in range(NS)]
    sqf = [p.sbuf("sqf%d" % i, [128, 256], BF16) for i in range(NS)]
    sqd = [p.sbuf("sqd%d" % i, [128, 256], BF16) for i in range(NS)]
    ropet = [p.sbuf("ropet%d" % i, [128, 2, 256], F32) for i in range(NS)]
    t1 = [p.sbuf("t1_%d" % i, [128, 256], F32) for i in range(NS)]
    t2 = [p.sbuf("t2_%d" % i, [128, 256], F32) for i in range(NS)]
    grow = [p.sbuf("grow%d" % i, [65, 3, 128], F32) for i in range(NS)]
    NPT = 6
    PT = [p.sbuf("PT%d" % i, [128, 512], BF16) for i in range(NPT)]
    yblk = [p.sbuf("yblk%d" % i, [128, 4, 256], BF16) for i in range(2)]
    a0 = p.sbuf("a0", [128, 4, 128], F32)
    rd = [p.sbuf("rd%d" % i, [128, 4], F32) for i in range(2)]
    tmpa = [p.sbuf("tmpa%d" % i, [128, 128], F32) for i in range(2)]
    yd = [p.sbuf("yd%d" % i, [128, 128], F32) for i in range(2)]
    st2 = [p.sbuf("stb%d" % i, [128, 8], F32) for i in range(2)]
    tp = p.psum("tp", [128, 8, 128], F32)
    pb = [p.psum("pb%d" % i, [128, 512], F32) for i in range(6)]
    bFK, bD, bG, bG2, bR, bV = pb
    BK = lambda i: "bank%d" % i

    p.dma("sp", ident[:], ident_d, writes=["c_ident"])
    p.dma("sp", triu32[:], triu_d, writes=["c_triu32"])
    p.dma("sp", ptm[:], pt_d, writes=["c_ptm"])
    p.dma("sp", sel32[:], sel_d, writes=["c_sel32"])
    p.dma("sp", gbc[:], g_norm.partition_broadcast(128), writes=["c_gbc"])
    p.dma("pool", wf[:], wf_d.rearrange("(kc p) c -> p kc c", p=128), writes=["c_wf"])
    p.dma("pool", wtk[:], wt_d.rearrange("(kc p) c -> p kc c", p=128), writes=["c_wtk"])
    p.dma("sp", lamt[:], lam_d.partition_broadcast(128), writes=["c_lamt"])
    p.dma("sp", dng[:], dng_d.partition_broadcast(128), writes=["c_dng"])
    p.memset("dve", rowc[:], 0.0, ["rowc"])
    p.dma("sp", rowc[64:65, 0:1], fb_d, reads=[], writes=["rowc"])
    p.dma("sp", rowc[64:65, 3:6], csc_d, reads=[], writes=["rowc"])
    p.copy("dve", triub[:], triu32[:], ["c_triu32"], ["c_triub"])
    if fused is not None:
        p.copy("dve", identb[:], ident[:], ["c_ident"], ["c_identb"])
    p.copy("dve", selb[:], sel32[:], ["c_sel32"], ["c_selb"])
    p.memset("dve", ones32[:], 1.0, ["c_ones32"])
    p.memset("dve", mhalf[:], -0.5, ["c_mhalf"])
    p.memset("dve", onesb[:], 1.0, ["c_onesb"])
    p.memset("dve", rmax[:], 0.0, ["rmax"])
    p.ts("dve", rowc[64:65, 1:2], rowc[64:65, 0:1], -1.0, None, ALU.mult, None, [], ["rowc"])
    p.ts("dve", dng[:], dng[:], 1.0 - LAMBDA_INIT, None, ALU.mult, None, ["c_dng"], ["c_dng"])
    p.memset("dve", fvx[:, :, 128:129], 1.0, ["fvx1"])
    p.memset("dve", dvx[:, :, 128:129], 1.0, ["dvx1"])
    p.tt("dve", lamw[:, 0:64], lamt[:, 0:64], lamt[:, 64:128], ALU.mult, ["c_lamt"], ["lamw"])
    p.tt("dve", lamw[:, 64:128], lamt[:, 128:192], lamt[:, 192:256], ALU.mult, ["c_lamt"], ["lamw"])
    p.add("dve", lambda e: e.reduce_sum(out=lams[:, 0:1], in_=lamw[:, 0:64], axis=AX.X), ["lamw"], ["lams"])
    p.add("dve", lambda e: e.reduce_sum(out=lams[:, 1:2], in_=lamw[:, 64:128], axis=AX.X), ["lamw"], ["lams"])
    p.act(lams[:, 2:4], lams[:, 0:2], AF.Exp, [], ["lams"])
    p.tt("dve", lams[:, 4:5], lams[:, 3:4], lams[:, 2:3], ALU.subtract, [], ["lams"])
    p.ts("dve", lams[:, 5:6], lams[:, 4:5], -LAMBDA_INIT, None, ALU.add, None, [], ["lams"])

    groups = []
    for n in range(nchunks):
        s = n % NS
        x3 = n % NXS
        p.begin_group()
        K_ = lambda name, s=s: (name, s)
        c0, c1 = n * 128, (n + 1) * 128
        p.dma("sp", ropet[s][:], rope_d[n], writes=[K_("ropet")])
        if fused is not None:
            xr0 = (((c0 % 2048) // 512) * 4 + c0 // 2048) * 512 + c0 % 512
            p.dma("sp", hnb[s][:], x[xr0:xr0 + 128, :], writes=[K_("hn")])
            tpb = tp[:].rearrange("p k t -> p (k t)").bitcast(BF16)[:, 0:1024].rearrange("p (k t) -> p k t", k=8)
            for j in range(8):
                p.tr(tpb[:, j, :], hnb[s][:, j * 128:(j + 1) * 128], identb[:], [K_("hn"), "c_identb"], ["tp"])
            p.copy("act", hT[s][:], tpb, [], ["tp", K_("hT")])
        else:
            p.dma("sp", xs[x3][:], x[c0:c1, :], writes=[("xs", x3)])
        st_ = st[s]
        if fused is None:
            p.memset("dve", st_[:, 0:1], 0.0, [K_("st")])
            p.act(junk[:], xs[x3][:], AF.Square, [("xs", x3)], [K_("st")], accum_out=st_[:, 0:1])
            p.ts("dve", st_[:, 1:2], st_[:, 0:1], 1.0 / D, EPS, ALU.mult, ALU.add, [], [K_("st")])
            p.tt("pool", st_[:, 3:4], st_[:, 1:2], mhalf[:], ALU.pow, ["c_mhalf"], [K_("st")])
            p.stt("dve", hn[s][:], xs[x3][:], st_[:, 3:4], gbc[:], ALU.mult, ALU.mult,
                  [("xs", x3), K_("st"), "c_gbc"], [K_("hn")])
            for j in range(8):
                p.tr(tp[:, j, :], hn[s][:, j * 128:(j + 1) * 128], ident[:], [K_("hn"), "c_ident"], ["tp"])
            p.copy("act", hT[s][:], tp[:], [], ["tp", K_("hT")])
        for j in range(2):
            for kc in range(8):
                p.mm(bFK[:, j * 128:(j + 1) * 128], wf[:, kc, j * 128:(j + 1) * 128], hT[s][:, kc, :],
                     kc == 0, kc == 7, [K_("hT"), "c_wf"], [BK(0)])
        for j in range(2):
            for kc in range(8):
                p.mm(bD[:, j * 128:(j + 1) * 128], wf[:, kc, 256 + j * 128:256 + (j + 1) * 128], hT[s][:, kc, :],
                     kc == 0, kc == 7, [K_("hT"), "c_wf"], [BK(1)])
        for kc in range(8):
            p.mm(bG[0:65, 0:128], wf[:, kc, 512:577], hT[s][:, kc, :], kc == 0, kc == 7, [K_("hT"), "c_wf"], [BK(2)])
        for kc in range(8):
            p.mm(bV[:, 0:256], hT[s][:, kc, :], wtk[:, kc, :], kc == 0, kc == 7, [K_("hT"), "c_wtk"], [BK(5)])
        p.copy("act", x32f[s][:], bFK[:, 0:256], [], [BK(0), K_("x32f")])
        p.copy("dve", fqT[:, c0:c1], x32f[s][:, 0:128], [K_("x32f")], ["fqT"])
        p.copy("dve", fkT[:, c0:c1], x32f[s][:, 128:256], [K_("x32f")], ["fkT"])
        p.tt("pool", sqf[s][:], x32f[s][:], x32f[s][:], ALU.mult, [K_("x32f")], [K_("sqf")])
        p.mm(bG[0:65, 128:384], selb[:, 0, :], sqf[s][:], True, True, ["c_selb", K_("sqf")], [BK(2)])
        p.copy("act", x32d[s][:], bD[:, 0:256], [], [BK(1), K_("x32d")])
        p.tt("pool", sqd[s][:], x32d[s][:], x32d[s][:], ALU.mult, [K_("x32d")], [K_("sqd")])
        p.mm(bG2[0:65, 0:256], selb[:, 1, :], sqd[s][:], True, True, ["c_selb", K_("sqd")], [BK(3)])
        p.mm(bG2[0:65, 256:512], selb[:, 2, :], sqd[s][:], True, True, ["c_selb", K_("sqd")], [BK(3)])
        p.mm(bR[:, 0:256], ptm[:], x32d[s][:], True, True, ["c_ptm", K_("x32d")], [BK(4)])
        p.tt("dve", t1[s][:], x32d[s][:], ropet[s][:, 0, :], ALU.mult, [K_("x32d"), K_("ropet")], [K_("t1")])
        p.tt("dve", t2[s][:], bR[:, 0:256], ropet[s][:, 1, :], ALU.mult, [K_("ropet")], [BK(4), K_("t2")])
        p.tt("pool", dqT[:, c0:c1], t1[s][:, 0:128], t2[s][:, 0:128], ALU.add, [K_("t1"), K_("t2")], ["dqT"])
        p.tt("pool", dkT[:, c0:c1], t1[s][:, 128:256], t2[s][:, 128:256], ALU.add, [K_("t1"), K_("t2")], ["dkT"])
        p.add("dve", lambda e: e.reduce_max(out=rtmp[64:65, 0:2],
                                            in_=bG[64:65, 128:384].rearrange("p (a t) -> p a t", a=2), axis=AX.X),
              [], [BK(2), "rtmp"])
        p.add("dve", lambda e: e.reduce_max(out=rtmp[64:65, 2:6],
                                            in_=bG2[64:65, 0:512].rearrange("p (a t) -> p a t", a=4), axis=AX.X),
              [], [BK(3), "rtmp"])
        p.tt("dve", rmax[64:65, 0:6], rmax[64:65, 0:6], rtmp[64:65, 0:6], ALU.max, ["rtmp"], ["rmax"])
        gr = grow[s]
        p.act(gr[64:65, 0, :], bG[64:65, 0:128], AF.Exp, ["rowc"], [BK(2), K_("grow")], bias=rowc[64:65, 1:2], scale=-1.0)
        p.act(gr[64:65, 1, :], gr[64:65, 0, :], AF.Ln, [], [K_("grow")], bias=1.0)
        p.add("dve", lambda e, gr=gr: e.tensor_tensor_scan(out=gr[64:65, 2, :], data0=gr[64:65, 1, :], data1=gr[64:65, 1, :],
                                                          initial=rowc[64:65, 2:3], op0=ALU.add, op1=ALU.bypass),
              ["rowc"], [K_("grow")], force=True)
        p.copy("dve", rowc[64:65, 2:3], gr[64:65, 2, 127:128], [K_("grow")], ["rowc"])
        p.ts("dve", rF[64:65, c0:c1], gr[64:65, 2, :], -1.0 / FOX_SCALE, None, ALU.mult, None, [K_("grow")], ["rF"])
        p.mm(bV[:, 256:257], gr[64:65, 2, :], ones32[64:65, 0:1], True, True, [K_("grow"), "c_ones32"], [BK(5)])
        p.copy("dve", fcol[:, n:n + 1], bV[:, 256:257], [], [BK(5), "fcol"])
        p.copy("act", fvx[:, n, 0:128], bV[:, 0:128], ["fvx1"], [BK(5), "fvx"])
        p.copy("act", dvx[:, n, 0:128], bV[:, 128:256], ["dvx1"], [BK(5), "dvx"])
        groups.append(p.end_group())
    p.interleave(groups, 3)

    p.tt("dve", rowc[64:65, 6:9], rmax[64:65, 0:6:2], rmax[64:65, 1:6:2], ALU.mult, ["rmax"], ["rowc"])
    p.act(rowc[64:65, 6:9], rowc[64:65, 6:9], AF.Sqrt, [], ["rowc"])
    p.tt("dve", rowc[64:65, 9:12], rowc[64:65, 6:9], rowc[64:65, 3:6], ALU.mult, [], ["rowc"])
    p.mm(bR[:, 0:3], ones32[64:65, :], rowc[64:65, 9:12], True, True, ["c_ones32", "rowc"], [BK(4)])
    p.copy("dve", negc[:, 0:3], bR[:, 0:3], [], [BK(4), "negc"])
    p.ts("dve", fcolc[:], fcol[:], negc[:, 0:1], None, ALU.add, None, ["fcol", "negc"], ["fcolc"])

    accs = [[pb[0], pb[1]], [pb[2], pb[3]]]
    acc_tok = [[BK(0), BK(1)], [BK(2), BK(3)]]
    tpv = tp[:].rearrange("p k t -> p (k t)")
    stb = [pb[4][:, :], pb[5][:, :], tpv[:, 0:512], tpv[:, 512:1024]]
    st_tok = [BK(4), BK(5), "tp_a", "tp_b"]
    NSTB = 4
    LOOKA = 3
    jobc = 0
    kbc = 0
    for qb in range(NQB):
        q0 = qb * 512
        yb = yblk[qb % 2]
        ytok = ("yblk", qb % 2)
        for job in range(3):
            a = jobc % 2
            jobc += 1
            accv = [accs[a][0][:, 0:258].rearrange("p (i c) -> p i c", i=2),
                    accs[a][1][:, 0:258].rearrange("p (i c) -> p i c", i=2)]
            p.memset("dve", accs[a][0][:, 0:258], 0.0, [acc_tok[a][0]])
            p.memset("dve", accs[a][1][:, 0:258], 0.0, [acc_tok[a][1]])
            if job == 0:
                kT_, qT_, vx_, scale_, prow = fkT, fqT, fvx, FOX_SCALE, slice(0, 128)
                ktok, qtok, vtok = "fkT", "fqT", "fvx"
            else:
                g_ = job - 1
                kT_, qT_, vx_, scale_, prow = dkT, dqT, dvx, DIFF_SCALE, slice(g_ * 64, (g_ + 1) * 64)
                ktok, qtok, vtok = "dkT", "dqT", "dvx"
            nkb = 4 * qb + 4
            pend = {}

            def part1(kb):
                nonlocal kbc
                j = kb - 4 * qb
                cs = max(j, 0) * 128
                sb_ = kbc % NSTB
                pt_ = PT[kbc % NPT]
                pttok = ("PT", kbc % NPT)
                kbc += 1
                ST = stb[sb_]
                if job == 0:
                    p.mm(ST[:, cs:512], kT_[prow, kb * 128:(kb + 1) * 128], qT_[prow, q0 + cs:q0 + 512], True, False,
                         [ktok, qtok], [st_tok[sb_]])
                    p.mm(ST[:, cs:512], onesb[64:65, :], rF[64:65, q0 + cs:q0 + 512], False, True,
                         ["c_onesb", "rF"], [st_tok[sb_]])
                    bias_ = fcolc[:, kb:kb + 1]
                    btok = "fcolc"
                else:
                    p.mm(ST[:, cs:512], kT_[prow, kb * 128:(kb + 1) * 128], qT_[prow, q0 + cs:q0 + 512], True, True,
                         [ktok, qtok], [st_tok[sb_]])
                    bias_ = negc[:, job:job + 1]
                    btok = "negc"
                p.act(pt_[:, cs:512], ST[:, cs:512], AF.Exp, [btok], [st_tok[sb_], pttok], bias=bias_, scale=scale_)
                if j >= 0:
                    p.tt("pool", pt_[:, cs:cs + 128], pt_[:, cs:cs + 128], triub[:], ALU.mult, ["c_triub"], [pttok])
                pend[kb] = (pt_, pttok, j)

            def part2(kb):
                pt_, pttok, j = pend.pop(kb)
                for i in range(max(j, 0), 4):
                    p.mm(accv[i // 2][:, i % 2, :], pt_[:, i * 128:(i + 1) * 128], vx_[:, kb, :], False, kb == 4 * qb + i,
                         [pttok, vtok], [acc_tok[a][i // 2]])

            for kb in range(min(LOOKA, nkb)):
                part1(kb)
            for kb in range(nkb):
                if kb + LOOKA < nkb:
                    part1(kb + LOOKA)
                part2(kb)
            r_ = rd[a]
            rtok = ("rd", a)
            for bi in range(2):
                p.add("dve", lambda e, r_=r_, bi=bi, accv=accv: e.reciprocal(out=r_[:, bi * 2:bi * 2 + 2],
                                                                          in_=accv[bi][:, :, 128]),
                      [], [acc_tok[a][bi], rtok])
            for i in range(4):
                src = accv[i // 2][:, i % 2, 0:128]
                atk = acc_tok[a][i // 2]
                if job == 0:
                    p.ts("dve", yb[:, i, 0:128], src, r_[:, i:i + 1], None, ALU.mult, None, [rtok], [atk, ytok])
                elif job == 1:
                    p.ts("dve", a0[:, i, :], src, r_[:, i:i + 1], None, ALU.mult, None, [rtok], [atk, "a0"])
                else:
                    w_ = i % 2
                    p.ts("dve", tmpa[w_][:], src, r_[:, i:i + 1], None, ALU.mult, None, [rtok], [atk, ("tmpa", w_)])
                    p.stt("dve", yd[w_][:], tmpa[w_][:], lams[:, 5:6], a0[:, i, :], ALU.mult, ALU.add,
                          [("tmpa", w_), "lams", "a0"], [("yd", w_)])
                    s2 = st2[w_]
                    s2t = ("st2", w_)
                    p.memset("dve", s2[:, 0:1], 0.0, [s2t])
                    p.act(junk[:, 0:128], yd[w_][:], AF.Square, [("yd", w_)], [s2t], accum_out=s2[:, 0:1])
                    p.ts("dve", s2[:, 1:2], s2[:, 0:1], 1.0 / 128, EPS, ALU.mult, ALU.add, [], [s2t])
                    p.tt("pool", s2[:, 3:4], s2[:, 1:2], mhalf[:], ALU.pow, ["c_mhalf"], [s2t])
                    p.stt("dve", yb[:, i, 128:256], yd[w_][:], s2[:, 3:4], dng[:], ALU.mult, ALU.mult,
                          [("yd", w_), s2t, "c_dng"], [ytok])
        p.dma("sp", y[q0:q0 + 512, :].rearrange("(i p) c -> p i c", p=128), yb[:], reads=[ytok], writes=[("y", qb)],
              key=("yblk", qb % 2))
        if fused is not None and (qb + 1) % 4 == 0:
            fused["gather_piece"](p, "y2", qb // 4, [("y", m) for m in range(qb - 3, qb + 1)])
    p.add("sp", lambda e: e.nop(), reads=[("y", qb) for qb in range(NQB)])
    if fused is not None:
        p.finish(barrier=True)
        return None
    p.emit()
    p.close()
    return nc


def odd_core_inputs(inp, x2b, h, S=SEQ):
    ident, triu, _ = _consts()
    w = inp["odd_w_in"][0]
    c = lambda a0: np.arange(a0 + h * 128, a0 + (h + 1) * 128)
    ffpad = np.zeros((D, 65), np.float32)
    ffpad[:, 64] = w[:, 1536 + h]
    wf = np.concatenate([w[:, c(0)], w[:, c(512)], w[:, c(1540)], w[:, c(2052)], ffpad], axis=1)
    wt = np.concatenate([w[:, c(1024)], w[:, c(2564)]], axis=1)
    nch = S // 128
    half = 8
    inv = (np.float32(500000.0) ** (-np.arange(half, dtype=np.float32) / half)).astype(np.float32)
    ang = np.arange(S, dtype=np.float32)[:, None] * inv[None, :]
    cos = np.cos(ang).astype(np.float32)
    sin = np.sin(ang).astype(np.float32)
    tab = np.zeros((nch, 128, 2, 256), np.float32)
    tab[:, :, 0, :] = 1.0
    ct = cos.reshape(nch, 128, 8).transpose(0, 2, 1)
    stt = sin.reshape(nch, 128, 8).transpose(0, 2, 1)
    for base in (0, 64):
        for hf in (0, 8):
            for qk in (0, 128):
                tab[:, base + hf:base + hf + 8, 0, qk:qk + 128] = ct
                tab[:, base + hf:base + hf + 8, 1, qk:qk + 128] = stt
    ptm = np.zeros((128, 128), np.float32)
    for base in (0, 64):
        for i in range(8):
            ptm[base + i + 8, base + i] = -1.0
            ptm[base + i, base + i + 8] = 1.0
    sel = np.zeros((128, 3, 65), np.float32)
    sel[:, 0, 64] = 1.0
    sel[0:64, 1, 64] = 1.0
    sel[64:128, 2, 64] = 1.0
    csc = (-1.02 * np.array([[FOX_SCALE, DIFF_SCALE, DIFF_SCALE]])).astype(np.float32)
    return {
        "x": None if x2b is None else np.ascontiguousarray(x2b[:S]),
        "g_norm": inp["odd_norm_mix"][0],
        "wf": np.ascontiguousarray(wf), "wt": np.ascontiguousarray(wt),
        "fb": inp["odd_fox_f_b"][0][h].reshape(1, 1).astype(np.float32),
        "lam": np.ascontiguousarray(inp["odd_diff_lambda"][0].reshape(256)),
        "dng": inp["odd_diff_norm_g"][0],
        "rope": tab, "ptm": ptm, "sel": sel, "cscale": csc, "ident": ident, "triu": triu,
    }


_PROGS = {}


def _prog(name, builder):
    if name not in _PROGS:
        _PROGS[name] = builder()
    return _PROGS[name]


def _run(nc, in_maps):
    res = run_bass_kernel_spmd(nc, in_maps, core_ids=list(range(NCORES)))
    return res.results


def _assemble_y(results):
    yfull = np.empty((2, SEQ, D), dtype=ml_dtypes.bfloat16)
    for c in range(NCORES):
        b, h = c // 4, c % 4
        yc = results[c]["y"]
        yfull[b, :, h * 128:(h + 1) * 128] = yc[:, 0:128]
        yfull[b, :, 512 + h * 128:512 + (h + 1) * 128] = yc[:, 128:256]
    yflat = yfull.reshape(2 * SEQ, D)
    return [np.ascontiguousarray(yflat[c * 2048:(c + 1) * 2048].T) for c in range(NCORES)]


def kernel_unfused(**inp):
    inp = {k: np.asarray(v) for k, v in inp.items()}
    ident = np.eye(128, dtype=np.float32)
    x = np.ascontiguousarray(inp["x"], dtype=np.float32)
    nc1 = _prog("even", build_even_kernel)
    r1 = _run(nc1, [even_core_inputs(inp, c // 4, c % 4) for c in range(NCORES)])
    yT = _assemble_y(r1)
    xflat = x.reshape(2 * SEQ, D)
    nc2 = _prog("ffn", lambda: build_tok_kernel("ffn"))
    r2 = _run(nc2, [{"xin": np.ascontiguousarray(xflat[c * 2048:(c + 1) * 2048]), "yT": yT[c],
                     "w_out": inp["even_w_out"][0], "g_norm": inp["even_norm_ffn"][0],
                     "w_gu": inp["ffn_w_gate_up"], "w_d": inp["ffn_w_down"], "ident": ident}
                    for c in range(NCORES)])
    x2 = np.concatenate([r2[c]["xout"] for c in range(NCORES)], axis=0).reshape(2, SEQ, D)
    nc3 = _prog("odd", build_odd_kernel)
    r3 = _run(nc3, [odd_core_inputs(inp, x2[c // 4], c % 4) for c in range(NCORES)])
    yT2 = _assemble_y(r3)
    x2flat = x2.reshape(2 * SEQ, D)
    w_r = np.ascontiguousarray(inp["moe_w_router"][0].reshape(8, 128, 8).transpose(1, 0, 2)).reshape(128, 64)
    nc4 = _prog("moe", lambda: build_tok_kernel("moe"))
    r4 = _run(nc4, [{"xin": np.ascontiguousarray(x2flat[c * 2048:(c + 1) * 2048]), "yT": yT2[c],
                     "w_out": inp["odd_w_out"][0], "g_norm": inp["odd_norm_ffn"][0],
                     "w_gu": inp["moe_w_gate_up"][0], "w_d": inp["moe_w_down"][0], "ident": ident,
                     "w_r": w_r, "g_fin": inp["final_norm"]}
                    for c in range(NCORES)])
    out = np.concatenate([r4[c]["xout"] for c in range(NCORES)], axis=0).reshape(2, SEQ, D)
    return out.astype(np.float32)


RG = [[0, 1, 2, 3], [4, 5, 6, 7]]


def build_fused():
    nc = bass.Bass("TRN2", target_bir_lowering=False)
    ov = {}
    names = {"in": [], "out": []}

    def tensor(name, shape, dt, kind):
        if name in ov:
            return ov[name]
        names["in" if kind == "ExternalInput" else "out"].append(name)
        return nc.dram_tensor(name, shape, dt, kind=kind)

    fused = {"nc": nc, "tensor": tensor, "semstack": "raw"}
    y1 = nc.dram_tensor("i_y1", [SEQ, 256], BF16)
    y1g = nc.dram_tensor("i_y1g", [4 * SEQ, 256], BF16)
    x2s = nc.dram_tensor("i_x2s", [2048, D], F32)
    x2h = nc.dram_tensor("i_x2h", [2048, D], BF16)
    x2g = nc.dram_tensor("i_x2g", [SEQ, D], BF16)
    y2 = nc.dram_tensor("i_y2", [SEQ, 256], BF16)
    y2g = nc.dram_tensor("i_y2g", [4 * SEQ, 256], BF16)

    gspec = {"y1": (y1, y1g, 2048), "x2": (x2h, x2g, 512), "y2": (y2, y2g, 2048)}

    def gather_piece(p, which, j, read_tokens):
        src, dst, rows = gspec[which]
        p.dma_fn("pool", lambda e: e.collective_compute(
            "AllGather", ALU.bypass, replica_groups=RG,
            ins=[src.ap()[j * rows:(j + 1) * rows, :].opt()],
            outs=[dst.ap()[j * 4 * rows:(j + 1) * 4 * rows, :].opt()]),
            reads=read_tokens, writes=[("gath", which, j)], key=("cc", which, j), inc=1)

    fused["gather_piece"] = gather_piece
    ov["ev_y"] = y1
    with nc.cleanup_on_exit():
        build_even_kernel(fused=fused)
    ov["tk0_yT"] = y1g
    ov["tk0_xout"] = x2s
    ov["tk0_hnout"] = x2h
    with nc.cleanup_on_exit():
        build_tok_kernel("ffn", fused=fused)
    ov["od_x"] = x2g
    ov["od_y"] = y2
    with nc.cleanup_on_exit():
        build_odd_kernel(fused=fused)
    ov["tk1_xin"] = x2s
    ov["tk1_yT"] = y2g
    with nc.cleanup_on_exit():
        build_tok_kernel("moe", fused=fused)
    return nc, names


def kernel(**inp):
    inp = {k: np.asarray(v) for k, v in inp.items()}
    ident = np.eye(128, dtype=np.float32)
    x = np.ascontiguousarray(inp["x"], dtype=np.float32)
    xflat = x.reshape(2 * SEQ, D)
    if "fused" not in _PROGS:
        _PROGS["fused"] = build_fused()
    nc, names = _PROGS["fused"]
    w_r = np.ascontiguousarray(inp["moe_w_router"][0].reshape(8, 128, 8).transpose(1, 0, 2)).reshape(128, 64)
    in_maps = []
    for c in range(NCORES):
        b, h = c // 4, c % 4
        m = {}
        for k, v in even_core_inputs(inp, b, h).items():
            m["ev_" + k] = v
        od = odd_core_inputs(inp, None, h)
        for k, v in od.items():
            if k != "x":
                m["od_" + k] = v
        m.update({"tk0_xin": np.ascontiguousarray(xflat[c * 2048:(c + 1) * 2048]),
                  "tk0_w_out": inp["even_w_out"][0], "tk0_g_norm": inp["even_norm_ffn"][0],
                  "tk0_w_gu": inp["ffn_w_gate_up"], "tk0_w_d": inp["ffn_w_down"], "tk0_ident": ident,
                  "tk0_g_next": inp["odd_norm_mix"][0]})
        m.update({"tk1_w_out": inp["odd_w_out"][0], "tk1_g_norm": inp["odd_norm_ffn"][0],
                  "tk1_w_gu": inp["moe_w_gate_up"][0], "tk1_w_d": inp["moe_w_down"][0], "tk1_ident": ident,
                  "tk1_w_r": w_r, "tk1_g_fin": inp["final_norm"]})
        assert set(m.keys()) == set(names["in"]), (set(m.keys()) ^ set(names["in"]))
        in_maps.append(m)
    res = run_bass_kernel_spmd(nc, in_maps, core_ids=list(range(NCORES)))
    out = np.concatenate([res.results[c]["tk1_xout"] for c in range(NCORES)], axis=0).reshape(2, SEQ, D)
    return out.astype(np.float32)
```

```python
from contextlib import ExitStack
import math
import numpy as np
import ml_dtypes
import concourse.bass as bass
import concourse.mybir as mybir
from concourse.bass_utils import run_bass_kernel_spmd

F32 = mybir.dt.float32
BF16 = mybir.dt.bfloat16
AF = mybir.ActivationFunctionType
ALU = mybir.AluOpType
AX = mybir.AxisListType

NCORES = 8
D = 1024
SEQ = 8192
EPS = 1e-6
SEM_EPOCH = 30000
DEBUG = False
E_OVERRIDE = None
PIPE_DEPTH = 5
DBG_SKIP = set()


class Op:
    __slots__ = ("eng", "fn", "deps", "is_dma", "sem", "val", "flag", "key", "force", "inc")

    def __init__(self, eng, fn, is_dma=False, key=None):
        self.eng = eng
        self.fn = fn
        self.deps = []
        self.is_dma = is_dma
        self.sem = None
        self.val = None
        self.flag = False
        self.key = key
        self.force = False
        self.inc = 16


class Prog:
    ENGS = ("pe", "act", "dve", "pool", "sp")

    def __init__(self, nc, pfx="", semstack=None):
        self.nc = nc
        self.pfx = pfx
        self.semstack = semstack
        self.ops = {e: [] for e in self.ENGS}
        self.last_w = {}
        self.readers = {}
        self.stack = ExitStack()
        self.dma_sems = {}
        self.dma_cnt = {}
        self.n_sems = 0
        self._group = None
        self.same_engine_sync = True

    def sbuf(self, name, shape, dtype):
        return self.stack.enter_context(self.nc.sbuf_tensor(self.pfx + name, list(shape), dtype))

    def psum(self, name, shape, dtype):
        return self.stack.enter_context(self.nc.psum_tensor(self.pfx + name, list(shape), dtype))

    def _new_sem(self, name):
        self.n_sems += 1
        if self.semstack == "raw":
            return self.nc.alloc_semaphore(name=self.pfx + name)
        return self.stack.enter_context(self.nc.semaphore(self.pfx + name))

    def finish(self, barrier=True):
        if barrier:
            dmas = [op for e in self.ENGS for op in self.ops[e] if op.is_dma]
            ends = []
            for e in self.ENGS:
                last = [op for op in self.ops[e] if not op.is_dma]
                op = self.add(e, lambda eng: eng.nop(), force=True)
                op.deps = last[-1:] if last else []
                ends.append(op)
            for e in self.ENGS:
                op = self.add(e, lambda eng: eng.nop(), force=True)
                op.deps = [x for x in ends if x.eng != e] + dmas
        self.emit()
        self.close()

    def _track(self, op, reads, writes):
        deps = []
        seen = set()

        def add(d):
            if d is None or id(d) in seen or d is op:
                return
            seen.add(id(d))
            deps.append(d)

        for t in reads:
            add(self.last_w.get(t))
        for t in writes:
            add(self.last_w.get(t))
            for r in self.readers.get(t, ()):
                add(r)
        op.deps = deps
        for t in reads:
            self.readers.setdefault(t, []).append(op)
        for t in writes:
            self.last_w[t] = op
            self.readers[t] = []

    def _commit(self, op, reads, writes):
        if self._group is not None:
            self._group.append((op, tuple(reads), tuple(writes)))
        else:
            self._track(op, reads, writes)
            self.ops[op.eng].append(op)
        return op

    def begin_group(self):
        self._group = []

    def end_group(self):
        g, self._group = self._group, None
        return g

    def interleave(self, groups, depth, window=8):
        if not groups:
            return
        last = []
        lastw = []
        for g in groups:
            d, dw = {}, {}
            for i, (op, reads, writes) in enumerate(g):
                for t in reads:
                    d[t] = i
                for t in writes:
                    d[t] = i
                    dw[t] = i
            last.append(d)
            lastw.append(dw)
        L = max(len(g) for g in groups)
        step = max(1, L // depth)
        pos = [0] * len(groups)
        t = 0
        done = 0
        first_active = 0
        while done < len(groups):
            progressed = False
            for gi in range(first_active, len(groups)):
                if gi * step > t:
                    break
                if pos[gi] >= len(groups[gi]):
                    continue
                op, reads, writes = groups[gi][pos[gi]]
                ok = True
                for gj in range(max(first_active, gi - window), gi):
                    lj = last[gj]
                    lwj = lastw[gj]
                    pj = pos[gj]
                    for tk in writes:
                        if tk in lj and pj <= lj[tk]:
                            ok = False
                            break
                    if ok:
                        for tk in reads:
                            if tk in lwj and pj <= lwj[tk]:
                                ok = False
                                break
                    if not ok:
                        break
                if not ok:
                    continue
                pos[gi] += 1
                progressed = True
                if pos[gi] == len(groups[gi]):
                    done += 1
                self._track(op, reads, writes)
                self.ops[op.eng].append(op)
            while first_active < len(groups) and pos[first_active] >= len(groups[first_active]):
                first_active += 1
            t += 1
            assert progressed or first_active >= len(groups) or first_active * step > t - 1, "interleave stuck"

    def add(self, eng, fn, reads=(), writes=(), force=False):
        op = Op(eng, fn)
        op.force = force
        return self._commit(op, reads, writes)

    def dma(self, q, out, in_, reads=(), writes=(), key=None, **kw):
        if key is None:
            key = ("auto", writes[0] if writes else reads[0])
        op = Op(q, None, is_dma=True, key=key)
        op.fn = lambda e: e.dma_start(out=out, in_=in_, **kw)
        return self._commit(op, reads, writes)

    def dma_fn(self, q, fn, reads=(), writes=(), key=None, inc=16):
        op = Op(q, fn, is_dma=True, key=key)
        op.inc = inc
        return self._commit(op, reads, writes)

    def mm(self, out, lhsT, rhs, start, stop, reads, writes):
        return self.add("pe", lambda e: e.matmul(out, lhsT=lhsT, rhs=rhs, start=start, stop=stop),
                        reads, writes)

    def tr(self, out, in_, ident, reads, writes):
        return self.add("pe", lambda e: e.transpose(out=out, in_=in_, identity=ident), reads, writes)

    def act(self, out, in_, func, reads, writes, eng="act", **kw):
        force = any(not isinstance(v, (int, float)) for k, v in kw.items() if k in ("bias", "scale"))
        return self.add(eng, lambda e: e.activation(out=out, in_=in_, func=func, **kw), reads, writes,
                        force=force)

    def copy(self, eng, out, in_, reads, writes):
        if eng == "act":
            return self.add("act", lambda e: e.copy(out=out, in_=in_), reads, writes)
        return self.add(eng, lambda e: e.tensor_copy(out=out, in_=in_), reads, writes)

    def tt(self, eng, out, in0, in1, op, reads, writes):
        return self.add(eng, lambda e: e.tensor_tensor(out=out, in0=in0, in1=in1, op=op), reads, writes)

    def ts(self, eng, out, in0, s1, s2, op0, op1, reads, writes):
        force = not (isinstance(s1, (int, float)) and (s2 is None or isinstance(s2, (int, float))))
        if op1 is None:
            return self.add(eng, lambda e: e.tensor_scalar(out=out, in0=in0, scalar1=s1, scalar2=None,
                                                           op0=op0), reads, writes, force=force)
        return self.add(eng, lambda e: e.tensor_scalar(out=out, in0=in0, scalar1=s1, scalar2=s2,
                                                       op0=op0, op1=op1), reads, writes, force=force)

    def stt(self, eng, out, in0, scalar, in1, op0, op1, reads, writes):
        force = not isinstance(scalar, (int, float))
        return self.add(eng, lambda e: e.scalar_tensor_tensor(out=out, in0=in0, scalar=scalar, in1=in1,
                                                              op0=op0, op1=op1), reads, writes, force=force)

    def memset(self, eng, ap, val, writes):
        return self.add(eng, lambda e: e.memset(ap, val), (), writes)

    def emit(self):
        nc = self.nc
        for e in self.ENGS:
            for op in self.ops[e]:
                for d in op.deps:
                    if d.is_dma:
                        continue
                    if d.eng != op.eng or op.is_dma or op.force or (self.same_engine_sync and op.eng != "pe"):
                        d.flag = True
        for e in self.ENGS:
            cnt = 0
            sem = None
            for op in self.ops[e]:
                if op.is_dma:
                    k = op.key
                    if k not in self.dma_sems:
                        self.dma_sems[k] = self._new_sem("d%d" % len(self.dma_sems))
                        self.dma_cnt[k] = 0
                    self.dma_cnt[k] += op.inc
                    op.sem = self.dma_sems[k]
                    op.val = self.dma_cnt[k]
                elif op.flag:
                    if sem is None or cnt >= SEM_EPOCH:
                        sem = self._new_sem("e_%s_%d" % (e, self.n_sems))
                        cnt = 0
                    cnt += 1
                    op.sem = sem
                    op.val = cnt
        engmap = {"pe": "tensor", "act": "scalar", "dve": "vector", "pool": "gpsimd", "sp": "sync"}
        with nc.Block() as block:
            for e in self.ENGS:
                ops = self.ops[e]
                if not ops:
                    continue

                def body(eng, ops=ops, e=e):
                    waited = {}
                    for op in ops:
                        need = {}
                        for d in op.deps:
                            if d.sem is None:
                                continue
                            if (not d.is_dma) and d.eng == e and not (
                                    op.is_dma or op.force or (self.same_engine_sync and e != "pe")):
                                continue
                            sid = id(d.sem)
                            if waited.get(sid, 0) >= d.val:
                                continue
                            if sid not in need or need[sid][1] < d.val:
                                need[sid] = (d.sem, d.val)
                        for sid, (s, v) in need.items():
                            eng.wait_ge(s, v)
                            waited[sid] = v
                        ins = op.fn(eng)
                        if op.is_dma:
                            ins.then_inc(op.sem, op.inc)
                        elif op.flag:
                            ins.then_inc(op.sem, 1)

                getattr(block, engmap[e])(body)

    def close(self):
        self.stack.close()


def norm_rows(p, x_ap, xtok, g_bc, hn_ap, hntok, st, sttok, junk, junktok, D_=D):
    p.memset("dve", st[:, 0:1], 0.0, [sttok])
    p.act(junk, x_ap, AF.Square, [xtok, sttok], [junktok, sttok], accum_out=st[:, 0:1])
    p.ts("dve", st[:, 1:2], st[:, 0:1], 1.0 / D_, EPS, ALU.mult, ALU.add, [sttok], [sttok])
    p.act(st[:, 2:3], st[:, 1:2], AF.Sqrt, [sttok], [sttok])
    p.add("dve", lambda e: e.reciprocal(out=st[:, 3:4], in_=st[:, 2:3]), [sttok], [sttok])
    p.stt("dve", hn_ap, x_ap, st[:, 3:4], g_bc, ALU.mult, ALU.mult, [xtok, sttok, "const"], [hntok])


def build_tok_kernel(kind, fused=None):
    moe = kind == "moe"
    NTOK = 2048
    PASS = 1024
    NPASS = NTOK // PASS
    TPP = PASS // 128
    if moe:
        E, DFF = E_OVERRIDE or 8, 3584
    else:
        E, DFF = 1, 2816
    NFC = DFF // 128
    UC = 2
    NU = NFC // UC
    PU = 4
    nparts = (NU + PU - 1) // PU
    base, extra = NU // nparts, NU % nparts
    parts, u0 = [], 0
    for i in range(nparts):
        n_ = base + (1 if i < extra else 0)
        parts.append(list(range(u0, u0 + n_)))
        u0 += n_

    if fused is None:
        nc = bass.Bass("TRN2", target_bir_lowering=False)
        pfx = ""
        _dt = lambda name, shape, dt, kind: nc.dram_tensor(name, shape, dt, kind=kind)
    else:
        nc, pfx = fused["nc"], "tk%d_" % (1 if moe else 0)
        _dt = lambda name, shape, dt, kind: fused["tensor"](pfx + name, shape, dt, kind)
    class _W:
        def dram_tensor(self, name, shape, dt, kind="Internal"):
            return _dt(name, shape, dt, kind)
    ncd = _W()
    xin = ncd.dram_tensor("xin", [NTOK, D], F32, kind="ExternalInput").ap()
    if fused is None:
        yT = ncd.dram_tensor("yT", [D, NTOK], BF16, kind="ExternalInput").ap()
    else:
        yg = ncd.dram_tensor("yT", [4 * SEQ, 256], BF16, kind="ExternalInput").ap()
    w_out = ncd.dram_tensor("w_out", [D, D], F32, kind="ExternalInput").ap()
    g_norm = ncd.dram_tensor("g_norm", [D], F32, kind="ExternalInput").ap()
    w_gu = ncd.dram_tensor("w_gu", [E, D, 2 * DFF], F32, kind="ExternalInput").ap()
    w_d = ncd.dram_tensor("w_d", [E, DFF, D], F32, kind="ExternalInput").ap()
    ident_d = ncd.dram_tensor("ident", [128, 128], F32, kind="ExternalInput").ap()
    if moe:
        w_r = ncd.dram_tensor("w_r", [128, 64], F32, kind="ExternalInput").ap()
        g_fin = ncd.dram_tensor("g_fin", [D], F32, kind="ExternalInput").ap()
    xout = ncd.dram_tensor("xout", [NTOK, D], F32, kind="ExternalOutput").ap()
    hoist = (fused is not None) and not moe
    if hoist:
        g_next = ncd.dram_tensor("g_next", [D], F32, kind="ExternalInput").ap()
        hnout = ncd.dram_tensor("hnout", [NTOK, D], BF16, kind="ExternalOutput").ap()

    p = Prog(nc, pfx, fused['semstack'] if fused else None)
    NSL = 3
    ident = p.sbuf("ident_sb", [128, 128], F32)
    gbc = p.sbuf("gbc", [128, D], F32)
    mhalf = p.sbuf("mhalf", [128, 1], F32)
    wo = p.sbuf("wo", [128, 8, D], BF16)
    acc = [p.sbuf("acc%d" % i, [128, D], F32) for i in range(TPP)]
    hT = p.sbuf("hT", [128, 8, PASS], BF16)
    yTs = p.sbuf("yTs", [128, 8, PASS], BF16)
    if fused is not None:
        identb = p.sbuf("identb", [128, 128], BF16)
        ysb = p.sbuf("ysb", [128, TPP, 4, 256], BF16)
    NCH_PART = max(len(pp) for pp in parts) * UC
    actT = p.sbuf("actT", [128, NCH_PART, PASS], BF16)
    NWS = 3
    wg = [p.sbuf("wg%d" % i, [128, 8, UC * 128], BF16) for i in range(NWS)]
    wu = [p.sbuf("wu%d" % i, [128, 8, UC * 128], BF16) for i in range(NWS)]
    wd = [[p.sbuf("wd%d_%d" % (i, j), [128, UC, D], BF16) for j in range(PU)] for i in range(2)]
    hn = [p.sbuf("hn%d" % i, [128, D], F32) for i in range(NSL)]
    junk = p.sbuf("junk", [128, D], BF16)
    if hoist:
        gnx = p.sbuf("gnx", [128, D], F32)
        hnb = [p.sbuf("hnb%d" % i, [128, D], BF16) for i in range(2)]
    st = [p.sbuf("st%d" % i, [128, 8], F32) for i in range(NSL)]
    sg = [p.sbuf("sg%d" % i, [128, 512], BF16) for i in range(2)]
    if moe:
        gfin = p.sbuf("gfin", [128, D], F32)
        wr = p.sbuf("wr", [128, 8, 8], F32)
        hT32 = [p.sbuf("hT32_%d" % i, [128, 8, 128], F32) for i in range(NSL)]
        gates = [p.sbuf("gates%d" % i, [128, 8], F32) for i in range(TPP)]
        rt = [p.sbuf("rt%d" % i, [128, 40], F32) for i in range(NSL)]
    tp = p.psum("tp", [128, 8, 128], F32)
    dn = [p.psum("dn%d" % i, [128, 512], F32) for i in range(2)]
    gA = [p.psum("gA%d" % i, [128, 512], F32) for i in range(2)]
    uB = [p.psum("uB%d" % i, [128, 512], F32) for i in range(2)]

    p.dma("sp", ident[:], ident_d, writes=["const_id"])
    p.memset("dve", mhalf[:], -0.5, ["c_mhalf"])
    if fused is not None:
        p.copy("dve", identb[:], ident[:], ["const_id"], ["const_idb"])
    p.dma("sp", gbc[:], g_norm.partition_broadcast(128), writes=["const_g"])
    if hoist:
        p.dma("sp", gnx[:], g_next.partition_broadcast(128), writes=["const_gnx"])
    p.dma("pool", wo[:], w_out.rearrange("(kc p) c -> p kc c", p=128), writes=["const_wo"])
    if moe:
        p.dma("sp", gfin[:], g_fin.partition_broadcast(128), writes=["const_gf"])
        p.dma("sp", wr[:], w_r.rearrange("p (kc e) -> p kc e", e=8), writes=["const_wr"])

    ucount = 0
    pcount = 0
    rank_cache = {}
    dncount = 0
    gucount = 0
    for ps_i in range(NPASS):
        t0p = ps_i * PASS
        pgroups = []
        for ti in range(TPP):
            p.begin_group()
            tok0 = t0p + ti * 128
            ytok = "yTs"
            if fused is None:
                if ti == 0:
                    p.dma("sp", yTs[:], yT.rearrange("(kc p) t -> p kc t", p=128)[:, :, t0p:t0p + PASS],
                          writes=[ytok])
            else:
                ytok = ("yTs", ti)
                if ti == 0:
                    for h_ in range(4):
                        def _ld(e, tl=t0p, h_=h_):
                            if "rank" not in rank_cache:
                                rank_cache["rank"] = e.snap(e.partition_id() % 4, min_val=0, max_val=3)
                            rank = rank_cache["rank"]
                            src = yg.rearrange("(k h t) c -> h k t c", k=4, h=4)[h_, bass.ds(rank, 1), tl:tl + PASS]
                            return e.dma_start(out=ysb[:, :, h_, :], in_=src.rearrange("k (i p) c -> (k p) i c", p=128))

                        p.dma_fn("sp", _ld, reads=[], writes=[("ysb", h_)], key=("ysb", h_))
                ys_ = ysb[:, ti]
                tpy = uB[1][:].bitcast(BF16).rearrange("p (k t) -> p k t", k=8)
                for h_ in range(4):
                    for part in range(2):
                        p.tr(tpy[:, part * 4 + h_, :], ys_[:, h_, part * 128:(part + 1) * 128], identb[:],
                             [("ysb", h_), "const_idb"], [("uB", 1)])
                p.copy("act", yTs[:, :, ti * 128:(ti + 1) * 128], tpy, [], [("uB", 1), ytok])
            a = acc[ti]
            atok = ("acc", ti)
            p.dma("sp", a[:], xin[tok0:tok0 + 128, :], writes=[atok], key=("acc", ti))
            for hf in range(2):
                d_ = dn[dncount % 2]
                dtok = ("dn", dncount % 2)
                dncount += 1
                for kc in range(8):
                    p.mm(d_[:], yTs[:, kc, ti * 128:(ti + 1) * 128], wo[:, kc, hf * 512:(hf + 1) * 512],
                         kc == 0, kc == 7, [ytok, "const_wo"], [dtok])
                p.tt("dve", a[:, hf * 512:(hf + 1) * 512], d_[:], a[:, hf * 512:(hf + 1) * 512], ALU.add,
                     [dtok, atok], [atok])
            s_ = st[ti % NSL]
            stok = ("st", ti % NSL)
            h_ = hn[ti % NSL]
            htok = ("hn", ti % NSL)
            p.memset("dve", s_[:, 0:1], 0.0, [stok])
            p.act(h_[:], a[:], AF.Square, [atok, stok], [stok, htok], accum_out=s_[:, 0:1])
            p.ts("dve", s_[:, 1:2], s_[:, 0:1], 1.0 / D, EPS, ALU.mult, ALU.add, [stok], [stok])
            p.tt("pool", s_[:, 3:4], s_[:, 1:2], mhalf[:], ALU.pow, [stok, "c_mhalf"], [stok])
            p.stt("dve", h_[:], a[:], s_[:, 3:4], gbc[:], ALU.mult, ALU.mult, [atok, stok, "const_g"], [htok])
            for j in range(8):
                p.tr(tp[:, j, :], h_[:, j * 128:(j + 1) * 128], ident[:], [htok, "const_id"], ["tp"])
            if not moe:
                p.copy("act", hT[:, :, ti * 128:(ti + 1) * 128], tp[:], ["tp"], [("hT", ti)])
            else:
                h32 = hT32[ti % NSL]
                h32tok = ("hT32", ti % NSL)
                p.copy("act", h32[:], tp[:], ["tp"], [h32tok])
                p.copy("dve", hT[:, :, ti * 128:(ti + 1) * 128], h32[:], [h32tok], [("hT", ti)])
            if moe and "router" in DBG_SKIP:
                p.memset("dve", gates[ti][:], 0.125, [("gates", ti)])
            elif moe:
                r_ = rt[ti % NSL]
                rtok = ("rt", ti % NSL)
                gtok = ("gates", ti)
                lg = gA[0][:, 0:8]
                for kc in range(8):
                    p.mm(lg, h32[:, kc, :], wr[:, kc, :], kc == 0, kc == 7, [h32tok, "const_wr"], [("gA", 0)])
                p.copy("dve", r_[:, 0:8], lg, [("gA", 0)], [rtok])
                p.add("dve", lambda e, r_=r_: e.max(out=r_[:, 8:16], in_=r_[:, 0:8]), [rtok], [rtok])
                p.ts("dve", r_[:, 16:17], r_[:, 8:9], -1.0, None, ALU.mult, None, [rtok], [rtok])
                p.act(r_[:, 24:32], r_[:, 0:8], AF.Exp, [rtok], [rtok], bias=r_[:, 16:17])
                p.ts("dve", r_[:, 32:40], r_[:, 0:8], r_[:, 9:10], None, ALU.is_ge, None, [rtok], [rtok])
                p.tt("dve", r_[:, 24:32], r_[:, 24:32], r_[:, 32:40], ALU.mult, [rtok], [rtok])
                p.add("dve", lambda e, r_=r_: e.reduce_sum(out=r_[:, 17:18], in_=r_[:, 24:32], axis=AX.X),
                      [rtok], [rtok])
                p.add("dve", lambda e, r_=r_: e.reciprocal(out=r_[:, 18:19], in_=r_[:, 17:18]), [rtok], [rtok])
                p.ts("dve", gates[ti][:], r_[:, 24:32], r_[:, 18:19], None, ALU.mult, None, [rtok], [gtok])
            pgroups.append(p.end_group())
        p.interleave(pgroups, 3)
        hT_all = [("hT", ti) for ti in range(TPP)]
        if DEBUG and ps_i == 0:
            dbg = nc.dram_tensor("dbg_hT", [128, 8, PASS], BF16, kind="ExternalOutput").ap()
            p.dma("sp", dbg, hT[:], reads=hT_all, writes=["dbg_hT"])
            dbg3 = nc.dram_tensor("dbg_hn", [256, D], F32, kind="ExternalOutput").ap()
            for i in range(2):
                p.dma("sp", dbg3[i * 128:(i + 1) * 128, :], hn[i][:], reads=[("hn", i)], writes=[("dbg_hn", i)])
            dbg2 = nc.dram_tensor("dbg_x1", [PASS, D], F32, kind="ExternalOutput").ap()
            for ti in range(TPP):
                p.dma("sp", dbg2[ti * 128:(ti + 1) * 128, :], acc[ti][:], reads=[("acc", ti)], writes=[("dbg_x1", ti)])
        for e_i in range(E):
            for part in parts:
                nch = len(part) * UC
                par = pcount % 2
                pcount += 1
                for ui, u in enumerate(part):
                    ws = ucount % NWS
                    ucount += 1
                    c0 = u * UC * 128
                    wsrc = w_gu[e_i].rearrange("(kc p) f -> p kc f", p=128)
                    p.dma("pool", wg[ws][:], wsrc[:, :, c0:c0 + UC * 128], writes=[("wg", ws)])
                    p.dma("pool", wu[ws][:], wsrc[:, :, DFF + c0:DFF + c0 + UC * 128], writes=[("wu", ws)])
                    p.dma("pool", wd[par][ui][:],
                          w_d[e_i].rearrange("(fc p) c -> p fc c", p=128)[:, u * UC:(u + 1) * UC, :],
                          writes=[("wd", par, ui)])
                    for fl in range(UC):
                        lc = ui * UC + fl
                        for tt_ in range(PASS // 512):
                            gi = gucount % 2
                            gucount += 1
                            ga, ub = gA[gi], uB[gi]
                            gtk, utk = ("gA", gi), ("uB", gi)
                            rhs_tok = hT_all[tt_ * 4:(tt_ + 1) * 4]
                            for kc in range(8):
                                p.mm(ga[:], wg[ws][:, kc, fl * 128:(fl + 1) * 128], hT[:, kc, tt_ * 512:(tt_ + 1) * 512],
                                     kc == 0, kc == 7, [("wg", ws)] + rhs_tok, [gtk])
                            for kc in range(8):
                                p.mm(ub[:], wu[ws][:, kc, fl * 128:(fl + 1) * 128], hT[:, kc, tt_ * 512:(tt_ + 1) * 512],
                                     kc == 0, kc == 7, [("wu", ws)] + rhs_tok, [utk])
                            s_ = sg[gi]
                            p.act(s_[:], ga[:], AF.Silu, [gtk], [("sg", gi)])
                            p.tt("dve", actT[:, lc, tt_ * 512:(tt_ + 1) * 512], ub[:], s_[:], ALU.mult,
                                 [utk, ("sg", gi)], [("actT", lc, tt_)])
                for ti in range(TPP):
                    a = acc[ti]
                    atok = ("acc", ti)
                    for hf in range(2):
                        d_ = dn[dncount % 2]
                        dtok = ("dn", dncount % 2)
                        dncount += 1
                        for lc in range(nch):
                            p.mm(d_[:], actT[:, lc, ti * 128:(ti + 1) * 128],
                                 wd[par][lc // UC][:, lc % UC, hf * 512:(hf + 1) * 512],
                                 lc == 0, lc == nch - 1, [("actT", lc, ti // 4), ("wd", par, lc // UC)], [dtok])
                        sc = gates[ti][:, e_i:e_i + 1] if moe else 1.0
                        rd = [dtok, atok] + ([("gates", ti)] if moe else [])
                        p.stt("dve", a[:, hf * 512:(hf + 1) * 512], d_[:], sc, a[:, hf * 512:(hf + 1) * 512],
                              ALU.mult, ALU.add, rd, [atok])
        for ti in range(TPP):
            tok0 = t0p + ti * 128
            a = acc[ti]
            atok = ("acc", ti)
            if moe and "final" not in DBG_SKIP:
                s_ = st[ti % NSL]
                stok = ("st", ti % NSL)
                o_ = hn[ti % NSL]
                otok = ("hn", ti % NSL)
                p.memset("dve", s_[:, 0:1], 0.0, [stok])
                p.act(o_[:], a[:], AF.Square, [atok, stok], [stok, otok], accum_out=s_[:, 0:1])
                p.ts("dve", s_[:, 1:2], s_[:, 0:1], 1.0 / D, EPS, ALU.mult, ALU.add, [stok], [stok])
                p.tt("pool", s_[:, 3:4], s_[:, 1:2], mhalf[:], ALU.pow, [stok, "c_mhalf"], [stok])
                p.stt("dve", o_[:], a[:], s_[:, 3:4], gfin[:], ALU.mult, ALU.mult, [atok, stok, "const_gf"], [otok])
                p.dma("sp", xout[tok0:tok0 + 128, :], o_[:], reads=[otok], writes=[("xout", ps_i, ti)],
                      key=("hn", ti % NSL))
            else:
                p.dma("sp", xout[tok0:tok0 + 128, :], a[:], reads=[atok], writes=[("xout", ps_i, ti)],
                      key=("acc", ti))
                if hoist:
                    s_ = st[ti % NSL]
                    stok = ("st", ti % NSL)
                    hb = hnb[ti % 2]
                    hbtok = ("hnb", ti % 2)
                    p.memset("dve", s_[:, 0:1], 0.0, [stok])
                    p.act(hb[:], a[:], AF.Square, [atok, stok], [stok, hbtok], accum_out=s_[:, 0:1])
                    p.ts("dve", s_[:, 1:2], s_[:, 0:1], 1.0 / D, EPS, ALU.mult, ALU.add, [stok], [stok])
                    p.tt("pool", s_[:, 3:4], s_[:, 1:2], mhalf[:], ALU.pow, [stok, "c_mhalf"], [stok])
                    p.stt("dve", hb[:], a[:], s_[:, 3:4], gnx[:], ALU.mult, ALU.mult, [atok, stok, "const_gnx"], [hbtok])
                    p.dma("sp", hnout[tok0:tok0 + 128, :], hb[:], reads=[hbtok], writes=[("hnout", ps_i, ti)],
                          key=("hnb", ti % 2))
                    if ti % 4 == 3:
                        fused["gather_piece"](p, "x2", ps_i * 2 + ti // 4,
                                              [("hnout", ps_i, t_) for t_ in range(ti - 3, ti + 1)])
    p.add("sp", lambda e: e.nop(), reads=[("xout", a_, b_) for a_ in range(NPASS) for b_ in range(TPP)])
    if fused is not None:
        p.finish(barrier=True)
        return None
    p.emit()
    p.close()
    return nc


def build_even_kernel(nchunks=SEQ // 128, fused=None):
    S = nchunks * 128
    NT1 = 386
    if fused is None:
        nc = bass.Bass("TRN2", target_bir_lowering=False)
        pfx = ""
        _dt = lambda name, shape, dt, kind: nc.dram_tensor(name, shape, dt, kind=kind)
    else:
        nc, pfx = fused["nc"], "ev_"
        _dt = lambda name, shape, dt, kind: fused["tensor"](pfx + name, shape, dt, kind)
    class _W:
        def dram_tensor(self, name, shape, dt, kind="Internal"):
            return _dt(name, shape, dt, kind)
    ncd = _W()
    x = ncd.dram_tensor("x", [S, D], F32, kind="ExternalInput").ap()
    g_norm = ncd.dram_tensor("g_norm", [D], F32, kind="ExternalInput").ap()
    wt_d = ncd.dram_tensor("wt", [D, 898], F32, kind="ExternalInput").ap()
    wf_d = ncd.dram_tensor("wf", [D, 256], F32, kind="ExternalInput").ap()
    cw_d = ncd.dram_tensor("cw", [128, 8], F32, kind="ExternalInput").ap()
    cb_d = ncd.dram_tensor("cb", [128, 2], F32, kind="ExternalInput").ap()
    gb_d = ncd.dram_tensor("gb", [2], F32, kind="ExternalInput").ap()
    mlg_d = ncd.dram_tensor("mlg", [128], F32, kind="ExternalInput").ap()
    sgg_d = ncd.dram_tensor("sgg", [128], F32, kind="ExternalInput").ap()
    sgwT_d = ncd.dram_tensor("sgwT", [128, 128], F32, kind="ExternalInput").ap()
    sgb_d = ncd.dram_tensor("sgb", [128, 1], F32, kind="ExternalInput").ap()
    ident_d = ncd.dram_tensor("ident", [128, 128], F32, kind="ExternalInput").ap()
    triu_d = ncd.dram_tensor("triu", [128, 128], F32, kind="ExternalInput").ap()
    negm_d = ncd.dram_tensor("negmask", [128, 128], F32, kind="ExternalInput").ap()
    y = ncd.dram_tensor("y", [S, 256], BF16, kind="ExternalOutput").ap()

    p = Prog(nc, pfx, fused['semstack'] if fused else None)
    ident = p.sbuf("ident_sb", [128, 128], F32)
    triu = p.sbuf("triu_sb", [128, 128], F32)
    negm = p.sbuf("negm_sb", [128, 128], F32)
    ones = p.sbuf("ones_sb", [128, 128], F32)
    mhalf = p.sbuf("mhalf", [128, 1], F32)
    gbc = p.sbuf("gbc", [128, D], F32)
    wt = p.sbuf("wt_sb", [128, 8, 898], BF16)
    wf = p.sbuf("wf_sb", [128, 8, 256], BF16)
    cw = p.sbuf("cw_sb", [128, 8], F32)
    cb = p.sbuf("cb_sb", [128, 2], F32)
    gb = p.sbuf("gb_sb", [128, 4], F32)
    mlg = p.sbuf("mlg_sb", [128, 128], F32)
    sgg = p.sbuf("sgg_sb", [128, 128], F32)
    sgw32 = p.sbuf("sgw32", [128, 128], F32)
    wTm = p.sbuf("wTm", [128, 128], BF16)
    sgb = p.sbuf("sgb_sb", [128, 1], F32)
    Cst = p.sbuf("Cst", [128, 129], F32)
    Cbf = p.sbuf("Cbf", [128, 129], BF16)
    mst = p.sbuf("mst", [128, 1], F32)
    cbuf = p.sbuf("cbuf", [128, 2, 131], F32)
    junk = p.sbuf("junk", [128, D], BF16)
    NS = 6
    NXS = 6
    xs = [p.sbuf("xs%d" % i, [128, D], F32) for i in range(NXS)]
    hn = [p.sbuf("hn%d" % i, [128, D], BF16) for i in range(NS)]
    identb = p.sbuf("identb", [128, 128], BF16)
    hT = [p.sbuf("hT%d" % i, [128, 8, 128], BF16) for i in range(NS)]
    st = [p.sbuf("st%d" % i, [128, 16], F32) for i in range(NS)]
    g = [p.sbuf("g%d" % i, [128, 32], F32) for i in range(NS)]
    vext = [p.sbuf("vext%d" % i, [128, 129], BF16) for i in range(NS)]
    so = [p.sbuf("so%d" % i, [128, 128], F32) for i in range(NS)]
    gu = [p.sbuf("gu%d" % i, [128, 128], F32) for i in range(NS)]
    gv = [p.sbuf("gv%d" % i, [128, 512], F32) for i in range(NS)]
    xh = [p.sbuf("xh%d" % i, [128, 640], F32) for i in range(NS)]
    xp = [p.sbuf("xp%d" % i, [128, 640], F32) for i in range(NS)]
    vsn = [p.sbuf("vsn%d" % i, [128, 128], BF16) for i in range(NS)]
    cc = [p.sbuf("cc%d" % i, [128, 2, 128], F32) for i in range(NS)]
    qk32 = [p.sbuf("qk32_%d" % i, [128, 2, 128], F32) for i in range(NS)]
    qT = [p.sbuf("qT%d" % i, [128, 128], BF16) for i in range(NS)]
    kT = [p.sbuf("kT%d" % i, [128, 128], BF16) for i in range(NS)]
    dg = [p.sbuf("dg%d" % i, [128, 128], F32) for i in range(NS)]
    dg2 = [p.sbuf("dg2_%d" % i, [128, 128], F32) for i in range(NS)]
    tA = [p.sbuf("tA%d" % i, [128, 128], F32) for i in range(NS)]
    tE = [p.sbuf("tE%d" % i, [128, 128], F32) for i in range(NS)]
    Em = [p.sbuf("Em%d" % i, [128, 128], F32) for i in range(NS)]
    sTw = [p.sbuf("sTw%d" % i, [128, 128], BF16) for i in range(NS)]
    intra = [p.sbuf("intra%d" % i, [128, 129], F32) for i in range(NS)]
    numx = [p.sbuf("numx%d" % i, [128, 129], F32) for i in range(NS)]
    gs = [p.sbuf("gs%d" % i, [128, 128], F32) for i in range(NS)]
    kw = [p.sbuf("kw%d" % i, [128, 128], BF16) for i in range(NS)]
    ktok = [p.sbuf("ktok%d" % i, [128, 128], F32) for i in range(NS)]
    ych = [p.sbuf("ych%d" % i, [128, 256], BF16) for i in range(NS)]
    tp = p.psum("tp", [128, 8, 128], F32)
    bA = p.psum("bA", [128, 512], F32)
    bB = p.psum("bB", [128, 512], F32)
    bQ = p.psum("bQ", [128, 512], F32)
    bS = p.psum("bS", [128, 512], F32)
    bT = p.psum("bT", [128, 512], F32)
    bO = p.psum("bO", [128, 512], F32)

    p.dma("sp", ident[:], ident_d, writes=["c_ident"])
    p.dma("sp", triu[:], triu_d, writes=["c_triu"])
    p.dma("sp", negm[:], negm_d, writes=["c_negm"])
    p.dma("sp", gbc[:], g_norm.partition_broadcast(128), writes=["c_gbc"])
    p.dma("pool", wt[:], wt_d.rearrange("(kc p) c -> p kc c", p=128), writes=["c_wt"])
    p.dma("pool", wf[:], wf_d.rearrange("(kc p) c -> p kc c", p=128), writes=["c_wf"])
    p.dma("sp", cw[:], cw_d, writes=["c_cw"])
    p.dma("sp", cb[:], cb_d, writes=["c_cb"])
    p.dma("sp", gb[:, 0:2], gb_d.partition_broadcast(128), writes=["c_gb"])
    p.dma("sp", mlg[:], mlg_d.partition_broadcast(128), writes=["c_mlg"])
    p.dma("sp", sgg[:], sgg_d.partition_broadcast(128), writes=["c_sgg"])
    p.dma("sp", sgw32[:], sgwT_d, writes=["c_sgw32"])
    p.dma("sp", sgb[:], sgb_d, writes=["c_sgb"])
    p.memset("dve", ones[:], 1.0, ["c_ones"])
    p.copy("dve", identb[:], ident[:], ["c_ident"], ["c_identb"])
    p.memset("dve", mhalf[:], -0.5, ["c_mhalf"])
    p.ts("dve", mlg[:], mlg[:], 0.5, None, ALU.mult, None, ["c_mlg"], ["c_mlg"])
    p.ts("dve", cw[:], cw[:], 0.5, None, ALU.mult, None, ["c_cw"], ["c_cw"])
    p.ts("dve", cb[:], cb[:], 0.5, None, ALU.mult, None, ["c_cb"], ["c_cb"])
    p.ts("dve", gb[:, 2:3], gb[:, 1:2], -1.0, None, ALU.mult, None, ["c_gb"], ["c_nbf"])
    p.tt("dve", wTm[:], sgw32[:], triu[:], ALU.mult, ["c_sgw32", "c_triu"], ["c_wTm"])
    p.memset("dve", Cst[:], 0.0, ["Cst"])
    p.memset("dve", Cbf[:], 0.0, ["Cbf"])
    p.memset("dve", mst[:], 0.0, ["mst"])
    p.memset("dve", cbuf[:], 0.0, ["cbuf"])
    for i in range(NS):
        p.memset("dve", vext[i][:, 128:129], 1.0, [("vext1", i)])

    groups = []
    for n in range(nchunks):
        s = n % NS
        x3 = n % NXS
        p.begin_group()
        K_ = lambda name, s=s: (name, s)
        p.dma("sp", xs[x3][:], x[n * 128:(n + 1) * 128, :], writes=[("xs", x3)])
        st_, g_ = st[s], g[s]
        p.memset("dve", st_[:, 0:1], 0.0, [K_("st")])
        p.act(hn[s][:], xs[x3][:], AF.Square, [("xs", x3), K_("st")], [K_("st"), K_("hn")], accum_out=st_[:, 0:1])
        p.ts("dve", st_[:, 1:2], st_[:, 0:1], 1.0 / D, EPS, ALU.mult, ALU.add, [K_("st")], [K_("st")])
        p.tt("pool", st_[:, 3:4], st_[:, 1:2], mhalf[:], ALU.pow, ["c_mhalf"], [K_("st")])
        p.stt("dve", hn[s][:], xs[x3][:], st_[:, 3:4], gbc[:], ALU.mult, ALU.mult,
              [("xs", x3), K_("st"), "c_gbc"], [K_("hn")])
        tpb = tp[:].rearrange("p k t -> p (k t)").bitcast(BF16)[:, 0:1024].rearrange("p (k t) -> p k t", k=8)
        for j in range(8):
            p.tr(tpb[:, j, :], hn[s][:, j * 128:(j + 1) * 128], identb[:], [K_("hn"), "c_identb"], ["tp"])
        p.copy("act", hT[s][:], tpb, [], ["tp", K_("hT")])
        for kc in range(8):
            p.mm(bA[:, 0:NT1], hT[s][:, kc, :], wt[:, kc, 0:NT1], kc == 0, kc == 7, [K_("hT"), "c_wt"], ["bA"])
        for kc in range(8):
            p.mm(bB[:, :], hT[s][:, kc, :], wt[:, kc, NT1:898], kc == 0, kc == 7, [K_("hT"), "c_wt"], ["bB"])
        for jq in range(2):
            for kc in range(8):
                p.mm(bQ[:, jq * 128:(jq + 1) * 128], wf[:, kc, jq * 128:(jq + 1) * 128], hT[s][:, kc, :],
                     kc == 0, kc == 7, [K_("hT"), "c_wf"], ["bQ"])
        p.copy("act", vext[s][:, 0:128], bA[:, 0:128], [("vext1", s)], ["bA", K_("vext")])
        p.act(so[s][:], bA[:, 128:256], AF.Tanh, [], ["bA", K_("so")], scale=0.5)
        p.act(xh[s][:, 0:128], bA[:, 256:384], AF.Copy, [], ["bA", K_("xh")], scale=0.5)
        p.ts("dve", g_[:, 0:1], bA[:, 384:385], gb[:, 0:1], None, ALU.add, None, ["c_gb"], ["bA", K_("g")])
        p.act(g_[:, 1:2], bA[:, 385:386], AF.Exp, ["c_nbf"], ["bA", K_("g")], bias=gb[:, 2:3], scale=-1.0)
        p.act(g_[:, 2:3], g_[:, 1:2], AF.Ln, [], [K_("g")], bias=1.0)
        p.act(xh[s][:, 128:640], bB[:, :], AF.Copy, [], ["bB", K_("xh")], scale=0.5)
        p.tt("pool", xp[s][:], xh[s][:], xh[s][:], ALU.mult, [K_("xh")], [K_("xp")])
        p.ts("pool", xp[s][:], xp[s][:], 4 * 0.044715, 1.0, ALU.mult, ALU.add, [], [K_("xp")])
        p.tt("pool", xp[s][:], xp[s][:], xh[s][:], ALU.mult, [K_("xh")], [K_("xp")])
        p.act(xp[s][:], xp[s][:], AF.Tanh, [], [K_("xp")], scale=2 * 0.7978845608028654)
        p.stt("dve", gu[s][:], xp[s][:, 0:128], 1.0, xh[s][:, 0:128], ALU.add, ALU.mult, [K_("xp"), K_("xh")], [K_("gu")])
        p.stt("dve", gv[s][:], xp[s][:, 128:640], 1.0, xh[s][:, 128:640], ALU.add, ALU.mult, [K_("xp"), K_("xh")], [K_("gv")])
        p.memset("dve", st_[:, 4:5], 0.0, [K_("st")])
        p.act(xp[s][:, 0:512], gv[s][:], AF.Square, [K_("gv")], [K_("st"), K_("xp")], accum_out=st_[:, 4:5])
        p.ts("dve", st_[:, 5:6], st_[:, 4:5], 1.0 / 512, EPS, ALU.mult, ALU.add, [], [K_("st")])
        p.tt("pool", st_[:, 7:8], st_[:, 5:6], mhalf[:], ALU.pow, ["c_mhalf"], [K_("st")])
        p.stt("dve", vsn[s][:], gv[s][:, 0:128], st_[:, 7:8], sgg[:],
              ALU.mult, ALU.mult, [K_("gv"), K_("st"), "c_sgg"], [K_("vsn")])
        p.mm(bT[:, 128:256], wTm[:], vsn[s][:], True, True, ["c_wTm", K_("vsn")], ["bT"])
        p.stt("dve", ych[s][:, 128:256], bT[:, 128:256], sgb[:, 0:1], gu[s][:], ALU.add, ALU.mult,
              ["c_sgb", K_("gu")], ["bT", K_("ych")])
        p.copy("act", cbuf[:, :, 3:131], bQ[:, 0:256].rearrange("p (j t) -> p j t", j=2), [], ["bQ", "cbuf"])
        for j in range(2):
            p.ts("dve", cc[s][:, j, :], cbuf[:, j, 3:131], cw[:, j * 4 + 3:j * 4 + 4], cb[:, j:j + 1], ALU.mult, ALU.add,
                 ["cbuf", "c_cw", "c_cb"], [K_("cc")])
            for tap in (2, 1, 0):
                p.stt("dve", cc[s][:, j, :], cbuf[:, j, tap:tap + 128], cw[:, j * 4 + tap:j * 4 + tap + 1],
                      cc[s][:, j, :], ALU.mult, ALU.add, ["cbuf", "c_cw"], [K_("cc")])
        p.copy("dve", cbuf[:, :, 0:3], cbuf[:, :, 128:131], [], ["cbuf"])
        p.act(qk32[s][:], cc[s][:], AF.Tanh, [K_("cc")], [K_("qk32")])
        p.stt("dve", qk32[s][:], qk32[s][:], 1.0, cc[s][:], ALU.add, ALU.mult, [K_("cc")], [K_("qk32")])
        p.ts("dve", qT[s][:], qk32[s][:, 0, :], 128.0 ** -0.5, None, ALU.mult, None, [K_("qk32")], [K_("qT")])
        p.copy("dve", kT[s][:], qk32[s][:, 1, :], [K_("qk32")], [K_("kT")])
        p.tr(bT[:, 256:384], qk32[s][:, 1, :], ident[:], [K_("qk32"), "c_ident"], ["bT"])
        p.copy("act", ktok[s][:], bT[:, 256:384], [], ["bT", K_("ktok")])
        p.mm(bS[:, 0:1], triu[:], g_[:, 2:3], True, True, ["c_triu", K_("g")], ["bS"])
        p.mm(bS[:, 1:2], ones[:], g_[:, 2:3], True, True, ["c_ones", K_("g")], ["bS"])
        p.copy("dve", g_[:, 3:5], bS[:, 0:2], [], ["bS", K_("g")])
        p.tt("dve", g_[:, 5:6], g_[:, 0:1], g_[:, 3:4], ALU.add, [], [K_("g")])
        p.ts("dve", dg[s][:], ident[:], g_[:, 5:6], None, ALU.mult, None, ["c_ident", K_("g")], [K_("dg")])
        p.mm(bS[:, 128:256], ones[:], dg[s][:], True, True, ["c_ones", K_("dg")], ["bS"])
        p.tt("dve", tA[s][:], bS[:, 128:256], negm[:], ALU.add, ["c_negm"], ["bS", K_("tA")])
        p.add("dve", lambda e, g_=g_, s=s: e.reduce_max(out=g_[:, 6:7], in_=tA[s][:], axis=AX.X), [K_("tA")], [K_("g")])
        p.add("dve", lambda e, g_=g_: e.reduce_max(out=g_[:, 7:8], in_=bS[:, 128:256], axis=AX.X), [], ["bS", K_("g")])
        p.tt("dve", g_[:, 8:9], g_[:, 6:7], mst[:], ALU.max, ["mst"], [K_("g")])
        p.tt("dve", g_[:, 9:10], g_[:, 7:8], mst[:], ALU.max, ["mst"], [K_("g")])
        p.tt("dve", g_[:, 10:11], mst[:], g_[:, 8:9], ALU.subtract, ["mst"], [K_("g")])
        p.tt("dve", g_[:, 11:12], mst[:], g_[:, 9:10], ALU.subtract, ["mst"], [K_("g")])
        p.tt("dve", g_[:, 12:13], g_[:, 5:6], g_[:, 9:10], ALU.subtract, [], [K_("g")])
        p.tt("dve", g_[:, 13:14], g_[:, 3:4], g_[:, 8:9], ALU.subtract, [], [K_("g")])
        p.act(g_[:, 14:18], g_[:, 10:14], AF.Exp, [], [K_("g")])
        p.ts("dve", g_[:, 18:19], g_[:, 8:9], -1.0, None, ALU.mult, None, [], [K_("g")])
        p.ts("dve", dg2[s][:], ident[:], g_[:, 18:19], None, ALU.mult, None, ["c_ident", K_("g")], [K_("dg2")])
        p.mm(bS[:, 256:384], ones[:], dg2[s][:], True, True, ["c_ones", K_("dg2")], ["bS"])
        p.ts("dve", tE[s][:], bS[:, 256:384], g_[:, 5:6], 0.0, ALU.add, ALU.min, [K_("g")], ["bS", K_("tE")])
        p.act(Em[s][:], tE[s][:], AF.Exp, [K_("tE")], [K_("Em")])
        p.tt("pool", Em[s][:], Em[s][:], triu[:], ALU.mult, ["c_triu"], [K_("Em")])
        p.mm(bT[:, 0:128], kT[s][:], qT[s][:], True, True, [K_("kT"), K_("qT")], ["bT"])
        p.tt("dve", sTw[s][:], bT[:, 0:128], Em[s][:], ALU.mult, [K_("Em")], ["bT", K_("sTw")])
        p.mm(bO[:, 0:129], sTw[s][:], vext[s][:], True, True, [K_("sTw"), K_("vext")], ["bO"])
        p.mm(bO[:, 129:258], qT[s][:], Cbf[:], True, True, [K_("qT"), "Cbf"], ["bO"])
        p.copy("act", intra[s][:], bO[:, 0:129], [], ["bO", K_("intra")])
        p.stt("dve", numx[s][:], bO[:, 129:258], g_[:, 14:15], intra[s][:], ALU.mult, ALU.add,
              [K_("g"), K_("intra")], ["bO", K_("numx")])
        p.ts("dve", g_[:, 27:28], numx[s][:, 128:129], -1.0, None, ALU.mult, None, [K_("numx")], [K_("g")])
        p.tt("dve", g_[:, 19:20], g_[:, 27:28], numx[s][:, 128:129], ALU.max, [K_("numx")], [K_("g")])
        p.tt("dve", g_[:, 19:20], g_[:, 19:20], g_[:, 17:18], ALU.max, [], [K_("g")])
        p.add("dve", lambda e, g_=g_: e.reciprocal(out=g_[:, 20:21], in_=g_[:, 19:20]), [], [K_("g")])
        p.memset("dve", st_[:, 8:9], 0.0, [K_("st")])
        p.act(tA[s][:], numx[s][:, 0:128], AF.Square, [K_("numx")], [K_("st"), K_("tA")], accum_out=st_[:, 8:9])
        p.tt("dve", g_[:, 21:22], g_[:, 20:21], g_[:, 20:21], ALU.mult, [], [K_("g")])
        p.tt("dve", g_[:, 22:23], g_[:, 21:22], st_[:, 8:9], ALU.mult, [K_("st")], [K_("g")])
        p.ts("dve", g_[:, 23:24], g_[:, 22:23], 1.0 / 128, EPS, ALU.mult, ALU.add, [], [K_("g")])
        p.tt("pool", g_[:, 25:26], g_[:, 23:24], mhalf[:], ALU.pow, ["c_mhalf"], [K_("g")])
        p.tt("dve", g_[:, 26:27], g_[:, 25:26], g_[:, 20:21], ALU.mult, [], [K_("g")])
        p.stt("dve", gs[s][:], so[s][:], 1.0, mlg[:], ALU.add, ALU.mult, [K_("so"), "c_mlg"], [K_("gs")])
        p.stt("dve", ych[s][:, 0:128], numx[s][:, 0:128], g_[:, 26:27], gs[s][:], ALU.mult, ALU.mult,
              [K_("numx"), K_("g"), K_("gs")], [K_("ych")])
        p.ts("dve", kw[s][:], ktok[s][:], g_[:, 16:17], None, ALU.mult, None, [K_("g"), K_("ktok")], [K_("kw")])
        p.mm(bO[:, 258:387], kw[s][:], vext[s][:], True, True, [K_("kw"), K_("vext")], ["bO"])
        p.stt("dve", Cst[:], Cst[:], g_[:, 15:16], bO[:, 258:387], ALU.mult, ALU.add, [K_("g")], ["bO", "Cst"])
        p.copy("act", Cbf[:], Cst[:], ["Cst"], ["Cbf"])
        p.tt("dve", mst[:], g_[:, 9:10], g_[:, 4:5], ALU.subtract, [K_("g")], ["mst"])
        p.dma("sp", y[n * 128:(n + 1) * 128, :], ych[s][:], reads=[K_("ych")], writes=[("y", n)], key=("ych", s))
        if fused is not None and (n + 1) % 16 == 0:
            fused["gather_piece"](p, "y1", n // 16, [("y", m) for m in range(n - 15, n + 1)])
        groups.append(p.end_group())
    p.interleave(groups, PIPE_DEPTH)
    p.add("sp", lambda e: e.nop(), reads=[("y", n) for n in range(nchunks)])
    if fused is not None:
        p.finish(barrier=True)
        return None
    p.emit()
    p.close()
    return nc


def _consts():
    i = np.arange(128)
    triu = (i[:, None] <= i[None, :]).astype(np.float32)
    negm = np.where(i[None, :] <= i[:, None], 0.0, -1e30).astype(np.float32)
    return np.eye(128, dtype=np.float32), triu, negm


def even_core_inputs(inp, b, h, S=SEQ):
    ident, triu, negm = _consts()
    w = inp["even_w_in"][0]
    c = lambda a0: np.arange(a0 + h * 128, a0 + (h + 1) * 128)
    vs_order = np.concatenate([np.arange(2568 + g * 128, 2568 + (g + 1) * 128) for g in [h] + [g for g in range(4) if g != h]])
    tcols = np.concatenate([c(1024), c(1536), c(2056), np.array([2048 + h, 2052 + h]), vs_order])
    fcols = np.concatenate([c(0), c(512)])
    cwf = inp["even_ml_conv_w"][0]
    cw = np.stack([cwf[:, h * 128:(h + 1) * 128].T, cwf[:, 512 + h * 128:512 + (h + 1) * 128].T], axis=1)
    cbf = inp["even_ml_conv_b"][0]
    cb = np.stack([cbf[h * 128:(h + 1) * 128], cbf[512 + h * 128:512 + (h + 1) * 128]], axis=1)
    return {
        "x": np.ascontiguousarray(inp["x"][b, :S]),
        "g_norm": inp["even_norm_mix"][0],
        "wt": np.ascontiguousarray(w[:, tcols]),
        "wf": np.ascontiguousarray(w[:, fcols]),
        "cw": np.ascontiguousarray(cw.reshape(128, 8)),
        "cb": np.ascontiguousarray(cb),
        "gb": np.ascontiguousarray(inp["even_ml_gate_b"][0][:, h]),
        "mlg": np.ascontiguousarray(inp["even_ml_norm_g"][0][h * 128:(h + 1) * 128]),
        "sgg": np.ascontiguousarray(inp["even_sg_norm_g"][0][h * 128:(h + 1) * 128]),
        "sgwT": np.ascontiguousarray(inp["even_sg_w"][0][h].T),
        "sgb": np.ascontiguousarray(inp["even_sg_b"][0][h].reshape(128, 1)),
        "ident": ident, "triu": triu, "negmask": negm,
    }


LAMBDA_INIT = 0.8 - 0.6 * math.exp(-0.3 * 1)
FOX_SCALE = 128.0 ** -0.5
DIFF_SCALE = 64.0 ** -0.5


def build_odd_kernel(nchunks=SEQ // 128, fused=None):
    S = nchunks * 128
    NQB = S // 512
    if fused is None:
        nc = bass.Bass("TRN2", target_bir_lowering=False)
        pfx = ""
        _dt = lambda name, shape, dt, kind: nc.dram_tensor(name, shape, dt, kind=kind)
    else:
        nc, pfx = fused["nc"], "od_"
        _dt = lambda name, shape, dt, kind: fused["tensor"](pfx + name, shape, dt, kind)
    class _W:
        def dram_tensor(self, name, shape, dt, kind="Internal"):
            return _dt(name, shape, dt, kind)
    ncd = _W()
    x = ncd.dram_tensor("x", [S, D], F32 if fused is None else BF16, kind="ExternalInput").ap()
    g_norm = ncd.dram_tensor("g_norm", [D], F32, kind="ExternalInput").ap()
    wf_d = ncd.dram_tensor("wf", [D, 577], F32, kind="ExternalInput").ap()
    wt_d = ncd.dram_tensor("wt", [D, 256], F32, kind="ExternalInput").ap()
    fb_d = ncd.dram_tensor("fb", [1, 1], F32, kind="ExternalInput").ap()
    lam_d = ncd.dram_tensor("lam", [256], F32, kind="ExternalInput").ap()
    dng_d = ncd.dram_tensor("dng", [128], F32, kind="ExternalInput").ap()
    rope_d = ncd.dram_tensor("rope", [nchunks, 128, 2, 256], F32, kind="ExternalInput").ap()
    pt_d = ncd.dram_tensor("ptm", [128, 128], F32, kind="ExternalInput").ap()
    sel_d = ncd.dram_tensor("sel", [128, 3, 65], F32, kind="ExternalInput").ap()
    csc_d = ncd.dram_tensor("cscale", [1, 3], F32, kind="ExternalInput").ap()
    ident_d = ncd.dram_tensor("ident", [128, 128], F32, kind="ExternalInput").ap()
    triu_d = ncd.dram_tensor("triu", [128, 128], F32, kind="ExternalInput").ap()
    y = ncd.dram_tensor("y", [S, 256], BF16, kind="ExternalOutput").ap()

    p = Prog(nc, pfx, fused['semstack'] if fused else None)
    ident = p.sbuf("ident_sb", [128, 128], F32)
    triu32 = p.sbuf("triu32", [128, 128], F32)
    triub = p.sbuf("triub", [128, 128], BF16)
    ptm = p.sbuf("ptm_sb", [128, 128], F32)
    sel32 = p.sbuf("sel32", [128, 3, 65], F32)
    selb = p.sbuf("selb", [128, 3, 65], BF16)
    gbc = p.sbuf("gbc", [128, D], F32)
    wf = p.sbuf("wf_sb", [128, 8, 577], BF16)
    wtk = p.sbuf("wtk_sb", [128, 8, 256], BF16)
    lamt = p.sbuf("lamt", [128, 256], F32)
    lamw = p.sbuf("lamw", [128, 256], F32)
    lams = p.sbuf("lams", [128, 8], F32)
    dng = p.sbuf("dng_sb", [128, 128], F32)
    rowc = p.sbuf("rowc", [65, 16], F32)
    rmax = p.sbuf("rmax", [65, 8], F32)
    rtmp = p.sbuf("rtmp", [65, 8], F32)
    ones32 = p.sbuf("ones32", [65, 128], F32)
    mhalf = p.sbuf("mhalf", [128, 1], F32)
    onesb = p.sbuf("onesb", [65, 128], BF16)
    fqT = p.sbuf("fqT", [128, S], BF16)
    fkT = p.sbuf("fkT", [128, S], BF16)
    dqT = p.sbuf("dqT", [128, S], BF16)
    dkT = p.sbuf("dkT", [128, S], BF16)
    fvx = p.sbuf("fvx", [128, nchunks, 129], BF16)
    dvx = p.sbuf("dvx", [128, nchunks, 129], BF16)
    rF = p.sbuf("rF", [65, S], BF16)
    fcol = p.sbuf("fcol", [128, nchunks], F32)
    fcolc = p.sbuf("fcolc", [128, nchunks], F32)
    negc = p.sbuf("negc", [128, 4], F32)
    junk = p.sbuf("junk", [128, D], BF16)
    NS = 3
    NXS = 3
    if fused is None:
        xs = [p.sbuf("xs%d" % i, [128, D], F32) for i in range(NXS)]
        hn = [p.sbuf("hn%d" % i, [128, D], F32) for i in range(NS)]
    else:
        hnb = [p.sbuf("hnb%d" % i, [128, D], BF16) for i in range(NS)]
        identb = p.sbuf("identb", [128, 128], BF16)
    hT = [p.sbuf("hT%d" % i, [128, 8, 128], BF16) for i in range(NS)]
    st = [p.sbuf("st%d" % i, [128, 8], F32) for i in range(NS)]
    x32f = [p.sbuf("x32f%d" % i, [128, 256], F32) for i in range(NS)]
    x32d = [p.sbuf("x32d%d" % i, [128, 256], F32) for i in range(NS)]
    sqf = [p.sbuf("sqf%d" % i, [128, 256], BF16) for i in range(NS)]
    sqd = [p.sbuf("sqd%d" % i, [128, 256], BF16) for i in range(NS)]
    ropet = [p.sbuf("ropet%d" % i, [128, 2, 256], F32) for i in range(NS)]
    t1 = [p.sbuf("t1_%d" % i, [128, 256], F32) for i in range(NS)]
    t2 = [p.sbuf("t2_%d" % i, [128, 256], F32) for i in range(NS)]
    grow = [p.sbuf("grow%d" % i, [65, 3, 128], F32) for i in range(NS)]
    NPT = 6
    PT = [p.sbuf("PT%d" % i, [128, 512], BF16) for i in range(NPT)]
    yblk = [p.sbuf("yblk%d" % i, [128, 4, 256], BF16) for i in range(2)]
    a0 = p.sbuf("a0", [128, 4, 128], F32)
    rd = [p.sbuf("rd%d" % i, [128, 4], F32) for i in range(2)]
    tmpa = [p.sbuf("tmpa%d" % i, [128, 128], F32) for i in range(2)]
    yd = [p.sbuf("yd%d" % i, [128, 128], F32) for i in range(2)]
    st2 = [p.sbuf("stb%d" % i, [128, 8], F32) for i in range(2)]
    tp = p.psum("tp", [128, 8, 128], F32)
    pb = [p.psum("pb%d" % i, [128, 512], F32) for i in range(6)]
    bFK, bD, bG, bG2, bR, bV = pb
    BK = lambda i: "bank%d" % i

    p.dma("sp", ident[:], ident_d, writes=["c_ident"])
    p.dma("sp", triu32[:], triu_d, writes=["c_triu32"])
    p.dma("sp", ptm[:], pt_d, writes=["c_ptm"])
    p.dma("sp", sel32[:], sel_d, writes=["c_sel32"])
    p.dma("sp", gbc[:], g_norm.partition_broadcast(128), writes=["c_gbc"])
    p.dma("pool", wf[:], wf_d.rearrange("(kc p) c -> p kc c", p=128), writes=["c_wf"])
    p.dma("pool", wtk[:], wt_d.rearrange("(kc p) c -> p kc c", p=128), writes=["c_wtk"])
    p.dma("sp", lamt[:], lam_d.partition_broadcast(128), writes=["c_lamt"])
    p.dma("sp", dng[:], dng_d.partition_broadcast(128), writes=["c_dng"])
    p.memset("dve", rowc[:], 0.0, ["rowc"])
    p.dma("sp", rowc[64:65, 0:1], fb_d, reads=[], writes=["rowc"])
    p.dma("sp", rowc[64:65, 3:6], csc_d, reads=[], writes=["rowc"])
    p.copy("dve", triub[:], triu32[:], ["c_triu32"], ["c_triub"])
    if fused is not None:
        p.copy("dve", identb[:], ident[:], ["c_ident"], ["c_identb"])
    p.copy("dve", selb[:], sel32[:], ["c_sel32"], ["c_selb"])
    p.memset("dve", ones32[:], 1.0, ["c_ones32"])
    p.memset("dve", mhalf[:], -0.5, ["c_mhalf"])
    p.memset("dve", onesb[:], 1.0, ["c_onesb"])
    p.memset("dve", rmax[:], 0.0, ["rmax"])
    p.ts("dve", rowc[64:65, 1:2], rowc[64:65, 0:1], -1.0, None, ALU.mult, None, [], ["rowc"])
    p.ts("dve", dng[:], dng[:], 1.0 - LAMBDA_INIT, None, ALU.mult, None, ["c_dng"], ["c_dng"])
    p.memset("dve", fvx[:, :, 128:129], 1.0, ["fvx1"])
    p.memset("dve", dvx[:, :, 128:129], 1.0, ["dvx1"])
    p.tt("dve", lamw[:, 0:64], lamt[:, 0:64], lamt[:, 64:128], ALU.mult, ["c_lamt"], ["lamw"])
    p.tt("dve", lamw[:, 64:128], lamt[:, 128:192], lamt[:, 192:256], ALU.mult, ["c_lamt"], ["lamw"])
    p.add("dve", lambda e: e.reduce_sum(out=lams[:, 0:1], in_=lamw[:, 0:64], axis=AX.X), ["lamw"], ["lams"])
    p.add("dve", lambda e: e.reduce_sum(out=lams[:, 1:2], in_=lamw[:, 64:128], axis=AX.X), ["lamw"], ["lams"])
    p.act(lams[:, 2:4], lams[:, 0:2], AF.Exp, [], ["lams"])
    p.tt("dve", lams[:, 4:5], lams[:, 3:4], lams[:, 2:3], ALU.subtract, [], ["lams"])
    p.ts("dve", lams[:, 5:6], lams[:, 4:5], -LAMBDA_INIT, None, ALU.add, None, [], ["lams"])

    groups = []
    for n in range(nchunks):
        s = n % NS
        x3 = n % NXS
        p.begin_group()
        K_ = lambda name, s=s: (name, s)
        c0, c1 = n * 128, (n + 1) * 128
        p.dma("sp", ropet[s][:], rope_d[n], writes=[K_("ropet")])
        if fused is not None:
            xr0 = (((c0 % 2048) // 512) * 4 + c0 // 2048) * 512 + c0 % 512
            p.dma("sp", hnb[s][:], x[xr0:xr0 + 128, :], writes=[K_("hn")])
            tpb = tp[:].rearrange("p k t -> p (k t)").bitcast(BF16)[:, 0:1024].rearrange("p (k t) -> p k t", k=8)
            for j in range(8):
                p.tr(tpb[:, j, :], hnb[s][:, j * 128:(j + 1) * 128], identb[:], [K_("hn"), "c_identb"], ["tp"])
            p.copy("act", hT[s][:], tpb, [], ["tp", K_("hT")])
        else:
            p.dma("sp", xs[x3][:], x[c0:c1, :], writes=[("xs", x3)])
        st_ = st[s]
        if fused is None:
            p.memset("dve", st_[:, 0:1], 0.0, [K_("st")])
            p.act(hn[s][:], xs[x3][:], AF.Square, [("xs", x3)], [K_("st"), K_("hn")], accum_out=st_[:, 0:1])
            p.ts("dve", st_[:, 1:2], st_[:, 0:1], 1.0 / D, EPS, ALU.mult, ALU.add, [], [K_("st")])
            p.tt("pool", st_[:, 3:4], st_[:, 1:2], mhalf[:], ALU.pow, ["c_mhalf"], [K_("st")])
            p.stt("dve", hn[s][:], xs[x3][:], st_[:, 3:4], gbc[:], ALU.mult, ALU.mult,
                  [("xs", x3), K_("st"), "c_gbc"], [K_("hn")])
            for j in range(8):
                p.tr(tp[:, j, :], hn[s][:, j * 128:(j + 1) * 128], ident[:], [K_("hn"), "c_ident"], ["tp"])
            p.copy("act", hT[s][:], tp[:], [], ["tp", K_("hT")])
        for j in range(2):
            for kc in range(8):
                p.mm(bFK[:, j * 128:(j + 1) * 128], wf[:, kc, j * 128:(j + 1) * 128], hT[s][:, kc, :],
                     kc == 0, kc == 7, [K_("hT"), "c_wf"], [BK(0)])
        for j in range(2):
            for kc in range(8):
                p.mm(bD[:, j * 128:(j + 1) * 128], wf[:, kc, 256 + j * 128:256 + (j + 1) * 128], hT[s][:, kc, :],
                     kc == 0, kc == 7, [K_("hT"), "c_wf"], [BK(1)])
        for kc in range(8):
            p.mm(bG[0:65, 0:128], wf[:, kc, 512:577], hT[s][:, kc, :], kc == 0, kc == 7, [K_("hT"), "c_wf"], [BK(2)])
        for kc in range(8):
            p.mm(bV[:, 0:256], hT[s][:, kc, :], wtk[:, kc, :], kc == 0, kc == 7, [K_("hT"), "c_wtk"], [BK(5)])
        p.copy("act", x32f[s][:], bFK[:, 0:256], [], [BK(0), K_("x32f")])
        p.copy("dve", fqT[:, c0:c1], x32f[s][:, 0:128], [K_("x32f")], ["fqT"])
        p.copy("dve", fkT[:, c0:c1], x32f[s][:, 128:256], [K_("x32f")], ["fkT"])
        p.tt("pool", sqf[s][:], x32f[s][:], x32f[s][:], ALU.mult, [K_("x32f")], [K_("sqf")])
        p.mm(bG[0:65, 128:384], selb[:, 0, :], sqf[s][:], True, True, ["c_selb", K_("sqf")], [BK(2)])
        p.copy("act", x32d[s][:], bD[:, 0:256], [], [BK(1), K_("x32d")])
        p.tt("pool", sqd[s][:], x32d[s][:], x32d[s][:], ALU.mult, [K_("x32d")], [K_("sqd")])
        p.mm(bG2[0:65, 0:256], selb[:, 1, :], sqd[s][:], True, True, ["c_selb", K_("sqd")], [BK(3)])
        p.mm(bG2[0:65, 256:512], selb[:, 2, :], sqd[s][:], True, True, ["c_selb", K_("sqd")], [BK(3)])
        p.mm(bR[:, 0:256], ptm[:], x32d[s][:], True, True, ["c_ptm", K_("x32d")], [BK(4)])
        p.tt("dve", t1[s][:], x32d[s][:], ropet[s][:, 0, :], ALU.mult, [K_("x32d"), K_("ropet")], [K_("t1")])
        p.tt("dve", t2[s][:], bR[:, 0:256], ropet[s][:, 1, :], ALU.mult, [K_("ropet")], [BK(4), K_("t2")])
        p.tt("pool", dqT[:, c0:c1], t1[s][:, 0:128], t2[s][:, 0:128], ALU.add, [K_("t1"), K_("t2")], ["dqT"])
        p.tt("pool", dkT[:, c0:c1], t1[s][:, 128:256], t2[s][:, 128:256], ALU.add, [K_("t1"), K_("t2")], ["dkT"])
        p.add("dve", lambda e: e.reduce_max(out=rtmp[64:65, 0:2],
                                            in_=bG[64:65, 128:384].rearrange("p (a t) -> p a t", a=2), axis=AX.X),
              [], [BK(2), "rtmp"])
        p.add("dve", lambda e: e.reduce_max(out=rtmp[64:65, 2:6],
                                            in_=bG2[64:65, 0:512].rearrange("p (a t) -> p a t", a=4), axis=AX.X),
              [], [BK(3), "rtmp"])
        p.tt("dve", rmax[64:65, 0:6], rmax[64:65, 0:6], rtmp[64:65, 0:6], ALU.max, ["rtmp"], ["rmax"])
        gr = grow[s]
        p.act(gr[64:65, 0, :], bG[64:65, 0:128], AF.Exp, ["rowc"], [BK(2), K_("grow")], bias=rowc[64:65, 1:2], scale=-1.0)
        p.act(gr[64:65, 1, :], gr[64:65, 0, :], AF.Ln, [], [K_("grow")], bias=1.0)
        p.add("dve", lambda e, gr=gr: e.tensor_tensor_scan(out=gr[64:65, 2, :], data0=gr[64:65, 1, :], data1=gr[64:65, 1, :],
                                                          initial=rowc[64:65, 2:3], op0=ALU.add, op1=ALU.bypass),
              ["rowc"], [K_("grow")], force=True)
        p.copy("dve", rowc[64:65, 2:3], gr[64:65, 2, 127:128], [K_("grow")], ["rowc"])
        p.ts("dve", rF[64:65, c0:c1], gr[64:65, 2, :], -1.0 / FOX_SCALE, None, ALU.mult, None, [K_("grow")], ["rF"])
        p.mm(bV[:, 256:257], gr[64:65, 2, :], ones32[64:65, 0:1], True, True, [K_("grow"), "c_ones32"], [BK(5)])
        p.copy("dve", fcol[:, n:n + 1], bV[:, 256:257], [], [BK(5), "fcol"])
        p.copy("act", fvx[:, n, 0:128], bV[:, 0:128], ["fvx1"], [BK(5), "fvx"])
        p.copy("act", dvx[:, n, 0:128], bV[:, 128:256], ["dvx1"], [BK(5), "dvx"])
        groups.append(p.end_group())
    p.interleave(groups, 3)

    p.tt("dve", rowc[64:65, 6:9], rmax[64:65, 0:6:2], rmax[64:65, 1:6:2], ALU.mult, ["rmax"], ["rowc"])
    p.act(rowc[64:65, 6:9], rowc[64:65, 6:9], AF.Sqrt, [], ["rowc"])
    p.tt("dve", rowc[64:65, 9:12], rowc[64:65, 6:9], rowc[64:65, 3:6], ALU.mult, [], ["rowc"])
    p.mm(bR[:, 0:3], ones32[64:65, :], rowc[64:65, 9:12], True, True, ["c_ones32", "rowc"], [BK(4)])
    p.copy("dve", negc[:, 0:3], bR[:, 0:3], [], [BK(4), "negc"])
    p.ts("dve", fcolc[:], fcol[:], negc[:, 0:1], None, ALU.add, None, ["fcol", "negc"], ["fcolc"])

    accs = [[pb[0], pb[1]], [pb[2], pb[3]]]
    acc_tok = [[BK(0), BK(1)], [BK(2), BK(3)]]
    tpv = tp[:].rearrange("p k t -> p (k t)")
    stb = [pb[4][:, :], pb[5][:, :], tpv[:, 0:512], tpv[:, 512:1024]]
    st_tok = [BK(4), BK(5), "tp_a", "tp_b"]
    NSTB = 4
    LOOKA = 3
    jobc = 0
    kbc = 0
    for qb in range(NQB):
        q0 = qb * 512
        yb = yblk[qb % 2]
        ytok = ("yblk", qb % 2)
        for job in range(3):
            a = jobc % 2
            jobc += 1
            accv = [accs[a][0][:, 0:258].rearrange("p (i c) -> p i c", i=2),
                    accs[a][1][:, 0:258].rearrange("p (i c) -> p i c", i=2)]
            p.memset("dve", accs[a][0][:, 0:258], 0.0, [acc_tok[a][0]])
            p.memset("dve", accs[a][1][:, 0:258], 0.0, [acc_tok[a][1]])
            if job == 0:
                kT_, qT_, vx_, scale_, prow = fkT, fqT, fvx, FOX_SCALE, slice(0, 128)
                ktok, qtok, vtok = "fkT", "fqT", "fvx"
            else:
                g_ = job - 1
                kT_, qT_, vx_, scale_, prow = dkT, dqT, dvx, DIFF_SCALE, slice(g_ * 64, (g_ + 1) * 64)
                ktok, qtok, vtok = "dkT", "dqT", "dvx"
            nkb = 4 * qb + 4
            pend = {}

            def part1(kb):
                nonlocal kbc
                j = kb - 4 * qb
                cs = max(j, 0) * 128
                sb_ = kbc % NSTB
                pt_ = PT[kbc % NPT]
                pttok = ("PT", kbc % NPT)
                kbc += 1
                ST = stb[sb_]
                if job == 0:
                    p.mm(ST[:, cs:512], kT_[prow, kb * 128:(kb + 1) * 128], qT_[prow, q0 + cs:q0 + 512], True, False,
                         [ktok, qtok], [st_tok[sb_]])
                    p.mm(ST[:, cs:512], onesb[64:65, :], rF[64:65, q0 + cs:q0 + 512], False, True,
                         ["c_onesb", "rF"], [st_tok[sb_]])
                    bias_ = fcolc[:, kb:kb + 1]
                    btok = "fcolc"
                else:
                    p.mm(ST[:, cs:512], kT_[prow, kb * 128:(kb + 1) * 128], qT_[prow, q0 + cs:q0 + 512], True, True,
                         [ktok, qtok], [st_tok[sb_]])
                    bias_ = negc[:, job:job + 1]
                    btok = "negc"
                p.act(pt_[:, cs:512], ST[:, cs:512], AF.Exp, [btok], [st_tok[sb_], pttok], bias=bias_, scale=scale_)
                if j >= 0:
                    p.tt("pool", pt_[:, cs:cs + 128], pt_[:, cs:cs + 128], triub[:], ALU.mult, ["c_triub"], [pttok])
                pend[kb] = (pt_, pttok, j)

            def part2(kb):
                pt_, pttok, j = pend.pop(kb)
                for i in range(max(j, 0), 4):
                    p.mm(accv[i // 2][:, i % 2, :], pt_[:, i * 128:(i + 1) * 128], vx_[:, kb, :], False, kb == 4 * qb + i,
                         [pttok, vtok], [acc_tok[a][i // 2]])

            for kb in range(min(LOOKA, nkb)):
                part1(kb)
            for kb in range(nkb):
                if kb + LOOKA < nkb:
                    part1(kb + LOOKA)
                part2(kb)
            r_ = rd[a]
            rtok = ("rd", a)
            for bi in range(2):
                p.add("dve", lambda e, r_=r_, bi=bi, accv=accv: e.reciprocal(out=r_[:, bi * 2:bi * 2 + 2],
                                                                          in_=accv[bi][:, :, 128]),
                      [], [acc_tok[a][bi], rtok])
            for i in range(4):
                src = accv[i // 2][:, i % 2, 0:128]
                atk = acc_tok[a][i // 2]
                if job == 0:
                    p.ts("dve", yb[:, i, 0:128], src, r_[:, i:i + 1], None, ALU.mult, None, [rtok], [atk, ytok])
                elif job == 1:
                    p.ts("dve", a0[:, i, :], src, r_[:, i:i + 1], None, ALU.mult, None, [rtok], [atk, "a0"])
                else:
                    w_ = i % 2
                    p.ts("dve", tmpa[w_][:], src, r_[:, i:i + 1], None, ALU.mult, None, [rtok], [atk, ("tmpa", w_)])
                    p.stt("dve", yd[w_][:], tmpa[w_][:], lams[:, 5:6], a0[:, i, :], ALU.mult, ALU.add,
                          [("tmpa", w_), "lams", "a0"], [("yd", w_)])
                    s2 = st2[w_]
                    s2t = ("st2", w_)
                    p.memset("dve", s2[:, 0:1], 0.0, [s2t])
                    p.act(tmpa[w_][:], yd[w_][:], AF.Square, [("yd", w_)], [s2t, ("tmpa", w_)], accum_out=s2[:, 0:1])
                    p.ts("dve", s2[:, 1:2], s2[:, 0:1], 1.0 / 128, EPS, ALU.mult, ALU.add, [], [s2t])
                    p.tt("pool", s2[:, 3:4], s2[:, 1:2], mhalf[:], ALU.pow, ["c_mhalf"], [s2t])
                    p.stt("dve", yb[:, i, 128:256], yd[w_][:], s2[:, 3:4], dng[:], ALU.mult, ALU.mult,
                          [("yd", w_), s2t, "c_dng"], [ytok])
        p.dma("sp", y[q0:q0 + 512, :].rearrange("(i p) c -> p i c", p=128), yb[:], reads=[ytok], writes=[("y", qb)],
              key=("yblk", qb % 2))
        if fused is not None and (qb + 1) % 4 == 0:
            fused["gather_piece"](p, "y2", qb // 4, [("y", m) for m in range(qb - 3, qb + 1)])
    p.add("sp", lambda e: e.nop(), reads=[("y", qb) for qb in range(NQB)])
    if fused is not None:
        p.finish(barrier=True)
        return None
    p.emit()
    p.close()
    return nc


def odd_core_inputs(inp, x2b, h, S=SEQ):
    ident, triu, _ = _consts()
    w = inp["odd_w_in"][0]
    c = lambda a0: np.arange(a0 + h * 128, a0 + (h + 1) * 128)
    ffpad = np.zeros((D, 65), np.float32)
    ffpad[:, 64] = w[:, 1536 + h]
    wf = np.concatenate([w[:, c(0)], w[:, c(512)], w[:, c(1540)], w[:, c(2052)], ffpad], axis=1)
    wt = np.concatenate([w[:, c(1024)], w[:, c(2564)]], axis=1)
    nch = S // 128
    half = 8
    inv = (np.float32(500000.0) ** (-np.arange(half, dtype=np.float32) / half)).astype(np.float32)
    ang = np.arange(S, dtype=np.float32)[:, None] * inv[None, :]
    cos = np.cos(ang).astype(np.float32)
    sin = np.sin(ang).astype(np.float32)
    tab = np.zeros((nch, 128, 2, 256), np.float32)
    tab[:, :, 0, :] = 1.0
    ct = cos.reshape(nch, 128, 8).transpose(0, 2, 1)
    stt = sin.reshape(nch, 128, 8).transpose(0, 2, 1)
    for base in (0, 64):
        for hf in (0, 8):
            for qk in (0, 128):
                tab[:, base + hf:base + hf + 8, 0, qk:qk + 128] = ct
                tab[:, base + hf:base + hf + 8, 1, qk:qk + 128] = stt
    ptm = np.zeros((128, 128), np.float32)
    for base in (0, 64):
        for i in range(8):
            ptm[base + i + 8, base + i] = -1.0
            ptm[base + i, base + i + 8] = 1.0
    sel = np.zeros((128, 3, 65), np.float32)
    sel[:, 0, 64] = 1.0
    sel[0:64, 1, 64] = 1.0
    sel[64:128, 2, 64] = 1.0
    csc = (-1.02 * np.array([[FOX_SCALE, DIFF_SCALE, DIFF_SCALE]])).astype(np.float32)
    return {
        "x": None if x2b is None else np.ascontiguousarray(x2b[:S]),
        "g_norm": inp["odd_norm_mix"][0],
        "wf": np.ascontiguousarray(wf), "wt": np.ascontiguousarray(wt),
        "fb": inp["odd_fox_f_b"][0][h].reshape(1, 1).astype(np.float32),
        "lam": np.ascontiguousarray(inp["odd_diff_lambda"][0].reshape(256)),
        "dng": inp["odd_diff_norm_g"][0],
        "rope": tab, "ptm": ptm, "sel": sel, "cscale": csc, "ident": ident, "triu": triu,
    }


_PROGS = {}


def _prog(name, builder):
    if name not in _PROGS:
        _PROGS[name] = builder()
    return _PROGS[name]


def _run(nc, in_maps):
    res = run_bass_kernel_spmd(nc, in_maps, core_ids=list(range(NCORES)))
    return res.results


def _assemble_y(results):
    yfull = np.empty((2, SEQ, D), dtype=ml_dtypes.bfloat16)
    for c in range(NCORES):
        b, h = c // 4, c % 4
        yc = results[c]["y"]
        yfull[b, :, h * 128:(h + 1) * 128] = yc[:, 0:128]
        yfull[b, :, 512 + h * 128:512 + (h + 1) * 128] = yc[:, 128:256]
    yflat = yfull.reshape(2 * SEQ, D)
    return [np.ascontiguousarray(yflat[c * 2048:(c + 1) * 2048].T) for c in range(NCORES)]


def kernel_unfused(**inp):
    inp = {k: np.asarray(v) for k, v in inp.items()}
    ident = np.eye(128, dtype=np.float32)
    x = np.ascontiguousarray(inp["x"], dtype=np.float32)
    nc1 = _prog("even", build_even_kernel)
    r1 = _run(nc1, [even_core_inputs(inp, c // 4, c % 4) for c in range(NCORES)])
    yT = _assemble_y(r1)
    xflat = x.reshape(2 * SEQ, D)
    nc2 = _prog("ffn", lambda: build_tok_kernel("ffn"))
    r2 = _run(nc2, [{"xin": np.ascontiguousarray(xflat[c * 2048:(c + 1) * 2048]), "yT": yT[c],
                     "w_out": inp["even_w_out"][0], "g_norm": inp["even_norm_ffn"][0],
                     "w_gu": inp["ffn_w_gate_up"], "w_d": inp["ffn_w_down"], "ident": ident}
                    for c in range(NCORES)])
    x2 = np.concatenate([r2[c]["xout"] for c in range(NCORES)], axis=0).reshape(2, SEQ, D)
    nc3 = _prog("odd", build_odd_kernel)
    r3 = _run(nc3, [odd_core_inputs(inp, x2[c // 4], c % 4) for c in range(NCORES)])
    yT2 = _assemble_y(r3)
    x2flat = x2.reshape(2 * SEQ, D)
    w_r = np.ascontiguousarray(inp["moe_w_router"][0].reshape(8, 128, 8).transpose(1, 0, 2)).reshape(128, 64)
    nc4 = _prog("moe", lambda: build_tok_kernel("moe"))
    r4 = _run(nc4, [{"xin": np.ascontiguousarray(x2flat[c * 2048:(c + 1) * 2048]), "yT": yT2[c],
                     "w_out": inp["odd_w_out"][0], "g_norm": inp["odd_norm_ffn"][0],
                     "w_gu": inp["moe_w_gate_up"][0], "w_d": inp["moe_w_down"][0], "ident": ident,
                     "w_r": w_r, "g_fin": inp["final_norm"]}
                    for c in range(NCORES)])
    out = np.concatenate([r4[c]["xout"] for c in range(NCORES)], axis=0).reshape(2, SEQ, D)
    return out.astype(np.float32)


RG = [[0, 1, 2, 3], [4, 5, 6, 7]]


def build_fused():
    nc = bass.Bass("TRN2", target_bir_lowering=False)
    ov = {}
    names = {"in": [], "out": []}

    def tensor(name, shape, dt, kind):
        if name in ov:
            return ov[name]
        names["in" if kind == "ExternalInput" else "out"].append(name)
        return nc.dram_tensor(name, shape, dt, kind=kind)

    fused = {"nc": nc, "tensor": tensor, "semstack": "raw"}
    y1 = nc.dram_tensor("i_y1", [SEQ, 256], BF16)
    y1g = nc.dram_tensor("i_y1g", [4 * SEQ, 256], BF16)
    x2s = nc.dram_tensor("i_x2s", [2048, D], F32)
    x2h = nc.dram_tensor("i_x2h", [2048, D], BF16)
    x2g = nc.dram_tensor("i_x2g", [SEQ, D], BF16)
    y2 = nc.dram_tensor("i_y2", [SEQ, 256], BF16)
    y2g = nc.dram_tensor("i_y2g", [4 * SEQ, 256], BF16)

    gspec = {"y1": (y1, y1g, 2048), "x2": (x2h, x2g, 512), "y2": (y2, y2g, 2048)}

    def gather_piece(p, which, j, read_tokens):
        src, dst, rows = gspec[which]
        p.dma_fn("pool", lambda e: e.collective_compute(
            "AllGather", ALU.bypass, replica_groups=RG,
            ins=[src.ap()[j * rows:(j + 1) * rows, :].opt()],
            outs=[dst.ap()[j * 4 * rows:(j + 1) * 4 * rows, :].opt()]),
            reads=read_tokens, writes=[("gath", which, j)], key=("cc", which, j), inc=1)

    fused["gather_piece"] = gather_piece
    ov["ev_y"] = y1
    with nc.cleanup_on_exit():
        build_even_kernel(fused=fused)
    ov["tk0_yT"] = y1g
    ov["tk0_xout"] = x2s
    ov["tk0_hnout"] = x2h
    with nc.cleanup_on_exit():
        build_tok_kernel("ffn", fused=fused)
    ov["od_x"] = x2g
    ov["od_y"] = y2
    with nc.cleanup_on_exit():
        build_odd_kernel(fused=fused)
    ov["tk1_xin"] = x2s
    ov["tk1_yT"] = y2g
    with nc.cleanup_on_exit():
        build_tok_kernel("moe", fused=fused)
    return nc, names


def kernel(**inp):
    inp = {k: np.asarray(v) for k, v in inp.items()}
    ident = np.eye(128, dtype=np.float32)
    x = np.ascontiguousarray(inp["x"], dtype=np.float32)
    xflat = x.reshape(2 * SEQ, D)
    if "fused" not in _PROGS:
        _PROGS["fused"] = build_fused()
    nc, names = _PROGS["fused"]
    w_r = np.ascontiguousarray(inp["moe_w_router"][0].reshape(8, 128, 8).transpose(1, 0, 2)).reshape(128, 64)
    in_maps = []
    for c in range(NCORES):
        b, h = c // 4, c % 4
        m = {}
        for k, v in even_core_inputs(inp, b, h).items():
            m["ev_" + k] = v
        od = odd_core_inputs(inp, None, h)
        for k, v in od.items():
            if k != "x":
                m["od_" + k] = v
        m.update({"tk0_xin": np.ascontiguousarray(xflat[c * 2048:(c + 1) * 2048]),
                  "tk0_w_out": inp["even_w_out"][0], "tk0_g_norm": inp["even_norm_ffn"][0],
                  "tk0_w_gu": inp["ffn_w_gate_up"], "tk0_w_d": inp["ffn_w_down"], "tk0_ident": ident,
                  "tk0_g_next": inp["odd_norm_mix"][0]})
        m.update({"tk1_w_out": inp["odd_w_out"][0], "tk1_g_norm": inp["odd_norm_ffn"][0],
                  "tk1_w_gu": inp["moe_w_gate_up"][0], "tk1_w_d": inp["moe_w_down"][0], "tk1_ident": ident,
                  "tk1_w_r": w_r, "tk1_g_fin": inp["final_norm"]})
        assert set(m.keys()) == set(names["in"]), (set(m.keys()) ^ set(names["in"]))
        in_maps.append(m)
    res = run_bass_kernel_spmd(nc, in_maps, core_ids=list(range(NCORES)))
    out = np.concatenate([res.results[c]["tk1_xout"] for c in range(NCORES)], axis=0).reshape(2, SEQ, D)
    return out.astype(np.float32)
```
